# Optimizing a Trainium2 kernel written in Bass

```python
import jax, jax.numpy as jnp
from jax import lax
import numpy as np

D_MODEL = 2048
BATCH = 2
SEQ = 4096
DEPTH = 2

RWKV_HEADS = 16
RWKV_HEAD_DIM = 64
D_RWKV = RWKV_HEADS * RWKV_HEAD_DIM
DECAY_LORA = 64
ICLR_LORA = 64
GATE_LORA = 160
LNX_EPS = 64e-5
POOL_WINDOWS = (2, 4, 8, 16)
N_POOL_GROUPS = 4
POOL_GROUP_DIM = 256
D_POOL = N_POOL_GROUPS * POOL_GROUP_DIM
N_EXPERTS = 16
EXPERT_FF = 2816
EC_CAPACITY_FACTOR = 2
NORM_EPS = 1e-6
D_SHIFT = 3 * D_RWKV + 2 * DECAY_LORA + 2 * ICLR_LORA + GATE_LORA
D_IN = D_SHIFT + D_POOL + 2 * D_MODEL

kernel_name = "hybrid_rwkv7_pool_ec_encoder"


def rms_norm(x, g):
    xf = x.astype(jnp.float32)
    y = xf * lax.rsqrt(jnp.mean(xf * xf, axis=-1, keepdims=True) + NORM_EPS)
    return (y * g.astype(jnp.float32)).astype(x.dtype)


def centred_shift(p):
    prev = jnp.pad(p[:, :-1], ((0, 0), (1, 0), (0, 0)))
    nxt = jnp.pad(p[:, 1:], ((0, 0), (0, 1), (0, 0)))
    return 0.5 * (prev + nxt) - p


def _heads(t):
    return t.reshape(t.shape[:-1] + (RWKV_HEADS, RWKV_HEAD_DIM))


def _dir_stack(f, b):
    return jnp.stack([f, jnp.flip(b, axis=1)], axis=0)


def rwkv7_scan(r, w, kk, b, k, v):
    xs = tuple(jnp.moveaxis(t, 2, 0) for t in (r, w, kk, b, k, v))
    n_dir, bsz, _, h, n = r.shape

    def step(S, inp):
        r_t, w_t, kk_t, b_t, k_t, v_t = inp
        sa = jnp.einsum("dbhvk,dbhk->dbhv", S, kk_t)
        S = (S * w_t[..., None, :]
             - sa[..., :, None] * b_t[..., None, :]
             + v_t[..., :, None] * k_t[..., None, :])
        y = jnp.einsum("dbhvk,dbhk->dbhv", S, r_t)
        return S, y

    S0 = jnp.zeros((n_dir, bsz, h, n, n), jnp.float32)
    _, y = lax.scan(step, S0, xs)
    return jnp.moveaxis(y, 0, 2)


def rwkv7_branch(ps, w0, w_lora_up, a0, a_lora_up, g_lora_up, k_k, k_a, r_k, lnx_g, lnx_b):
    f32 = jnp.float32
    ps = ps.astype(f32)
    cuts = [int(c) for c in np.cumsum([D_RWKV, D_RWKV, D_RWKV, DECAY_LORA, DECAY_LORA, ICLR_LORA, ICLR_LORA])]
    r, k, v, wd_f, wd_b, ad_f, ad_b, gd = jnp.split(ps, cuts, axis=-1)
    wd = jnp.stack([wd_f, wd_b], axis=0)
    ad = jnp.stack([ad_f, ad_b], axis=0)
    w_pre = w0.astype(f32)[:, None, None, :] + jnp.einsum("dbtl,dlc->dbtc", jnp.tanh(wd), w_lora_up.astype(f32))
    decay = jnp.exp(-jnp.exp(-jax.nn.softplus(-w_pre) - 0.5))
    a = jax.nn.sigmoid(a0.astype(f32)[:, None, None, :] + jnp.einsum("dbtl,dlc->dbtc", ad, a_lora_up.astype(f32)))
    g = jnp.einsum("btl,lc->btc", jax.nn.sigmoid(gd), g_lora_up.astype(f32))
    kk = _heads(k * k_k.astype(f32))
    kk = kk / jnp.maximum(jnp.sqrt(jnp.sum(kk * kk, axis=-1, keepdims=True)), 1e-12)
    k_dir = k[None] * (1.0 + (a - 1.0) * k_a.astype(f32))
    a_h, k_h, dec_h = _heads(a), _heads(k_dir), _heads(decay)
    r_h, v_h = _heads(r), _heads(v)
    y2 = rwkv7_scan(
        _dir_stack(r_h, r_h),
        _dir_stack(dec_h[0], dec_h[1]),
        _dir_stack(kk, kk),
        _dir_stack(kk * a_h[0], kk * a_h[1]),
        _dir_stack(k_h[0], k_h[1]),
        _dir_stack(v_h, v_h),
    )
    y = y2[0] + jnp.flip(y2[1], axis=1)
    mu = jnp.mean(y, axis=-1, keepdims=True)
    var = jnp.mean(jnp.square(y - mu), axis=-1, keepdims=True)
    y = (y - mu) * lax.rsqrt(var + LNX_EPS)
    y = y.reshape(y.shape[:2] + (D_RWKV,)) * lnx_g.astype(f32) + lnx_b.astype(f32)
    k_bonus = 0.5 * (k_h[0] + k_h[1])
    bonus = jnp.sum(r_h * k_bonus * r_k.astype(f32), axis=-1, keepdims=True) * v_h
    y = (y + bonus.reshape(y.shape)) * g
    return y


def centred_pool_minus_self(p, window):
    bsz, t_len, c = p.shape
    pf = p.astype(jnp.float32)
    cs = jnp.concatenate([jnp.zeros((bsz, 1, c), jnp.float32), jnp.cumsum(pf, axis=1)], axis=1)
    t = jnp.arange(t_len)
    half = window // 2
    lo = jnp.clip(t - half, 0, t_len)
    hi = jnp.clip(t + half, 0, t_len)
    cnt = (hi - lo).astype(jnp.float32)
    mean = (jnp.take(cs, hi, axis=1) - jnp.take(cs, lo, axis=1)) / cnt[None, :, None]
    return mean - pf


def pool_branch(p, pool_w, pool_scale):
    groups = jnp.split(p, N_POOL_GROUPS, axis=-1)
    z = jnp.stack([centred_pool_minus_self(gp, w) for gp, w in zip(groups, POOL_WINDOWS)], axis=2)
    y = jnp.einsum("btgc,gcd->btgd", z, pool_w.astype(jnp.float32))
    y = y.reshape(y.shape[:2] + (D_POOL,)) * pool_scale.astype(jnp.float32)
    return y.astype(p.dtype)


def ec_moe(u, w_router, w_gate, w_up, w_down):
    bsz, t_len, _ = u.shape
    cap = EC_CAPACITY_FACTOR * t_len // N_EXPERTS
    aff = jax.nn.softmax(jnp.einsum("btd,de->bte", u, w_router).astype(jnp.float32), axis=-1)
    gates, idx = lax.top_k(jnp.swapaxes(aff, 1, 2), cap)
    bidx = jnp.arange(bsz)[:, None, None]
    xs = u[bidx, idx]
    h = jax.nn.silu(jnp.einsum("becd,edf->becf", xs, w_gate)) * jnp.einsum("becd,edf->becf", xs, w_up)
    y = jnp.einsum("becf,efd->becd", h, w_down) * gates[..., None].astype(u.dtype)
    return jnp.zeros_like(u).at[bidx, idx].add(y)


def setup_inputs(seed: int = 0) -> dict:
    key = jax.random.key(seed)
    ks = jax.random.split(key, 32)
    f32 = jnp.float32
    L, D = DEPTH, D_MODEL

    def nrm(k, shape, scale):
        return jax.random.normal(k, shape, f32) * scale

    w0_base = jnp.tile(jnp.linspace(-6.0, 1.0, RWKV_HEAD_DIM, dtype=f32), RWKV_HEADS)
    return {
        "x": nrm(ks[0], (BATCH, SEQ, D), 1.0),
        "norm_mix_g": 1.0 + nrm(ks[1], (L, D), 0.02),
        "w_in": nrm(ks[2], (L, D, D_IN), D ** -0.5),
        "mu_shift": jax.random.uniform(ks[3], (L, D_SHIFT), f32, 0.2, 0.8),
        "w0": w0_base + nrm(ks[4], (L, 2, D_RWKV), 0.1),
        "w_lora_up": nrm(ks[5], (L, 2, DECAY_LORA, D_RWKV), 0.1 * DECAY_LORA ** -0.5),
        "a0": nrm(ks[6], (L, 2, D_RWKV), 0.1),
        "a_lora_up": nrm(ks[7], (L, 2, ICLR_LORA, D_RWKV), 0.1 * ICLR_LORA ** -0.5),
        "g_lora_up": nrm(ks[8], (L, GATE_LORA, D_RWKV), GATE_LORA ** -0.5),
        "k_k": 0.85 + nrm(ks[9], (L, D_RWKV), 0.05),
        "k_a": 1.0 + nrm(ks[10], (L, D_RWKV), 0.05),
        "r_k": nrm(ks[11], (L, RWKV_HEADS, RWKV_HEAD_DIM), 0.1),
        "lnx_g": 1.0 + nrm(ks[12], (L, D_RWKV), 0.02),
        "lnx_b": nrm(ks[13], (L, D_RWKV), 0.02),
        "pool_w": nrm(ks[14], (L, N_POOL_GROUPS, POOL_GROUP_DIM, POOL_GROUP_DIM), POOL_GROUP_DIM ** -0.5),
        "pool_scale": 1.0 + nrm(ks[15], (L, D_POOL), 0.1),
        "w_up_a": nrm(ks[16], (L, D_RWKV, D), D_RWKV ** -0.5),
        "w_up_b": nrm(ks[17], (L, D_POOL, D), D_POOL ** -0.5),
        "w_o": nrm(ks[18], (L, D, D), D ** -0.5),
        "norm_moe_g": 1.0 + nrm(ks[19], (L, D), 0.02),
        "w_router": nrm(ks[20], (L, D, N_EXPERTS), D ** -0.5),
        "w_gate_e": nrm(ks[21], (L, N_EXPERTS, D, EXPERT_FF), D ** -0.5),
        "w_up_e": nrm(ks[22], (L, N_EXPERTS, D, EXPERT_FF), D ** -0.5),
        "w_down_e": nrm(ks[23], (L, N_EXPERTS, EXPERT_FF, D), EXPERT_FF ** -0.5),
        "final_g": 1.0 + nrm(ks[24], (D,), 0.02),
    }


def reference(x, norm_mix_g, w_in, mu_shift, w0, w_lora_up, a0, a_lora_up, g_lora_up, k_k, k_a, r_k,
              lnx_g, lnx_b, pool_w, pool_scale, w_up_a, w_up_b, w_o, norm_moe_g, w_router,
              w_gate_e, w_up_e, w_down_e, final_g):
    for l in range(DEPTH):
        u = rms_norm(x, norm_mix_g[l])
        proj = jnp.einsum("btd,dn->btn", u, w_in[l])
        p_rwkv = proj[..., :D_SHIFT]
        p_rwkv = p_rwkv + mu_shift[l].astype(p_rwkv.dtype) * centred_shift(p_rwkv)
        p_pool = proj[..., D_SHIFT:D_SHIFT + D_POOL]
        gate_a = proj[..., D_SHIFT + D_POOL:D_SHIFT + D_POOL + D_MODEL]
        gate_b = proj[..., D_SHIFT + D_POOL + D_MODEL:]
        y_a = rwkv7_branch(p_rwkv, w0[l], w_lora_up[l], a0[l], a_lora_up[l], g_lora_up[l],
                           k_k[l], k_a[l], r_k[l], lnx_g[l], lnx_b[l]).astype(x.dtype)
        y_b = pool_branch(p_pool, pool_w[l], pool_scale[l])
        merged = (jax.nn.sigmoid(gate_a) * jnp.einsum("btc,cd->btd", y_a, w_up_a[l])
                  + jax.nn.sigmoid(gate_b) * jnp.einsum("btc,cd->btd", y_b, w_up_b[l]))
        x = x + jnp.einsum("btd,de->bte", merged, w_o[l])
        v = rms_norm(x, norm_moe_g[l])
        x = x + ec_moe(v, w_router[l], w_gate_e[l], w_up_e[l], w_down_e[l])
    return rms_norm(x, final_g)
```

```python
from contextlib import ExitStack
import concourse.bass as bass
import concourse.mybir as mybir

F32 = mybir.dt.float32
F32R = mybir.dt.float32r
BF16 = mybir.dt.bfloat16
I32 = mybir.dt.int32
U32 = mybir.dt.uint32
AF = mybir.ActivationFunctionType
ALU = mybir.AluOpType
AX = mybir.AxisListType

ENGS = ("pe", "dve", "act", "pool", "sp")
N_DMA_SEMS = 48


class Res:
    __slots__ = ("name", "w", "r")

    def __init__(self, name):
        self.name = name
        self.w = None
        self.r = []


class Instr:
    __slots__ = ("eng", "fn", "deps", "stream", "seq", "waited", "val", "is_dma", "known")

    def __init__(self, eng, fn):
        self.eng = eng
        self.fn = fn
        self.deps = []
        self.stream = None
        self.seq = 0
        self.waited = False
        self.val = 0
        self.is_dma = False
        self.known = None


class KB:
    _count = 0

    def __init__(self, nc, same_engine_sync=True):
        KB._count += 1
        self.tag = f"k{KB._count}_"
        self.nc = nc
        self.es = ExitStack()
        self.q = {e: [] for e in ENGS}
        self.stream_last = {}
        self.stream_cnt = {}
        self.dma_rr = 0
        self.same_engine_sync = same_engine_sync
        self.n_res = 0
        self.out_dmas = []

    def sb(self, name, shape, dtype=F32):
        return self.es.enter_context(self.nc.sbuf_tensor(self.tag + name, list(shape), dtype))

    def ps(self, name, shape, dtype=F32):
        return self.es.enter_context(self.nc.psum_tensor(self.tag + name, list(shape), dtype))

    def res(self, name=None):
        self.n_res += 1
        return Res(name or f"r{self.n_res}")

    def _known_after(self, d):
        k = dict(d.known)
        if k.get(d.stream, 0) < d.seq:
            k[d.stream] = d.seq
        return k

    def _add(self, ins, reads, writes, pe_accum=False):
        eng = ins.eng
        deps = []
        for r in reads:
            if r.w is not None:
                deps.append(r.w)
        for w in writes:
            if w.w is not None:
                if not (pe_accum and w.w.eng == "pe" and not w.w.is_dma):
                    deps.append(w.w)
            deps.extend(w.r)
        prev = self.q[eng][-1] if self.q[eng] else None
        known = dict(prev.known) if prev is not None else {}
        if ins.is_dma:
            p = self.stream_last.get(ins.stream)
            if p is not None:
                deps.append(p)
        final = []
        deps = sorted(set(deps), key=lambda d: -d.seq)
        for d in deps:
            if d is ins:
                continue
            if (not d.is_dma) and d.eng == eng and not self.same_engine_sync:
                continue
            if known.get(d.stream, 0) >= d.seq:
                continue
            final.append(d)
            d.waited = True
            ka = self._known_after(d)
            for s, v in ka.items():
                if known.get(s, 0) < v:
                    known[s] = v
        ins.deps = final
        ins.known = known
        self.q[eng].append(ins)
        for r in reads:
            r.r.append(ins)
        for w in writes:
            w.w = ins
            w.r = []
        return ins

    def op(self, eng, fn, reads=(), writes=(), pe_accum=False):
        if eng == "pool":
            eng = "dve"
        ins = Instr(eng, fn)
        ins.stream = eng
        self.stream_cnt[eng] = self.stream_cnt.get(eng, 0) + 1
        ins.seq = self.stream_cnt[eng]
        self._add(ins, list(reads), list(writes), pe_accum=pe_accum)
        self.stream_last[eng] = ins
        return ins

    def dma(self, out, in_, reads=(), writes=(), eng="sp", is_output=False, **kw):
        if eng == "pool":
            eng = "sp"
        fn = lambda e: e.dma_start(out=out, in_=in_, **kw)
        ins = Instr(eng, fn)
        ins.is_dma = True
        slot = self.dma_rr % N_DMA_SEMS
        self.dma_rr += 1
        ins.stream = ("dma", slot)
        self.stream_cnt[ins.stream] = self.stream_cnt.get(ins.stream, 0) + 1
        ins.seq = self.stream_cnt[ins.stream]
        ins.waited = True
        self._add(ins, list(reads), list(writes))
        self.stream_last[ins.stream] = ins
        if is_output:
            self.out_dmas.append(ins)
        return ins

    def gen(self, eng, fn, reads=(), writes=()):
        return self.op(eng, fn, reads, writes)

    def emit(self):
        nc = self.nc
        lastd = [v for k, v in self.stream_last.items() if isinstance(k, tuple)]
        if lastd:
            self.out_dmas = lastd
            fin = Instr("sp", None)
            fin.stream = "sp"
            self.stream_cnt["sp"] = self.stream_cnt.get("sp", 0) + 1
            fin.seq = self.stream_cnt["sp"]
            prev = self.q["sp"][-1] if self.q["sp"] else None
            known = dict(prev.known) if prev is not None else {}
            for d in self.out_dmas:
                if known.get(d.stream, 0) < d.seq:
                    fin.deps.append(d)
            fin.known = known
            self.q["sp"].append(fin)
        sems = {}
        for e in ENGS:
            sems[e] = self.es.enter_context(nc.semaphore(self.tag + f"s_{e}"))
            v = 0
            for ins in self.q[e]:
                if ins.is_dma:
                    continue
                if ins.waited:
                    v += 1
                    ins.val = v
        for s in range(N_DMA_SEMS):
            sems[("dma", s)] = self.es.enter_context(nc.semaphore(self.tag + f"s_dma{s}"))
        engmap = {"pe": "tensor", "dve": "vector", "act": "scalar", "pool": "gpsimd", "sp": "sync"}
        q = self.q

        def run(ename, e):
            for ins in q[ename]:
                for d in ins.deps:
                    if d.is_dma:
                        e.wait_ge(sems[d.stream], 16 * d.seq)
                    else:
                        e.wait_ge(sems[d.stream], d.val)
                if ins.fn is None:
                    continue
                r = ins.fn(e)
                if ins.is_dma:
                    r.then_inc(sems[ins.stream], 16)
                elif ins.waited:
                    r.then_inc(sems[ins.stream], 1)

        allsems = list(sems.values())
        with nc.Block() as cblock:
            @cblock.gpsimd
            def _(e):
                for sm in allsems:
                    e.sem_clear(sm)

        with nc.Block() as block:
            @block.tensor
            def _(e):
                run("pe", e)

            @block.vector
            def _(e):
                run("dve", e)

            @block.scalar
            def _(e):
                run("act", e)

            @block.gpsimd
            def _(e):
                run("pool", e)

            @block.sync
            def _(e):
                run("sp", e)
        self.es.close()

    def stats(self):
        return {e: len(self.q[e]) for e in ENGS}


import numpy as np
import concourse.bass as bass
import concourse.mybir as mybir

D = 2048
T = 4096
D_IN = 8608
D_SHIFT = 3488
EPS = 1e-6
NE = 16
FF = 2816
CAP = 512


class Tl:
    def __init__(self, kb, name, shape, dtype=F32):
        self.t = kb.sb(name, shape, dtype)
        self.r = kb.res(name)

    def __getitem__(self, k):
        return self.t[k]


class PS:
    def __init__(self, kb, n=8):
        self.kb = kb
        self.t = [kb.ps(f"ps{i}", [128, 512]) for i in range(n)]
        self.r = [kb.res(f"ps{i}") for i in range(n)]
        self.i = 0
        self.n = n

    def next(self):
        i = self.i % self.n
        self.i += 1
        return self.t[i], self.r[i]


def rs(*tiles):
    return [x.r if hasattr(x, "r") and not isinstance(x, Res) else x for x in tiles]


def col_chunks():
    ch = [(i * 128, 128) for i in range(27)] + [(3456, 32)] + [(3488 + i * 128, 128) for i in range(40)]
    return ch


def stage_inproj(nc, xsrc, g_ap, w_ap, projT):
    kb = KB(nc)
    KC = D // 128
    NT = 1024
    ut = kb.sb("ut", [128, KC, NT], F32R)
    ut_r = [kb.res(f"ut{k}") for k in range(KC)]
    xa = [Tl(kb, f"xa{i}", [128, NT]) for i in range(2)]
    gt = Tl(kb, "gt", [128, KC])
    onesf = Tl(kb, "onesf", [128, 128])
    ones = Tl(kb, "ones", [128, 128], F32R)
    sq = [Tl(kb, f"sq{i}", [128, NT], F32R) for i in range(2)]
    rstd = Tl(kb, "rstd", [128, NT])
    wt = [Tl(kb, f"wt{i}", [128, KC, 512], F32R) for i in range(2)]
    ot = [Tl(kb, f"ot{i}", [128, NT]) for i in range(2)]
    ps = PS(kb)
    kb.op("pool", lambda e: e.memset(onesf[:], 1.0), writes=rs(onesf))
    kb.op("dve", lambda e: e.tensor_copy(out=ones[:], in_=onesf[:]), reads=rs(onesf), writes=rs(ones))
    kb.dma(gt[:], g_ap.rearrange("(k p) -> p k", p=128), writes=rs(gt), eng="sp", allow_slow_non_contiguous=True)
    xv = xsrc.rearrange("(k p) n -> p k n", p=128)
    wv = w_ap.rearrange("(k p) n -> p k n", p=128)
    chunks = col_chunks()
    groups = [chunks[i:i + 4] for i in range(0, len(chunks), 4)]
    gi_glob = 0
    ci = 0
    for s in range(T // NT):
        tsl = slice(s * NT, (s + 1) * NT)
        p0t, p0r = ps.next()
        p1t, p1r = ps.next()
        pss = [(p0t, p0r), (p1t, p1r)]
        for k in range(KC):
            a = k % 2
            kb.dma(xa[a][:], xv[:, k, tsl], writes=rs(xa[a]), eng="sp")
            kb.op("act", lambda e, a=a: e.activation(out=sq[a][:], in_=xa[a][:], func=AF.Square),
                  reads=rs(xa[a]), writes=rs(sq[a]))
            for h in range(2):
                kb.op("pe", lambda e, k=k, a=a, h=h, pt=pss[h][0]: e.matmul(pt[:], lhsT=ones[:], rhs=sq[a][:, h * 512:(h + 1) * 512],
                                                                        start=(k == 0), stop=(k == KC - 1)),
                      reads=rs(ones, sq[a]), writes=[pss[h][1]], pe_accum=True)
        for h in range(2):
            sl = slice(h * 512, (h + 1) * 512)
            kb.op("act", lambda e, h=h, sl=sl, pt=pss[h][0]: e.activation(out=rstd[:, sl], in_=pt[:], func=AF.Sqrt, scale=1.0 / D, bias=EPS),
                  reads=[pss[h][1]], writes=rs(rstd))
        kb.op("dve", lambda e: e.reciprocal(out=rstd[:], in_=rstd[:]), writes=rs(rstd))
        for k in range(KC):
            a = k % 2
            kb.dma(xa[a][:], xv[:, k, tsl], writes=rs(xa[a]), eng="sp")
            kb.op("dve", lambda e, k=k, a=a: e.scalar_tensor_tensor(out=ut[:, k, :], in0=xa[a][:], scalar=gt[:, k:k + 1], in1=rstd[:],
                                                                op0=ALU.mult, op1=ALU.mult),
                  reads=rs(gt, rstd, xa[a]), writes=[ut_r[k]])
        for grp in groups:
            b = gi_glob % 2
            c0 = grp[0][0]
            c1 = grp[-1][0] + grp[-1][1]
            kb.dma(wt[b][:, :, 0:c1 - c0], wv[:, :, c0:c1], writes=rs(wt[b]), eng=("sp" if gi_glob % 2 == 0 else "act"))
            gi_glob += 1
            for (cs, cw) in grp:
                ob = ci % 2
                for h in range(2):
                    pt, pr = ps.next()
                    for k in range(KC):
                        if cw == 128:
                            lhsT = wt[b][:, k, cs - c0:cs - c0 + cw]
                            rhs = ut[:, k, h * 512:(h + 1) * 512]
                        else:
                            lhsT = wt[b][:, k, cs - c0:cs - c0 + cw].bitcast(F32)
                            rhs = ut[:, k, h * 512:(h + 1) * 512].bitcast(F32)
                        kb.op("pe", lambda e, pt=pt, lhsT=lhsT, rhs=rhs, k=k, cw=cw: e.matmul(pt[0:cw, :], lhsT=lhsT, rhs=rhs,
                                                                                       start=(k == 0), stop=(k == KC - 1)),
                              reads=[wt[b].r, ut_r[k]], writes=[pr], pe_accum=True)
                    sl = slice(h * 512, (h + 1) * 512)
                    if cs >= 4512:
                        kb.op("act", lambda e, ob=ob, pt=pt, sl=sl, cw=cw: e.activation(out=ot[ob][0:cw, sl], in_=pt[0:cw, :], func=AF.Sigmoid),
                              reads=[pr], writes=rs(ot[ob]))
                    elif h == 0:
                        kb.op("act", lambda e, ob=ob, pt=pt, sl=sl, cw=cw: e.copy(out=ot[ob][0:cw, sl], in_=pt[0:cw, :]),
                              reads=[pr], writes=rs(ot[ob]))
                    else:
                        kb.op("dve", lambda e, ob=ob, pt=pt, sl=sl, cw=cw: e.tensor_copy(out=ot[ob][0:cw, sl], in_=pt[0:cw, :]),
                              reads=[pr], writes=rs(ot[ob]))
                kb.dma(projT[cs:cs + cw, tsl], ot[ob][0:cw, :], reads=rs(ot[ob]), eng="pool", is_output=True)
                ci += 1
    kb.emit()


def load_col(kb, tile, ap1d, eng="sp"):
    n = ap1d.shape[0]
    kb.dma(tile[0:n, 0:1], ap1d.rearrange("(p o) -> p o", o=1), writes=rs(tile), eng=eng)


def load_halo(kb, P, src_rows, t0, S, eng="sp"):
    n = src_rows.shape[0]
    lo = max(t0 - 1, 0)
    hi = min(t0 + S + 1, T)
    kb.dma(P[0:n, lo - (t0 - 1):hi - (t0 - 1)], src_rows[:, lo:hi], writes=rs(P), eng=eng)
    if t0 == 0:
        kb.op("pool", lambda e: e.memset(P[0:n, 0:1], 0.0), writes=rs(P))
    if t0 + S == T:
        kb.op("pool", lambda e: e.memset(P[0:n, S + 1:S + 2], 0.0), writes=rs(P))


def shift_rows(kb, out_ap, P, tmp, hmu, omu, n, S, out_res):
    kb.op("pool", lambda e: e.tensor_tensor(out=tmp[0:n, :], in0=P[0:n, 0:S], in1=P[0:n, 2:S + 2], op=ALU.add),
          reads=rs(P), writes=rs(tmp))
    kb.op("pool", lambda e: e.tensor_scalar(out=tmp[0:n, :], in0=tmp[0:n, :], scalar1=hmu[0:n, 0:1], scalar2=None, op0=ALU.mult),
          reads=rs(hmu), writes=rs(tmp))
    kb.op("dve", lambda e: e.scalar_tensor_tensor(out=out_ap, in0=P[0:n, 1:S + 1], scalar=omu[0:n, 0:1], in1=tmp[0:n, :],
                                                  op0=ALU.mult, op1=ALU.add),
          reads=rs(P, omu, tmp), writes=[out_res])


def mu_cols(kb, mu, hmu, omu, ap1d, n):
    load_col(kb, mu, ap1d)
    kb.op("dve", lambda e: e.tensor_scalar(out=hmu[0:n, :], in0=mu[0:n, :], scalar1=0.5, scalar2=None, op0=ALU.mult),
          reads=rs(mu), writes=rs(hmu))
    kb.op("dve", lambda e: e.tensor_scalar(out=omu[0:n, :], in0=mu[0:n, :], scalar1=-1.0, scalar2=1.0, op0=ALU.mult, op1=ALU.add),
          reads=rs(mu), writes=rs(omu))


def stage_lora(nc, projT, mu_ap, loraA):
    kb = KB(nc)
    S = 1024
    P = [Tl(kb, f"P{i}", [128, S + 2]) for i in range(2)]
    tmp = Tl(kb, "tmp", [128, S])
    sh = Tl(kb, "sh", [128, S])
    o = [Tl(kb, f"o{i}", [128, S], F32R) for i in range(2)]
    mu = [Tl(kb, f"mu{i}", [128, 1]) for i in range(4)]
    hmu = [Tl(kb, f"hmu{i}", [128, 1]) for i in range(4)]
    omu = [Tl(kb, f"omu{i}", [128, 1]) for i in range(4)]
    blocks = [(0, 128, AF.Tanh), (128, 128, AF.Copy), (256, 128, AF.Sigmoid), (384, 32, AF.Sigmoid)]
    for bi, (r0, n, fn) in enumerate(blocks):
        mu_cols(kb, mu[bi], hmu[bi], omu[bi], mu_ap[3072 + r0:3072 + r0 + n], n)
    it = 0
    for s in range(T // S):
        t0 = s * S
        for bi, (r0, n, fn) in enumerate(blocks):
            p = P[it % 2]
            oo = o[it % 2]
            it += 1
            load_halo(kb, p, projT[3072 + r0:3072 + r0 + n, :], t0, S)
            shift_rows(kb, sh[0:n, :], p, tmp, hmu[bi], omu[bi], n, S, sh.r)
            if fn == AF.Copy:
                kb.op("act", lambda e, oo=oo, n=n: e.copy(out=oo[0:n, :], in_=sh[0:n, :]), reads=rs(sh), writes=rs(oo))
            else:
                kb.op("act", lambda e, oo=oo, n=n, fn=fn: e.activation(out=oo[0:n, :], in_=sh[0:n, :], func=fn), reads=rs(sh), writes=rs(oo))
            kb.dma(loraA[r0:r0 + n, t0:t0 + S], oo[0:n, :], reads=rs(oo), eng="act")
    kb.emit()


CEXP = float(np.exp(-0.5))


def stage_rwkv(nc, projT, loraA, yfT, yaT, W, cst, hp_list=range(8), dbg=None):
    kb = KB(nc)
    S = 1024
    NCH = S // 64
    ps = PS(kb)
    cf = {k: Tl(kb, "c_" + k, [128, 512], BF16) for k in ("m_up", "m_lo", "m_upi", "m_loi", "itile")}
    cstage = Tl(kb, "cstage", [128, 512])
    for k in cf:
        kb.dma(cstage[:], cst[k], writes=rs(cstage))
        kb.op("dve", lambda e, k=k: e.tensor_copy(out=cf[k][:], in_=cstage[:]), reads=rs(cstage), writes=rs(cf[k]))
    identf = Tl(kb, "identf", [128, 64])
    ident = Tl(kb, "ident", [128, 64], BF16)
    kb.dma(identf[:], cst["ident"], writes=rs(identf))
    kb.op("dve", lambda e: e.tensor_copy(out=ident[:], in_=identf[:]), reads=rs(identf), writes=rs(ident))
    bonesf = Tl(kb, "bonesf", [128, 128])
    bones = Tl(kb, "bones", [128, 128], F32R)
    kb.dma(bonesf[:], cst["bones"], writes=rs(bonesf))
    kb.op("dve", lambda e: e.tensor_copy(out=bones[:], in_=bonesf[:]), reads=rs(bonesf), writes=rs(bones))
    cmask = Tl(kb, "cmask", [128, S])
    kb.dma(cmask[:], cst["cmask"], writes=rs(cmask))
    colnames = ["mu_r", "mu_k", "mu_v", "w0f", "w0b", "a0f", "a0b", "k_k", "k_a", "r_k", "lnx_g", "lnx_b"]
    col = {n: Tl(kb, "col_" + n, [128, 1]) for n in colnames}
    hmu = {n: Tl(kb, "h" + n, [128, 1]) for n in ("mu_r", "mu_k", "mu_v")}
    omu = {n: Tl(kb, "o" + n, [128, 1]) for n in ("mu_r", "mu_k", "mu_v")}
    omka = Tl(kb, "omka", [128, 1])
    hka = Tl(kb, "hka", [128, 1])
    zf = Tl(kb, "zf", [128, 128])
    kb.op("dve", lambda e: e.memset(zf[:], 0.0), writes=rs(zf))
    wlp = [Tl(kb, f"wlp{i}", [128, 128], F32R) for i in range(2)]
    alp = [Tl(kb, f"alp{i}", [128, 128], F32R) for i in range(2)]
    gl0 = Tl(kb, "gl0", [128, 128], F32R)
    gl1 = Tl(kb, "gl1", [128, 128], F32R)
    for i in range(2):
        oh = slice(64, 128) if i == 0 else slice(0, 64)
        kb.op("dve", lambda e, i=i, oh=oh: e.tensor_copy(out=wlp[i][oh, :], in_=zf[oh, :]), reads=rs(zf), writes=rs(wlp[i]))
        kb.op("dve", lambda e, i=i, oh=oh: e.tensor_copy(out=alp[i][oh, :], in_=zf[oh, :]), reads=rs(zf), writes=rs(alp[i]))
    for (pa_, pb_) in ((32, 64), (64, 128)):
        kb.op("dve", lambda e, pa_=pa_, pb_=pb_: e.tensor_copy(out=gl1[pa_:pb_, :], in_=zf[pa_:pb_, :]), reads=rs(zf), writes=rs(gl1))
    P2 = [Tl(kb, n, [128, S + 2]) for n in ("P2a", "P2b")]
    Pr, Pk, Pv = P2[0], P2[1], P2[0]
    names32 = ["r_", "k_", "v_", "tA", "tB", "sgw", "a_", "kk", "kkn", "b_", "Pp", "E_", "R_", "Sf", "yc", "po1", "po2", "po3"]
    F = {n: Tl(kb, n, [128, S]) for n in names32}
    F["lw"] = F["sgw"]
    F["kdir"] = F["kk"]
    F["yf"] = F["Sf"]
    F["af"] = F["E_"]
    sqr = Tl(kb, "sqr", [128, S], F32R)
    tw = Tl(kb, "tw", [128, S], F32R)
    adp = Tl(kb, "adp", [128, S], F32R)
    sg0 = Tl(kb, "sg0", [128, S], F32R)
    sg1 = Tl(kb, "sg1", [128, S], F32R)
    for q in range(S // 128):
        for (pa_, pb_) in ((32, 64), (64, 128)):
            kb.op("dve", lambda e, q=q, pa_=pa_, pb_=pb_: e.tensor_copy(out=sg1[pa_:pb_, q * 128:(q + 1) * 128], in_=zf[pa_:pb_, :]), reads=rs(zf), writes=rs(sg1))
    yout = sqr
    tot = Tl(kb, "tot", [128, NCH])
    wC = Tl(kb, "wC", [128, NCH])
    namesb = ["qt", "rt", "kt", "nbt", "Kh", "Bn", "vb"]
    Bf = {n: Tl(kb, n, [128, S], BF16) for n in namesb}
    pb_names = ["Vtm", "Qtm", "Khtm", "Bhtm", "Um", "Utm", "Pa", "Pb", "Pta", "Ptb", "Xa", "Xb", "AkkT", "ArkT", "nArbT", "AV", "Gtm", "Qh", "PhiT", "RhT"]
    PB = [{n: Tl(kb, f"{n}{bi}", [128, 512], BF16) for n in pb_names} for bi in range(2)]
    Psi = [Tl(kb, f"Psi{bi}", [128, 512]) for bi in range(2)]
    Hbf = Tl(kb, "Hbf", [128, 64], BF16)

    def ew(eng, fname, reads, writes, **kw):
        kb.op(eng, lambda e: getattr(e, fname)(**kw), reads=rs(*reads), writes=rs(*writes))

    def psbf(pt):
        return pt[:].bitcast(BF16)

    HS = [slice(0, 64), slice(64, 128)]

    for hp in hp_list:
        c0 = hp * 128
        csl = slice(c0, c0 + 128)
        load_col(kb, col["mu_r"], W["mu"][c0:c0 + 128])
        load_col(kb, col["mu_k"], W["mu"][1024 + c0:1024 + c0 + 128])
        load_col(kb, col["mu_v"], W["mu"][2048 + c0:2048 + c0 + 128])
        load_col(kb, col["w0f"], W["w0"][0, csl])
        load_col(kb, col["w0b"], W["w0"][1, csl])
        load_col(kb, col["a0f"], W["a0"][0, csl])
        load_col(kb, col["a0b"], W["a0"][1, csl])
        for n in ("k_k", "k_a", "r_k", "lnx_g", "lnx_b"):
            load_col(kb, col[n], W[n][csl])
        for n in ("mu_r", "mu_k", "mu_v"):
            ew("dve", "tensor_scalar", [col[n]], [hmu[n]], out=hmu[n][:], in0=col[n][:], scalar1=0.5, scalar2=None, op0=ALU.mult)
            ew("dve", "tensor_scalar", [col[n]], [omu[n]], out=omu[n][:], in0=col[n][:], scalar1=-1.0, scalar2=1.0, op0=ALU.mult, op1=ALU.add)
        ew("dve", "tensor_scalar", [col["k_a"]], [omka], out=omka[:], in0=col["k_a"][:], scalar1=-1.0, scalar2=1.0, op0=ALU.mult, op1=ALU.add)
        ew("dve", "tensor_scalar", [col["k_a"]], [hka], out=hka[:], in0=col["k_a"][:], scalar1=0.5, scalar2=None, op0=ALU.mult)
        for i in range(2):
            kb.dma(wlp[i][HS[i], :], W["wl"][i, :, csl], writes=rs(wlp[i]))
            kb.dma(alp[i][HS[i], :], W["al"][i, :, csl], writes=rs(alp[i]))
        kb.dma(gl0[:], W["gl"][0:128, csl], writes=rs(gl0))
        kb.dma(gl1[0:32, :], W["gl"][128:160, csl], writes=rs(gl1))
        for d in range(2):
            ds = HS[d]
            ew("pool", "memset", [], [Hbf], ap=Hbf[:], constant=0.0)
            slabs = list(range(T // S))
            if d == 1:
                slabs = slabs[::-1]
            for s in slabs:
                t0 = s * S
                tsl = slice(t0, t0 + S)
                load_halo(kb, Pr, projT[c0:c0 + 128, :], t0, S, eng="sp")
                load_halo(kb, Pk, projT[1024 + c0:1024 + c0 + 128, :], t0, S, eng="act")
                kb.dma(tw[:], loraA[0:128, tsl], writes=rs(tw), eng="act")
                kb.dma(adp[:], loraA[128:256, tsl], writes=rs(adp), eng="sp")
                shift_rows(kb, F["r_"][:], Pr, F["tA"], hmu["mu_r"], omu["mu_r"], 128, S, F["r_"].r)
                load_halo(kb, Pv, projT[2048 + c0:2048 + c0 + 128, :], t0, S, eng="sp")
                shift_rows(kb, F["k_"][:], Pk, F["tA"], hmu["mu_k"], omu["mu_k"], 128, S, F["k_"].r)
                shift_rows(kb, F["v_"][:], Pv, F["tA"], hmu["mu_v"], omu["mu_v"], 128, S, F["v_"].r)
                w0c = col["w0f"] if d == 0 else col["w0b"]
                a0c = col["a0f"] if d == 0 else col["a0b"]
                for hf in range(2):
                    sl = slice(hf * 512, (hf + 1) * 512)
                    pt, pr = ps.next()
                    kb.op("pe", lambda e, pt=pt, sl=sl, d=d: e.matmul(pt[:], lhsT=wlp[d][:], rhs=tw[:, sl], start=True, stop=True),
                          reads=rs(wlp[d], tw), writes=[pr])
                    kb.op("act", lambda e, pt=pt, sl=sl, w0c=w0c: e.activation(out=F["sgw"][:, sl], in_=pt[:], func=AF.Sigmoid, bias=w0c[:, 0:1]),
                          reads=[pr] + rs(w0c), writes=rs(F["sgw"]))
                    pt, pr = ps.next()
                    kb.op("pe", lambda e, pt=pt, sl=sl, d=d: e.matmul(pt[:], lhsT=alp[d][:], rhs=adp[:, sl], start=True, stop=True),
                          reads=rs(alp[d], adp), writes=[pr])
                    kb.op("act", lambda e, pt=pt, sl=sl, a0c=a0c: e.activation(out=F["a_"][:, sl], in_=pt[:], func=AF.Sigmoid, bias=a0c[:, 0:1]),
                          reads=[pr] + rs(a0c), writes=rs(F["a_"]))
                ew("dve", "tensor_scalar", [F["k_"], col["k_k"]], [F["kk"]], out=F["kk"][:], in0=F["k_"][:], scalar1=col["k_k"][:, 0:1], scalar2=None, op0=ALU.mult)
                ew("act", "activation", [F["kk"]], [sqr], out=sqr[:], in_=F["kk"][:], func=AF.Square)
                for hf in range(2):
                    sl = slice(hf * 512, (hf + 1) * 512)
                    pt, pr = ps.next()
                    kb.op("pe", lambda e, pt=pt, sl=sl: e.matmul(pt[:], lhsT=bones[:], rhs=sqr[:, sl], start=True, stop=True),
                          reads=rs(bones, sqr), writes=[pr])
                    kb.op("act", lambda e, pt=pt, sl=sl: e.activation(out=F["tB"][:, sl], in_=pt[:], func=AF.Sqrt),
                          reads=[pr], writes=rs(F["tB"]))
                ew("dve", "tensor_scalar", [F["tB"]], [F["tB"]], out=F["tB"][:], in0=F["tB"][:], scalar1=1e-12, scalar2=None, op0=ALU.max)
                ew("dve", "reciprocal", [F["tB"]], [F["tB"]], out=F["tB"][:], in_=F["tB"][:])
                ew("dve", "tensor_tensor", [F["kk"], F["tB"]], [F["kkn"]], out=F["kkn"][:], in0=F["kk"][:], in1=F["tB"][:], op=ALU.mult)
                ew("dve", "tensor_scalar", [F["a_"], col["k_a"], omka], [F["tB"]], out=F["tB"][:], in0=F["a_"][:], scalar1=col["k_a"][:, 0:1], scalar2=omka[:, 0:1], op0=ALU.mult, op1=ALU.add)
                ew("dve", "tensor_tensor", [F["k_"], F["tB"]], [F["kdir"]], out=F["kdir"][:], in0=F["k_"][:], in1=F["tB"][:], op=ALU.mult)
                ew("pool", "tensor_tensor", [F["kkn"], F["a_"]], [F["b_"]], out=F["b_"][:], in0=F["kkn"][:], in1=F["a_"][:], op=ALU.mult)
                ew("pool", "tensor_scalar", [F["sgw"]], [F["lw"]], out=F["lw"][:], in0=F["sgw"][:], scalar1=-CEXP, scalar2=None, op0=ALU.mult)
                ew("dve", "tensor_tensor_scan", [cmask, F["lw"]], [F["Pp"]], out=F["Pp"][:], data0=cmask[:], data1=F["lw"][:], initial=0.0, op0=ALU.mult, op1=ALU.add)
                ew("pool", "tensor_tensor", [F["Pp"], F["lw"]], [F["E_"]], out=F["E_"][:], in0=F["Pp"][:], in1=F["lw"][:], op=ALU.subtract)
                Pv3 = F["Pp"][:].rearrange("p (c t) -> p c t", t=64)
                ew("dve", "tensor_copy", [F["Pp"]], [tot], out=tot[:], in_=Pv3[:, :, 63])
                ew("dve", "tensor_tensor", [tot, F["Pp"]], [F["R_"]], out=F["R_"][:].rearrange("p (c t) -> p c t", t=64),
                   in0=tot[:].unsqueeze(2).to_broadcast([128, NCH, 64]), in1=Pv3, op=ALU.subtract)
                ew("act", "activation", [tot], [wC], out=wC[:], in_=tot[:], func=AF.Exp)
                if d == 0:
                    Lc, Lp, Lr = F["Pp"], F["E_"], F["R_"]
                else:
                    ew("dve", "tensor_tensor", [tot, F["E_"]], [F["Sf"]], out=F["Sf"][:].rearrange("p (c t) -> p c t", t=64),
                       in0=tot[:].unsqueeze(2).to_broadcast([128, NCH, 64]), in1=F["E_"][:].rearrange("p (c t) -> p c t", t=64), op=ALU.subtract)
                    Lc, Lp, Lr = F["Sf"], F["R_"], F["E_"]
                ew("act", "activation", [Lc], [F["po1"]], out=F["po1"][:], in_=Lc[:], func=AF.Exp)
                ew("act", "activation", [Lp], [F["po2"]], out=F["po2"][:], in_=Lp[:], func=AF.Exp)
                ew("act", "activation", [Lc], [F["po3"]], out=F["po3"][:], in_=Lc[:], func=AF.Exp, scale=-1.0)
                ew("act", "activation", [Lr], [F["tA"]], out=F["tA"][:], in_=Lr[:], func=AF.Exp)
                ew("dve", "tensor_tensor", [F["kkn"], F["po2"]], [Bf["qt"]], out=Bf["qt"][:], in0=F["kkn"][:], in1=F["po2"][:], op=ALU.mult)
                ew("pool", "tensor_tensor", [F["r_"], F["po1"]], [Bf["rt"]], out=Bf["rt"][:], in0=F["r_"][:], in1=F["po1"][:], op=ALU.mult)
                ew("dve", "tensor_tensor", [F["kdir"], F["po3"]], [Bf["kt"]], out=Bf["kt"][:], in0=F["kdir"][:], in1=F["po3"][:], op=ALU.mult)
                ew("dve", "scalar_tensor_tensor", [F["b_"], F["po3"]], [Bf["nbt"]], out=Bf["nbt"][:], in0=F["b_"][:], scalar=-1.0, in1=F["po3"][:], op0=ALU.mult, op1=ALU.mult)
                ew("pool", "tensor_tensor", [F["kdir"], F["tA"]], [Bf["Kh"]], out=Bf["Kh"][:], in0=F["kdir"][:], in1=F["tA"][:], op=ALU.mult)
                ew("dve", "scalar_tensor_tensor", [F["b_"], F["tA"]], [Bf["Bn"]], out=Bf["Bn"][:], in0=F["b_"][:], scalar=-1.0, in1=F["tA"][:], op0=ALU.mult, op1=ALU.mult)
                ew("act", "copy", [F["v_"]], [Bf["vb"]], out=Bf["vb"][:], in_=F["v_"][:])
                if dbg is not None and d == 0 and s == 0:
                    for nm in ("r_", "k_", "v_", "a_", "kkn", "kdir", "b_", "lw", "Pp"):
                        kb.dma(dbg[nm], F[nm][:], reads=rs(F[nm]))
                m_s = cf["m_up"] if d == 0 else cf["m_lo"]
                m_st = cf["m_lo"] if d == 0 else cf["m_up"]
                m_i = cf["m_upi"] if d == 0 else cf["m_loi"]
                BI = [0, 1]

                def mm_batch(bi, out_fn, lhs_fn, rhs_fn, reads, n_acc=1, lhs2=None, rhs2=None):
                    pt, pr = ps.next()
                    for g in range(8):
                        for h in range(2):
                            hs = HS[h]
                            gs = slice(64 * g, 64 * g + 64)
                            if lhs2 is None:
                                kb.op("pe", lambda e, pt=pt, hs=hs, gs=gs, g=g: e.matmul(out_fn(pt, hs, gs), lhsT=lhs_fn(hs, g), rhs=rhs_fn(hs, g), start=True, stop=True),
                                      reads=reads, writes=[pr], pe_accum=True)
                            else:
                                kb.op("pe", lambda e, pt=pt, hs=hs, gs=gs, g=g: e.matmul(out_fn(pt, hs, gs), lhsT=lhs_fn(hs, g), rhs=rhs_fn(hs, g), start=True, stop=False),
                                      reads=reads, writes=[pr], pe_accum=True)
                                kb.op("pe", lambda e, pt=pt, hs=hs, gs=gs, g=g: e.matmul(out_fn(pt, hs, gs), lhsT=lhs2(hs, g), rhs=rhs2(hs, g), start=False, stop=True),
                                      reads=reads, writes=[pr], pe_accum=True)
                    return pt, pr

                def f32out(pt, hs, gs):
                    return pt[hs, gs]

                def cm(tile, bi):
                    return lambda hs, g: tile[hs, bi * 512 + 64 * g: bi * 512 + 64 * g + 64]

                def tmj(tile):
                    return lambda hs, g: tile[hs, 64 * g:64 * g + 64]

                for (src, dst) in (("vb", "Vtm"), ("qt", "Qtm"), ("Kh", "Khtm"), ("Bn", "Bhtm")):
                    for bi in BI:
                        pt, pr = ps.next()
                        pv = psbf(pt)
                        for g in range(8):
                            for h in range(2):
                                hs = HS[h]
                                kb.op("pe", lambda e, pv=pv, hs=hs, g=g, src=src, bi=bi: e.transpose(out=pv[hs, 64 * g:64 * g + 64], in_=Bf[src][hs, bi * 512 + 64 * g: bi * 512 + 64 * g + 64], identity=ident[hs, :]),
                                      reads=rs(Bf[src], ident), writes=[pr], pe_accum=True)
                        kb.op("act", lambda e, pv=pv, dst=dst, bi=bi: e.copy(out=PB[bi][dst][:], in_=pv[:, 0:512]), reads=[pr], writes=rs(PB[bi][dst]))
                for bi in BI:
                    pt, pr = mm_batch(bi, f32out, cm(Bf["nbt"], bi), cm(Bf["qt"], bi), rs(Bf["nbt"], Bf["qt"]))
                    ew("dve", "tensor_tensor", [pr, m_s], [PB[bi]["Um"]], out=PB[bi]["Um"][:], in0=pt[:], in1=m_s[:], op=ALU.mult)
                    ew("pool", "tensor_tensor", [PB[bi]["Um"], cf["itile"]], [PB[bi]["Xa"]], out=PB[bi]["Xa"][:], in0=PB[bi]["Um"][:], in1=cf["itile"][:], op=ALU.add)
                for bi in BI:
                    pt, pr = mm_batch(bi, f32out, cm(Bf["qt"], bi), cm(Bf["nbt"], bi), rs(Bf["nbt"], Bf["qt"]))
                    ew("dve", "tensor_tensor", [pr, m_st], [PB[bi]["Utm"]], out=PB[bi]["Utm"][:], in0=pt[:], in1=m_st[:], op=ALU.mult)
                for (lname, rname, mask, dst) in (("kt", "qt", m_s, "AkkT"), ("kt", "rt", m_i, "ArkT"), ("nbt", "rt", m_i, "nArbT")):
                    for bi in BI:
                        pt, pr = mm_batch(bi, f32out, cm(Bf[lname], bi), cm(Bf[rname], bi), rs(Bf[lname], Bf[rname]))
                        ew("dve", "tensor_tensor", [pr, mask], [PB[bi][dst]], out=PB[bi][dst][:], in0=pt[:], in1=mask[:], op=ALU.mult)
                for bi in BI:
                    pt, pr = mm_batch(bi, f32out, tmj(PB[bi]["AkkT"]), tmj(PB[bi]["Vtm"]), rs(PB[bi]["AkkT"], PB[bi]["Vtm"]))
                    ew("act", "copy", [pr], [PB[bi]["AV"]], out=PB[bi]["AV"][:], in_=pt[:])
                Pc = {bi: ("Um", "Utm") for bi in BI}
                Xc = {bi: "Xa" for bi in BI}
                for lev in range(1, 6):
                    pn, ptn = ("Pa", "Pta") if lev % 2 == 1 else ("Pb", "Ptb")
                    for bi in BI:
                        P_, Pt_ = Pc[bi]
                        if lev < 5:
                            pt, pr = mm_batch(bi, f32out, tmj(PB[bi][Pt_]), tmj(PB[bi][P_]), rs(PB[bi][Pt_], PB[bi][P_]))
                            ew("act", "copy", [pr], [PB[bi][pn]], out=PB[bi][pn][:], in_=pt[:])
                        pt, pr = mm_batch(bi, f32out, tmj(PB[bi][P_]), tmj(PB[bi][Pt_]), rs(PB[bi][Pt_], PB[bi][P_]))
                        ew("act", "copy", [pr], [PB[bi][ptn]], out=PB[bi][ptn][:], in_=pt[:])
                    for bi in BI:
                        xo = Xc[bi]
                        xn = "Xb" if xo == "Xa" else "Xa"
                        pt, pr = mm_batch(bi, f32out, tmj(PB[bi][ptn]), tmj(PB[bi][xo]), rs(PB[bi][ptn], PB[bi][xo]))
                        ew("dve", "tensor_tensor", [pr, PB[bi][xo]], [PB[bi][xn]], out=PB[bi][xn][:], in0=pt[:], in1=PB[bi][xo][:], op=ALU.add)
                        Xc[bi] = xn
                        Pc[bi] = (pn, ptn)
                for bi in BI:
                    X5 = PB[bi][Xc[bi]]
                    pt, pr = mm_batch(bi, f32out, tmj(X5), tmj(PB[bi]["AV"]), rs(X5, PB[bi]["AV"]))
                    ew("act", "copy", [pr], [PB[bi]["Gtm"]], out=PB[bi]["Gtm"][:], in_=pt[:])
                    pt, pr = mm_batch(bi, f32out, tmj(X5), tmj(PB[bi]["Qtm"]), rs(X5, PB[bi]["Qtm"]))
                    ew("act", "copy", [pr], [PB[bi]["Qh"]], out=PB[bi]["Qh"][:], in_=pt[:])
                for bi in BI:
                    pt, pr = mm_batch(bi, f32out, tmj(PB[bi]["Qh"]), tmj(PB[bi]["Bhtm"]), rs(PB[bi]["Qh"], PB[bi]["Bhtm"]))
                    for g in range(8):
                        cidx = bi * 8 + g
                        gs = slice(64 * g, 64 * g + 64)
                        ew("dve", "scalar_tensor_tensor", [pr, cf["itile"], wC], [PB[bi]["PhiT"]], out=PB[bi]["PhiT"][:, gs], in0=cf["itile"][:, gs],
                           scalar=wC[:, cidx:cidx + 1], in1=pt[:, gs], op0=ALU.mult, op1=ALU.add)
                    pt, pr = mm_batch(bi, f32out, tmj(PB[bi]["Qh"]), tmj(PB[bi]["nArbT"]), rs(PB[bi]["Qh"], PB[bi]["nArbT"]))
                    ew("dve", "tensor_tensor", [pr, Bf["rt"]], [PB[bi]["RhT"]], out=PB[bi]["RhT"][:], in0=pt[:], in1=Bf["rt"][:, bi * 512:(bi + 1) * 512], op=ALU.add)
                    pt, pr = mm_batch(bi, f32out, tmj(PB[bi]["Khtm"]), tmj(PB[bi]["Vtm"]), rs(PB[bi]["Khtm"], PB[bi]["Vtm"], PB[bi]["Bhtm"], PB[bi]["Gtm"]),
                                      lhs2=tmj(PB[bi]["Bhtm"]), rhs2=tmj(PB[bi]["Gtm"]))
                    ew("act", "copy", [pr], [Psi[bi]], out=Psi[bi][:], in_=pt[:])
                order = list(range(NCH))
                if d == 1:
                    order = order[::-1]
                for cidx in order:
                    bi, g = divmod(cidx, 8)
                    gs = slice(64 * g, 64 * g + 64)
                    pt, pr = ps.next()
                    for h in range(2):
                        hs = HS[h]
                        kb.op("pe", lambda e, pt=pt, hs=hs, gs=gs, bi=bi: e.matmul(pt[hs, 0:64], lhsT=PB[bi]["Vtm"][hs, gs], rhs=PB[bi]["ArkT"][hs, gs], start=True, stop=False),
                              reads=rs(PB[bi]["Vtm"], PB[bi]["ArkT"]), writes=[pr], pe_accum=True)
                        kb.op("pe", lambda e, pt=pt, hs=hs, gs=gs, bi=bi: e.matmul(pt[hs, 0:64], lhsT=PB[bi]["Gtm"][hs, gs], rhs=PB[bi]["nArbT"][hs, gs], start=False, stop=False),
                              reads=rs(PB[bi]["Gtm"], PB[bi]["nArbT"]), writes=[pr], pe_accum=True)
                        kb.op("pe", lambda e, pt=pt, hs=hs, gs=gs, bi=bi: e.matmul(pt[hs, 0:64], lhsT=Hbf[hs, :], rhs=PB[bi]["RhT"][hs, gs], start=False, stop=True),
                              reads=rs(Hbf, PB[bi]["RhT"]), writes=[pr], pe_accum=True)
                    ew("act", "copy", [pr], [F["yc"]], out=F["yc"][:, cidx * 64:(cidx + 1) * 64], in_=pt[:, 0:64])
                    pt2, pr2 = ps.next()
                    for h in range(2):
                        hs = HS[h]
                        kb.op("pe", lambda e, pt2=pt2, hs=hs, gs=gs, bi=bi: e.matmul(pt2[hs, 0:64], lhsT=PB[bi]["PhiT"][hs, gs], rhs=Hbf[hs, :], start=True, stop=True),
                              reads=rs(PB[bi]["PhiT"], Hbf), writes=[pr2], pe_accum=True)
                    ew("dve", "tensor_tensor", [pr2, Psi[bi]], [Hbf], out=Hbf[:], in0=pt2[:, 0:64], in1=Psi[bi][:, gs], op=ALU.add)
                if d == 0:
                    kb.dma(yfT[c0:c0 + 128, tsl], F["yc"][:], reads=rs(F["yc"]), eng="pool")
                else:
                    kb.dma(F["yf"][:], yfT[c0:c0 + 128, tsl], writes=rs(F["yf"]), eng="sp")
                    kb.dma(sg0[:], loraA[256:384, tsl], writes=rs(sg0), eng="act")
                    kb.dma(sg1[0:32, :], loraA[384:416, tsl], writes=rs(sg1), eng="act")
                    for hf in range(2):
                        sl = slice(hf * 512, (hf + 1) * 512)
                        pt, pr = ps.next()
                        kb.op("pe", lambda e, pt=pt, sl=sl: e.matmul(pt[:], lhsT=alp[0][:], rhs=adp[:, sl], start=True, stop=True),
                              reads=rs(alp[0], adp), writes=[pr])
                        kb.op("act", lambda e, pt=pt, sl=sl: e.activation(out=F["af"][:, sl], in_=pt[:], func=AF.Sigmoid, bias=col["a0f"][:, 0:1]),
                              reads=[pr] + rs(col["a0f"]), writes=rs(F["af"]))
                    ew("dve", "tensor_tensor", [F["af"], F["a_"]], [F["af"]], out=F["af"][:], in0=F["af"][:], in1=F["a_"][:], op=ALU.add)
                    ew("dve", "tensor_scalar", [F["af"], hka, omka], [F["af"]], out=F["af"][:], in0=F["af"][:], scalar1=hka[:, 0:1], scalar2=omka[:, 0:1], op0=ALU.mult, op1=ALU.add)
                    ew("dve", "tensor_tensor", [F["af"], F["k_"]], [F["af"]], out=F["af"][:], in0=F["af"][:], in1=F["k_"][:], op=ALU.mult)
                    ew("dve", "scalar_tensor_tensor", [F["af"], col["r_k"], F["r_"]], [sqr], out=sqr[:], in0=F["af"][:], scalar=col["r_k"][:, 0:1], in1=F["r_"][:], op0=ALU.mult, op1=ALU.mult)
                    ew("dve", "tensor_tensor", [F["yc"], F["yf"]], [F["yc"]], out=F["yc"][:], in0=F["yc"][:], in1=F["yf"][:], op=ALU.add)
                    ew("act", "copy", [F["yc"]], [tw], out=tw[:], in_=F["yc"][:])
                    ew("act", "activation", [F["yc"]], [adp], out=adp[:], in_=F["yc"][:], func=AF.Square)
                    for hf in range(2):
                        sl = slice(hf * 512, (hf + 1) * 512)
                        for (src, dstn, scale) in ((tw, "po1", 1.0 / 64), (adp, "po2", 1.0 / 64), (sqr, "po3", 1.0)):
                            pt, pr = ps.next()
                            kb.op("pe", lambda e, pt=pt, sl=sl, src=src: e.matmul(pt[:], lhsT=bones[:], rhs=src[:, sl], start=True, stop=True),
                                  reads=rs(bones, src), writes=[pr])
                            kb.op("act", lambda e, pt=pt, sl=sl, dstn=dstn, scale=scale: e.mul(out=F[dstn][:, sl], in_=pt[:], mul=scale),
                                  reads=[pr], writes=rs(F[dstn]))
                        pt, pr = ps.next()
                        kb.op("pe", lambda e, pt=pt, sl=sl: e.matmul(pt[:], lhsT=gl0[:], rhs=sg0[:, sl], start=True, stop=False),
                              reads=rs(gl0, sg0), writes=[pr], pe_accum=True)
                        kb.op("pe", lambda e, pt=pt, sl=sl: e.matmul(pt[:], lhsT=gl1[:], rhs=sg1[:, sl], start=False, stop=True),
                              reads=rs(gl1, sg1), writes=[pr], pe_accum=True)
                        kb.op("act", lambda e, pt=pt, sl=sl: e.copy(out=F["tA"][:, sl], in_=pt[:]), reads=[pr], writes=rs(F["tA"]))
                    ew("dve", "tensor_tensor", [F["po1"]], [F["tB"]], out=F["tB"][:], in0=F["po1"][:], in1=F["po1"][:], op=ALU.mult)
                    ew("dve", "tensor_tensor", [F["po2"], F["tB"]], [F["po2"]], out=F["po2"][:], in0=F["po2"][:], in1=F["tB"][:], op=ALU.subtract)
                    ew("act", "activation", [F["po2"]], [F["po2"]], out=F["po2"][:], in_=F["po2"][:], func=AF.Sqrt, bias=64e-5)
                    ew("dve", "reciprocal", [F["po2"]], [F["po2"]], out=F["po2"][:], in_=F["po2"][:])
                    ew("dve", "tensor_tensor", [F["yc"], F["po1"]], [F["yc"]], out=F["yc"][:], in0=F["yc"][:], in1=F["po1"][:], op=ALU.subtract)
                    ew("dve", "tensor_tensor", [F["yc"], F["po2"]], [F["yc"]], out=F["yc"][:], in0=F["yc"][:], in1=F["po2"][:], op=ALU.mult)
                    ew("dve", "tensor_scalar", [F["yc"], col["lnx_g"], col["lnx_b"]], [F["yc"]], out=F["yc"][:], in0=F["yc"][:], scalar1=col["lnx_g"][:, 0:1], scalar2=col["lnx_b"][:, 0:1], op0=ALU.mult, op1=ALU.add)
                    ew("dve", "tensor_tensor", [F["po3"], F["v_"]], [F["po3"]], out=F["po3"][:], in0=F["po3"][:], in1=F["v_"][:], op=ALU.mult)
                    ew("dve", "tensor_tensor", [F["yc"], F["po3"]], [F["yc"]], out=F["yc"][:], in0=F["yc"][:], in1=F["po3"][:], op=ALU.add)
                    ew("dve", "tensor_tensor", [F["yc"], F["tA"]], [yout], out=yout[:], in0=F["yc"][:], in1=F["tA"][:], op=ALU.mult)
                    kb.dma(yaT[c0:c0 + 128, tsl], yout[:], reads=rs(yout), eng="pool")
    kb.emit()


def stage_pool(nc, projT, pool_w, pool_scale, ybT, cnt_inv):
    kb = KB(nc)
    S = 1024
    HL = 8
    ps = PS(kb)
    Pp = [Tl(kb, f"Pp{i}", [128, S + 2 * HL]) for i in range(2)]
    acc = Tl(kb, "acc", [128, S + 2 * HL])
    acc2 = Tl(kb, "acc2", [128, S + 2 * HL])
    z = [Tl(kb, f"z{i}", [128, S], F32R) for i in range(2)]
    ci = Tl(kb, "ci", [128, S])
    pw = [Tl(kb, f"pw{i}", [128, 256], F32R) for i in range(2)]
    sc = Tl(kb, "sc", [128, 1])
    ot = [Tl(kb, f"ot{i}", [128, S], F32R) for i in range(2)]
    oi = 0
    for g, win in enumerate((2, 4, 8, 16)):
        half = win // 2
        for kc in range(2):
            kb.dma(pw[kc][:], pool_w[g, kc * 128:(kc + 1) * 128, :], writes=rs(pw[kc]))
        for s in range(T // S):
            t0 = s * S
            kb.dma(ci[:], cnt_inv[g:g + 1, t0:t0 + S].partition_broadcast(128), writes=rs(ci), eng="act")
            for kc in range(2):
                r0 = 3488 + g * 256 + kc * 128
                P = Pp[kc]
                lo = max(t0 - HL, 0)
                hi = min(t0 + S + HL, T)
                kb.dma(P[:, lo - (t0 - HL):hi - (t0 - HL)], projT[r0:r0 + 128, lo:hi], writes=rs(P), eng="sp")
                if t0 == 0:
                    kb.op("pool", lambda e, P=P: e.memset(P[:, 0:HL], 0.0), writes=rs(P))
                if t0 + S == T:
                    kb.op("pool", lambda e, P=P: e.memset(P[:, S + HL:S + 2 * HL], 0.0), writes=rs(P))
                W_ = S + 2 * HL
                kb.op("dve", lambda e, P=P: e.tensor_tensor(out=acc[:, 1:W_], in0=P[:, 0:W_ - 1], in1=P[:, 1:W_], op=ALU.add),
                      reads=rs(P), writes=rs(acc))
                cur, oth = acc, acc2
                wlen = 2
                while wlen < win:
                    kb.op("dve", lambda e, cur=cur, oth=oth, wlen=wlen: e.tensor_tensor(out=oth[:, 2 * wlen - 1:W_], in0=cur[:, 2 * wlen - 1:W_], in1=cur[:, wlen - 1:W_ - wlen], op=ALU.add),
                          reads=rs(cur), writes=rs(oth))
                    cur, oth = oth, cur
                    wlen *= 2
                off = HL + half - 1
                kb.op("dve", lambda e, cur=cur, off=off: e.tensor_tensor(out=acc2[:, 0:S] if cur is acc else acc[:, 0:S], in0=cur[:, off:off + S], in1=ci[:], op=ALU.mult),
                      reads=rs(cur, ci), writes=rs(acc2 if cur is acc else acc))
                mt = acc2 if cur is acc else acc
                kb.op("dve", lambda e, mt=mt, P=P, kc=kc: e.tensor_tensor(out=z[kc][:], in0=mt[:, 0:S], in1=P[:, HL:HL + S], op=ALU.subtract),
                      reads=rs(mt, P), writes=rs(z[kc]))
            for oc in range(2):
                c0 = g * 256 + oc * 128
                load_col(kb, sc, pool_scale[c0:c0 + 128])
                o = ot[oi % 2]
                oi += 1
                for hf in range(2):
                    sl = slice(hf * 512, (hf + 1) * 512)
                    pt, pr = ps.next()
                    for kc in range(2):
                        kb.op("pe", lambda e, pt=pt, kc=kc, oc=oc, sl=sl: e.matmul(pt[:], lhsT=pw[kc][:, oc * 128:(oc + 1) * 128], rhs=z[kc][:, sl], start=(kc == 0), stop=(kc == 1)),
                              reads=rs(pw[kc], z[kc]), writes=[pr], pe_accum=True)
                    kb.op("act", lambda e, pt=pt, o=o, sl=sl: e.activation(out=o[:, sl], in_=pt[:], func=AF.Identity, scale=sc[:, 0:1]),
                          reads=[pr] + rs(sc), writes=rs(o))
                kb.dma(ybT[c0:c0 + 128, t0:t0 + S], o[:], reads=rs(o), eng="pool")
    kb.emit()


def stage_mix(nc, xin, projT, yaT, ybT, w_up_a, w_up_b, w_o, g2, w_router, xmT, v_tm, affT, identf_ap):
    kb = KB(nc)
    N = 512
    KC = D // 128
    ps = PS(kb)
    ya = Tl(kb, "ya", [128, 8, N], F32R)
    yb = Tl(kb, "yb", [128, 8, N], F32R)
    mg = kb.sb("mg", [128, KC, N], F32R)
    mg_r = [kb.res(f"mg{k}") for k in range(KC)]
    xm = kb.sb("xm", [128, KC, N])
    xm_r = [kb.res(f"xm{k}") for k in range(KC)]
    vt = kb.sb("vt", [128, KC, N])
    vt_r = [kb.res(f"vt{k}") for k in range(KC)]
    wa = [Tl(kb, f"wa{i}", [128, 8, 128], F32R) for i in range(2)]
    wb = [Tl(kb, f"wb{i}", [128, 8, 128], F32R) for i in range(2)]
    wo = [Tl(kb, f"wo{i}", [128, KC, 128], F32R) for i in range(2)]
    sgA = [Tl(kb, f"sgA{i}", [128, N]) for i in range(2)]
    sgB = [Tl(kb, f"sgB{i}", [128, N]) for i in range(2)]
    xi = [Tl(kb, f"xi{i}", [128, N]) for i in range(2)]
    t1 = Tl(kb, "t1", [128, N])
    t2 = Tl(kb, "t2", [128, N])
    sq = [Tl(kb, f"sq{i}", [128, N], F32R) for i in range(2)]
    rstd = Tl(kb, "rstd", [128, N])
    gt = Tl(kb, "gt", [128, KC])
    onesf = Tl(kb, "onesf", [128, 128])
    ones = Tl(kb, "ones", [128, 128], F32R)
    idf = Tl(kb, "idf", [128, 128])
    wr = Tl(kb, "wr", [128, KC, NE])
    ex = Tl(kb, "ex", [NE, N])
    rsum = Tl(kb, "rsum", [NE, N])
    vo = [Tl(kb, f"vo{i}", [128, D], BF16) for i in range(2)]
    kb.op("pool", lambda e: e.memset(onesf[:], 1.0), writes=rs(onesf))
    kb.op("dve", lambda e: e.tensor_copy(out=ones[:], in_=onesf[:]), reads=rs(onesf), writes=rs(ones))
    kb.dma(idf[:], identf_ap, writes=rs(idf))
    kb.dma(gt[:], g2.rearrange("(k p) -> p k", p=128), writes=rs(gt), allow_slow_non_contiguous=True)
    kb.dma(wr[:], w_router.rearrange("(k p) e -> p k e", p=128), writes=rs(wr))
    wav = w_up_a.rearrange("(k p) n -> p k n", p=128)
    wbv = w_up_b.rearrange("(k p) n -> p k n", p=128)
    wov = w_o.rearrange("(k p) n -> p k n", p=128)
    it = 0
    voi = 0
    for s in range(T // N):
        tsl = slice(s * N, (s + 1) * N)
        kb.dma(ya[:], yaT[:, tsl].rearrange("(k p) n -> p k n", p=128), writes=rs(ya), eng="sp")
        kb.dma(yb[:], ybT[:, tsl].rearrange("(k p) n -> p k n", p=128), writes=rs(yb), eng="act")
        for oc in range(KC):
            b = it % 2
            it += 1
            osl = slice(oc * 128, (oc + 1) * 128)
            kb.dma(wa[b][:], wav[:, :, osl], writes=rs(wa[b]), eng="sp")
            kb.dma(wb[b][:], wbv[:, :, osl], writes=rs(wb[b]), eng="act")
            kb.dma(sgA[b][:], projT[4512 + oc * 128:4512 + (oc + 1) * 128, tsl], writes=rs(sgA[b]), eng="sp")
            kb.dma(sgB[b][:], projT[6560 + oc * 128:6560 + (oc + 1) * 128, tsl], writes=rs(sgB[b]), eng="act")
            pa, par = ps.next()
            for kc in range(8):
                kb.op("pe", lambda e, pa=pa, b=b, kc=kc: e.matmul(pa[:], lhsT=wa[b][:, kc, :], rhs=ya[:, kc, :], start=(kc == 0), stop=(kc == 7)),
                      reads=rs(wa[b], ya), writes=[par], pe_accum=True)
            pb_, pbr = ps.next()
            for kc in range(8):
                kb.op("pe", lambda e, pb_=pb_, b=b, kc=kc: e.matmul(pb_[:], lhsT=wb[b][:, kc, :], rhs=yb[:, kc, :], start=(kc == 0), stop=(kc == 7)),
                      reads=rs(wb[b], yb), writes=[pbr], pe_accum=True)
            kb.op("dve", lambda e, pa=pa, b=b: e.tensor_tensor(out=t1[:], in0=pa[:], in1=sgA[b][:], op=ALU.mult), reads=[par] + rs(sgA[b]), writes=rs(t1))
            kb.op("dve", lambda e, pb_=pb_, b=b: e.tensor_tensor(out=t2[:], in0=pb_[:], in1=sgB[b][:], op=ALU.mult), reads=[pbr] + rs(sgB[b]), writes=rs(t2))
            kb.op("pool", lambda e, oc=oc: e.tensor_tensor(out=mg[:, oc, :], in0=t1[:], in1=t2[:], op=ALU.add), reads=rs(t1, t2), writes=[mg_r[oc]])
        pss, pssr = ps.next()
        for oc in range(KC):
            b = it % 2
            it += 1
            osl = slice(oc * 128, (oc + 1) * 128)
            kb.dma(wo[b][:], wov[:, :, osl], writes=rs(wo[b]), eng="sp")
            kb.dma(xi[b][:], xin[osl, tsl], writes=rs(xi[b]), eng="act")
            po, por = ps.next()
            if po is pss:
                po, por = ps.next()
            for kc in range(KC):
                kb.op("pe", lambda e, po=po, b=b, kc=kc: e.matmul(po[:], lhsT=wo[b][:, kc, :], rhs=mg[:, kc, :], start=(kc == 0), stop=(kc == KC - 1)),
                      reads=[wo[b].r, mg_r[kc]], writes=[por], pe_accum=True)
            kb.op("dve", lambda e, po=po, b=b, oc=oc: e.tensor_tensor(out=xm[:, oc, :], in0=po[:], in1=xi[b][:], op=ALU.add), reads=[por] + rs(xi[b]), writes=[xm_r[oc]])
            kb.dma(xmT[osl, tsl], xm[:, oc, :], reads=[xm_r[oc]], eng="pool")
            a = oc % 2
            kb.op("act", lambda e, a=a, oc=oc: e.activation(out=sq[a][:], in_=xm[:, oc, :], func=AF.Square), reads=[xm_r[oc]], writes=rs(sq[a]))
            kb.op("pe", lambda e, a=a, oc=oc, pss=pss: e.matmul(pss[:], lhsT=ones[:], rhs=sq[a][:], start=(oc == 0), stop=(oc == KC - 1)),
                  reads=rs(ones, sq[a]), writes=[pssr], pe_accum=True)
        kb.op("act", lambda e, pss=pss: e.activation(out=rstd[:], in_=pss[:], func=AF.Sqrt, scale=1.0 / D, bias=EPS), reads=[pssr], writes=rs(rstd))
        kb.op("dve", lambda e: e.reciprocal(out=rstd[:], in_=rstd[:]), writes=rs(rstd))
        for oc in range(KC):
            kb.op("dve", lambda e, oc=oc: e.scalar_tensor_tensor(out=vt[:, oc, :], in0=xm[:, oc, :], scalar=gt[:, oc:oc + 1], in1=rstd[:], op0=ALU.mult, op1=ALU.mult),
                  reads=[xm_r[oc]] + rs(gt, rstd), writes=[vt_r[oc]])
        pl, plr = ps.next()
        for kc in range(KC):
            kb.op("pe", lambda e, pl=pl, kc=kc: e.matmul(pl[0:NE, :], lhsT=wr[:, kc, :], rhs=vt[:, kc, :], start=(kc == 0), stop=(kc == KC - 1)),
                  reads=[wr.r, vt_r[kc]], writes=[plr], pe_accum=True)
        kb.op("act", lambda e, pl=pl: e.activation(out=ex[:], in_=pl[0:NE, :], func=AF.Exp), reads=[plr], writes=rs(ex))
        pq, pqr = ps.next()
        kb.op("pe", lambda e, pq=pq: e.matmul(pq[0:NE, :], lhsT=onesf[0:NE, 0:NE], rhs=ex[:], start=True, stop=True), reads=rs(onesf, ex), writes=[pqr])
        kb.op("dve", lambda e, pq=pq: e.reciprocal(out=rsum[:], in_=pq[0:NE, :]), reads=[pqr], writes=rs(rsum))
        kb.op("dve", lambda e: e.tensor_tensor(out=ex[:], in0=ex[:], in1=rsum[:], op=ALU.mult), reads=rs(rsum), writes=rs(ex))
        kb.dma(affT[:, tsl], ex[:], reads=rs(ex), eng="sp")
        for tb in range(N // 128):
            o = vo[voi % 2]
            voi += 1
            for q in range(4):
                ptt, ptr = ps.next()
                for j in range(4):
                    oc = q * 4 + j
                    kb.op("pe", lambda e, ptt=ptt, oc=oc, tb=tb, j=j: e.transpose(out=ptt[:, j * 128:(j + 1) * 128], in_=vt[:, oc, tb * 128:(tb + 1) * 128], identity=idf[:]),
                          reads=[vt_r[oc]] + rs(idf), writes=[ptr], pe_accum=True)
                eng = "act" if q % 2 == 0 else "dve"
                if eng == "act":
                    kb.op("act", lambda e, ptt=ptt, o=o, q=q: e.copy(out=o[:, q * 512:(q + 1) * 512], in_=ptt[:]), reads=[ptr], writes=rs(o))
                else:
                    kb.op("dve", lambda e, ptt=ptt, o=o, q=q: e.tensor_copy(out=o[:, q * 512:(q + 1) * 512], in_=ptt[:]), reads=[ptr], writes=rs(o))
            kb.dma(v_tm[s * N + tb * 128:s * N + (tb + 1) * 128, :], o[:], reads=rs(o), eng="pool")
    kb.emit()


def stage_route(nc, affT, posD, gateD):
    kb = KB(nc)
    aff = Tl(kb, "aff", [NE, T])
    junk = Tl(kb, "junk", [NE, T])
    onesr = Tl(kb, "onesr", [NE, T])
    cs = Tl(kb, "cs", [NE, T])
    lo = Tl(kb, "lo", [NE, 1])
    hi = Tl(kb, "hi", [NE, 1])
    mid = Tl(kb, "mid", [NE, 1])
    cnt = Tl(kb, "cnt", [NE, 1])
    ge = Tl(kb, "ge", [NE, 1])
    d1 = Tl(kb, "d1", [NE, 1])
    d2 = Tl(kb, "d2", [NE, 1])

    def ew(eng, fname, reads, writes, **kw):
        kb.op(eng, lambda e: getattr(e, fname)(**kw), reads=rs(*reads), writes=rs(*writes))

    kb.dma(aff[:], affT, writes=rs(aff))
    ew("pool", "memset", [], [onesr], ap=onesr[:], constant=1.0)
    ew("pool", "memset", [], [lo], ap=lo[:], constant=0.0)
    ew("pool", "memset", [], [hi], ap=hi[:], constant=1.0)
    for it in range(30):
        ew("dve", "tensor_tensor", [lo, hi], [mid], out=mid[:], in0=lo[:], in1=hi[:], op=ALU.add)
        ew("dve", "tensor_scalar", [mid], [mid], out=mid[:], in0=mid[:], scalar1=0.5, scalar2=None, op0=ALU.mult)
        ew("dve", "tensor_scalar", [aff, mid], [junk, cnt], out=junk[:], in0=aff[:], scalar1=mid[:, 0:1], scalar2=None, op0=ALU.is_ge, op1=ALU.add, accum_out=cnt[:])
        ew("dve", "tensor_scalar", [cnt], [ge], out=ge[:], in0=cnt[:], scalar1=float(CAP) - 0.5, scalar2=None, op0=ALU.is_ge)
        ew("dve", "tensor_tensor", [mid, lo], [d1], out=d1[:], in0=mid[:], in1=lo[:], op=ALU.subtract)
        ew("dve", "tensor_tensor", [hi, mid], [d2], out=d2[:], in0=hi[:], in1=mid[:], op=ALU.subtract)
        ew("dve", "scalar_tensor_tensor", [d1, ge, lo], [lo], out=lo[:], in0=d1[:], scalar=ge[:, 0:1], in1=lo[:], op0=ALU.mult, op1=ALU.add)
        ew("dve", "scalar_tensor_tensor", [d2, ge, mid], [hi], out=hi[:], in0=d2[:], scalar=ge[:, 0:1], in1=mid[:], op0=ALU.mult, op1=ALU.add)
    ew("dve", "tensor_scalar", [aff, lo], [junk], out=junk[:], in0=aff[:], scalar1=lo[:, 0:1], scalar2=None, op0=ALU.is_ge)
    ew("dve", "tensor_tensor_scan", [onesr, junk], [cs], out=cs[:], data0=onesr[:], data1=junk[:], initial=0.0, op0=ALU.mult, op1=ALU.add)
    ew("dve", "tensor_tensor", [cs, junk], [cs], out=cs[:], in0=cs[:], in1=junk[:], op=ALU.mult)
    ew("dve", "tensor_scalar", [cs], [cs], out=cs[:], in0=cs[:], scalar1=-1.0, scalar2=None, op0=ALU.add)
    kb.dma(posD, cs[:], reads=rs(cs))
    ew("dve", "tensor_tensor", [aff, junk], [aff], out=aff[:], in0=aff[:], in1=junk[:], op=ALU.mult)
    kb.dma(gateD, aff[:], reads=rs(aff), eng="act")
    kb.emit()


def stage_experts(nc, posD, v_tm, w_gate, w_up, w_down, yD, iota_row, experts=range(NE)):
    kb = KB(nc)
    KC = D // 128
    NF = FF // 128
    NTI = T // 128
    ps = PS(kb)
    iot = Tl(kb, "iot", [128, 512])
    kb.dma(iot[:], iota_row, writes=rs(iot))
    ptm = Tl(kb, "ptm", [128, NTI])
    sel = [Tl(kb, f"sel{i}", [128, 512], BF16) for i in range(2)]
    vt = [Tl(kb, f"vt{i}", [128, D], BF16) for i in range(2)]
    xs = kb.sb("xs", [128, KC, 512], F32R)
    xs_r = [kb.res(f"xs{k}") for k in range(KC)]
    hT = kb.sb("hT", [128, NF, 512], F32R)
    hT_r = [kb.res(f"hT{k}") for k in range(NF)]
    wbuf = [Tl(kb, f"wb{i}", [128, KC * 256], F32R) for i in range(4)]
    sil = Tl(kb, "sil", [128, 512])
    yo = [Tl(kb, f"yo{i}", [128, D], F32R) for i in range(2)]
    vv = v_tm.rearrange("(p n) d -> p n d", n=NTI)
    wi = 0
    yi = 0
    si = 0
    for e_ in experts:
        kb.dma(ptm[:], posD[e_, :].rearrange("(p n) -> p n", n=NTI), writes=rs(ptm))
        for half in range(2):
            banks = [ps.next() for _ in range(8)]
            for n in range(NTI):
                b = si % 2
                si += 1
                kb.dma(vt[b][:], vv[:, n, :], writes=rs(vt[b]), eng=("sp" if n % 2 == 0 else "act"))
                kb.op("dve", lambda e, b=b, n=n: e.tensor_scalar(out=sel[b][:], in0=iot[:], scalar1=ptm[:, n:n + 1], scalar2=None, op0=ALU.is_equal),
                      reads=rs(iot, ptm), writes=rs(sel[b]))
                for ci in range(8):
                    c = half * 8 + ci
                    pt, pr = banks[ci]
                    kb.op("pe", lambda e, pt=pt, b=b, c=c, n=n: e.matmul(pt[:], lhsT=vt[b][:, c * 128:(c + 1) * 128], rhs=sel[b][:], start=(n == 0), stop=(n == NTI - 1)),
                          reads=rs(vt[b], sel[b]), writes=[pr], pe_accum=True)
            for ci in range(8):
                c = half * 8 + ci
                pt, pr = banks[ci]
                if ci % 2 == 0:
                    kb.op("act", lambda e, pt=pt, c=c: e.copy(out=xs[:, c, :], in_=pt[:]), reads=[pr], writes=[xs_r[c]])
                else:
                    kb.op("dve", lambda e, pt=pt, c=c: e.tensor_copy(out=xs[:, c, :], in_=pt[:]), reads=[pr], writes=[xs_r[c]])
        wgv = w_gate[e_].rearrange("(k p) f -> p k f", p=128)
        wuv = w_up[e_].rearrange("(k p) f -> p k f", p=128)
        for fp in range(NF // 2):
            bg = wbuf[(wi * 2) % 4]
            bu = wbuf[(wi * 2 + 1) % 4]
            wi += 1
            fsl = slice(fp * 256, (fp + 1) * 256)
            kb.dma(bg[:].rearrange("p (k f) -> p k f", f=256), wgv[:, :, fsl], writes=rs(bg), eng="sp")
            kb.dma(bu[:].rearrange("p (k f) -> p k f", f=256), wuv[:, :, fsl], writes=rs(bu), eng="act")
            bg3 = bg[:].rearrange("p (k f) -> p k f", f=256)
            bu3 = bu[:].rearrange("p (k f) -> p k f", f=256)
            for j in range(2):
                f = fp * 2 + j
                pg, pgr = ps.next()
                for k in range(KC):
                    kb.op("pe", lambda e, pg=pg, bg3=bg3, j=j, k=k: e.matmul(pg[:], lhsT=bg3[:, k, j * 128:(j + 1) * 128], rhs=xs[:, k, :], start=(k == 0), stop=(k == KC - 1)),
                          reads=[bg.r, xs_r[k]], writes=[pgr], pe_accum=True)
                pu, pur = ps.next()
                for k in range(KC):
                    kb.op("pe", lambda e, pu=pu, bu3=bu3, j=j, k=k: e.matmul(pu[:], lhsT=bu3[:, k, j * 128:(j + 1) * 128], rhs=xs[:, k, :], start=(k == 0), stop=(k == KC - 1)),
                          reads=[bu.r, xs_r[k]], writes=[pur], pe_accum=True)
                kb.op("act", lambda e, pg=pg: e.activation(out=sil[:], in_=pg[:], func=AF.Silu), reads=[pgr], writes=rs(sil))
                kb.op("dve", lambda e, pu=pu, f=f: e.tensor_tensor(out=hT[:, f, :], in0=pu[:], in1=sil[:], op=ALU.mult), reads=[pur] + rs(sil), writes=[hT_r[f]])
        wdv = w_down[e_].rearrange("(f p) d -> p f d", p=128)
        for jp in range(2):
            banks = [ps.next() for _ in range(8)]
            for f in range(NF):
                wb_ = wbuf[wi % 4]
                wi += 1
                kb.dma(wb_[:, 0:D], wdv[:, f, :], writes=rs(wb_), eng=("sp" if f % 2 == 0 else "act"))
                for jj in range(2):
                    j = jp * 2 + jj
                    for q in range(4):
                        pt, pr = banks[jj * 4 + q]
                        kb.op("pe", lambda e, pt=pt, wb_=wb_, f=f, j=j, q=q: e.matmul(pt[:], lhsT=hT[:, f, j * 128:(j + 1) * 128], rhs=wb_[:, q * 512:(q + 1) * 512], start=(f == 0), stop=(f == NF - 1)),
                              reads=[hT_r[f], wb_.r], writes=[pr], pe_accum=True)
            for jj in range(2):
                j = jp * 2 + jj
                o = yo[yi % 2]
                yi += 1
                for q in range(4):
                    pt, pr = banks[jj * 4 + q]
                    if q % 2 == 0:
                        kb.op("act", lambda e, pt=pt, o=o, q=q: e.copy(out=o[:, q * 512:(q + 1) * 512], in_=pt[:]), reads=[pr], writes=rs(o))
                    else:
                        kb.op("dve", lambda e, pt=pt, o=o, q=q: e.tensor_copy(out=o[:, q * 512:(q + 1) * 512], in_=pt[:]), reads=[pr], writes=rs(o))
                kb.dma(yD[e_, j * 128:(j + 1) * 128, :], o[:], reads=rs(o), eng="pool")
    kb.emit()


def stage_combine(nc, posD, gateD, yD, xmT, xoutT, slot_col):
    kb = KB(nc)
    N = 512
    ps = PS(kb)
    scol = Tl(kb, "scol", [128, 4])
    kb.dma(scol[:], slot_col, writes=rs(scol))
    posb = [Tl(kb, f"posb{i}", [128, N]) for i in range(2)]
    gateb = [Tl(kb, f"gateb{i}", [128, N]) for i in range(2)]
    selg = [[Tl(kb, f"selg{i}_{j}", [128, N], F32R) for j in range(4)] for i in range(2)]
    ye = [Tl(kb, f"ye{i}", [128, 4, 1024], F32R) for i in range(2)]
    xi = [Tl(kb, f"xi{i}", [128, N]) for i in range(2)]
    xo = [Tl(kb, f"xo{i}", [128, N]) for i in range(2)]
    it = 0
    oi = 0
    for tb in range(T // N):
        tsl = slice(tb * N, (tb + 1) * N)
        for chalf in range(2):
            banks = [ps.next() for _ in range(8)]
            for e_ in range(NE):
                b = it % 2
                it += 1
                kb.dma(posb[b][:], posD[e_:e_ + 1, tsl].partition_broadcast(128), writes=rs(posb[b]), eng="sp")
                kb.dma(gateb[b][:], gateD[e_:e_ + 1, tsl].partition_broadcast(128), writes=rs(gateb[b]), eng="act")
                kb.dma(ye[b][:], yD[e_, :, chalf * 1024:(chalf + 1) * 1024].rearrange("(j p) d -> p j d", p=128), writes=rs(ye[b]), eng="sp")
                for j in range(4):
                    kb.op("dve", lambda e, b=b, j=j: e.scalar_tensor_tensor(out=selg[b][j][:], in0=posb[b][:], scalar=scol[:, j:j + 1], in1=gateb[b][:], op0=ALU.is_equal, op1=ALU.mult),
                          reads=rs(posb[b], gateb[b], scol), writes=rs(selg[b][j]))
                for ci in range(8):
                    pt, pr = banks[ci]
                    for j in range(4):
                        kb.op("pe", lambda e, pt=pt, b=b, j=j, ci=ci, e_=e_: e.matmul(pt[:], lhsT=ye[b][:, j, ci * 128:(ci + 1) * 128], rhs=selg[b][j][:], start=(e_ == 0 and j == 0), stop=(e_ == NE - 1 and j == 3)),
                              reads=rs(ye[b], selg[b][j]), writes=[pr], pe_accum=True)
            for ci in range(8):
                c = chalf * 8 + ci
                pt, pr = banks[ci]
                ob = oi % 2
                oi += 1
                kb.dma(xi[ob][:], xmT[c * 128:(c + 1) * 128, tsl], writes=rs(xi[ob]), eng="act")
                kb.op("dve", lambda e, pt=pt, ob=ob: e.tensor_tensor(out=xo[ob][:], in0=pt[:], in1=xi[ob][:], op=ALU.add), reads=[pr] + rs(xi[ob]), writes=rs(xo[ob]))
                kb.dma(xoutT[c * 128:(c + 1) * 128, tsl], xo[ob][:], reads=rs(xo[ob]), eng="pool")
    kb.emit()


def stage_final(nc, xT, g_ap, outT):
    kb = KB(nc)
    KC = D // 128
    N = 512
    ps = PS(kb)
    x = kb.sb("x", [128, KC, N])
    x_r = [kb.res(f"x{k}") for k in range(KC)]
    sq = [Tl(kb, f"sq{i}", [128, N], F32R) for i in range(2)]
    rstd = Tl(kb, "rstd", [128, N])
    gt = Tl(kb, "gt", [128, KC])
    onesf = Tl(kb, "onesf", [128, 128])
    ones = Tl(kb, "ones", [128, 128], F32R)
    o = [Tl(kb, f"o{i}", [128, N]) for i in range(2)]
    kb.op("pool", lambda e: e.memset(onesf[:], 1.0), writes=rs(onesf))
    kb.op("dve", lambda e: e.tensor_copy(out=ones[:], in_=onesf[:]), reads=rs(onesf), writes=rs(ones))
    kb.dma(gt[:], g_ap.rearrange("(k p) -> p k", p=128), writes=rs(gt), allow_slow_non_contiguous=True)
    xv = xT.rearrange("(k p) n -> p k n", p=128)
    ov = outT.rearrange("(k p) n -> p k n", p=128)
    oi = 0
    for s in range(T // N):
        tsl = slice(s * N, (s + 1) * N)
        pt, pr = ps.next()
        for k in range(KC):
            kb.dma(x[:, k, :], xv[:, k, tsl], writes=[x_r[k]], eng=("sp" if k % 2 == 0 else "act"))
            a = k % 2
            kb.op("act", lambda e, a=a, k=k: e.activation(out=sq[a][:], in_=x[:, k, :], func=AF.Square), reads=[x_r[k]], writes=rs(sq[a]))
            kb.op("pe", lambda e, pt=pt, a=a, k=k: e.matmul(pt[:], lhsT=ones[:], rhs=sq[a][:], start=(k == 0), stop=(k == KC - 1)),
                  reads=rs(ones, sq[a]), writes=[pr], pe_accum=True)
        kb.op("act", lambda e, pt=pt: e.activation(out=rstd[:], in_=pt[:], func=AF.Sqrt, scale=1.0 / D, bias=EPS), reads=[pr], writes=rs(rstd))
        kb.op("dve", lambda e: e.reciprocal(out=rstd[:], in_=rstd[:]), writes=rs(rstd))
        for k in range(KC):
            ob = oi % 2
            oi += 1
            kb.op("dve", lambda e, k=k, ob=ob: e.scalar_tensor_tensor(out=o[ob][:], in0=x[:, k, :], scalar=gt[:, k:k + 1], in1=rstd[:], op0=ALU.mult, op1=ALU.mult),
                  reads=[x_r[k]] + rs(gt, rstd), writes=rs(o[ob]))
            kb.dma(ov[:, k, tsl], o[ob][:], reads=rs(o[ob]), eng="pool", is_output=True)
    kb.emit()


def make_consts():
    p = np.arange(128)[:, None] % 64
    t = np.arange(512)[None, :] % 64
    c = {}
    c["m_up"] = (p < t).astype(np.float32)
    c["m_lo"] = (p > t).astype(np.float32)
    c["m_upi"] = (p <= t).astype(np.float32)
    c["m_loi"] = (p >= t).astype(np.float32)
    c["itile"] = (p == t).astype(np.float32)
    c["ident"] = (p == np.arange(64)[None, :]).astype(np.float32)
    c["bones"] = ((np.arange(128)[:, None] // 64) == (np.arange(128)[None, :] // 64)).astype(np.float32)
    cm = np.ones((128, 1024), np.float32)
    cm[:, ::64] = 0.0
    c["cmask"] = cm
    return c


from concourse.bass_utils import run_bass_kernel_spmd

N_CORES = 2
DEPTH = 2


def make_consts_all():
    c = make_consts()
    t = np.arange(T)
    ci = np.zeros((4, T), np.float32)
    for g, w in enumerate((2, 4, 8, 16)):
        h = w // 2
        lo = np.clip(t - h, 0, T)
        hi = np.clip(t + h, 0, T)
        ci[g] = 1.0 / (hi - lo).astype(np.float32)
    c["cnt_inv"] = ci
    c["iota_row"] = np.tile(np.arange(512, dtype=np.float32)[None, :], (128, 1))
    c["slot_col"] = (np.arange(4)[None, :] * 128 + np.arange(128)[:, None]).astype(np.float32)
    c["identf"] = np.eye(128, dtype=np.float32)
    return c


_WSPEC = {
    "norm_mix_g": ([DEPTH, D], F32), "w_in": ([DEPTH, D, D_IN], F32R), "mu_shift": ([DEPTH, 3488], F32),
    "w0": ([DEPTH, 2, 1024], F32), "w_lora_up": ([DEPTH, 2, 64, 1024], F32R), "a0": ([DEPTH, 2, 1024], F32),
    "a_lora_up": ([DEPTH, 2, 64, 1024], F32R), "g_lora_up": ([DEPTH, 160, 1024], F32R), "k_k": ([DEPTH, 1024], F32),
    "k_a": ([DEPTH, 1024], F32), "r_k": ([DEPTH, 1024], F32), "lnx_g": ([DEPTH, 1024], F32), "lnx_b": ([DEPTH, 1024], F32),
    "pool_w": ([DEPTH, 4, 256, 256], F32R), "pool_scale": ([DEPTH, 1024], F32), "w_up_a": ([DEPTH, 1024, D], F32R),
    "w_up_b": ([DEPTH, 1024, D], F32R), "w_o": ([DEPTH, D, D], F32R), "norm_moe_g": ([DEPTH, D], F32),
    "w_router": ([DEPTH, D, NE], F32), "w_gate_e": ([DEPTH, NE, D, FF], F32R), "w_up_e": ([DEPTH, NE, D, FF], F32R),
    "w_down_e": ([DEPTH, NE, FF, D], F32R), "final_g": ([D], F32),
}


def build_program(C):
    nc = bass.Bass("TRN2", target_bir_lowering=False)
    nc.dge_precook = False

    def din(name, shape, dt=F32):
        return nc.dram_tensor(name, list(shape), dt, kind="ExternalInput").ap()

    def dint(name, shape, dt=F32):
        return nc.dram_tensor(name, list(shape), dt, kind="Internal").ap()

    xT = din("xT", [D, T])
    Wt = {k: din(k, shp, dt) for k, (shp, dt) in _WSPEC.items()}
    cst = {k: din("c_" + k, v.shape) for k, v in C.items()}
    outT = nc.dram_tensor("outT", [D, T], F32, kind="ExternalOutput").ap()
    projT = dint("projT", [D_IN, T])
    loraA = dint("loraA", [416, T], F32R)
    yfT = dint("yfT", [1024, T])
    yaT = dint("yaT", [1024, T], F32R)
    ybT = dint("ybT", [1024, T], F32R)
    xmT = dint("xmT", [D, T])
    v_tm = dint("v_tm", [T, D], BF16)
    affT = dint("affT", [NE, T])
    posD = dint("posD", [NE, T])
    gateD = dint("gateD", [NE, T])
    yD = dint("yD", [NE, CAP, D], F32R)
    xs = [xT, dint("xA", [D, T]), dint("xB", [D, T])]
    for l in range(DEPTH):
        xin = xs[l]
        xout = xs[l + 1]
        stage_inproj(nc, xin, Wt["norm_mix_g"][l], Wt["w_in"][l], projT)
        stage_lora(nc, projT, Wt["mu_shift"][l], loraA)
        W = {"mu": Wt["mu_shift"][l], "w0": Wt["w0"][l], "wl": Wt["w_lora_up"][l], "a0": Wt["a0"][l], "al": Wt["a_lora_up"][l],
             "gl": Wt["g_lora_up"][l], "k_k": Wt["k_k"][l], "k_a": Wt["k_a"][l], "r_k": Wt["r_k"][l], "lnx_g": Wt["lnx_g"][l],
             "lnx_b": Wt["lnx_b"][l]}
        for hp in range(8):
            stage_rwkv(nc, projT, loraA, yfT, yaT, W, cst, hp_list=[hp])
        stage_pool(nc, projT, Wt["pool_w"][l], Wt["pool_scale"][l], ybT, cst["cnt_inv"])
        stage_mix(nc, xin, projT, yaT, ybT, Wt["w_up_a"][l], Wt["w_up_b"][l], Wt["w_o"][l], Wt["norm_moe_g"][l], Wt["w_router"][l],
                  xmT, v_tm, affT, cst["identf"])
        stage_route(nc, affT, posD, gateD)
        stage_experts(nc, posD, v_tm, Wt["w_gate_e"][l], Wt["w_up_e"][l], Wt["w_down_e"][l], yD, cst["iota_row"])
        stage_combine(nc, posD, gateD, yD, xmT, xout, cst["slot_col"])
    stage_final(nc, xs[DEPTH], Wt["final_g"], outT)
    return nc


def kernel(**inputs):
    C = make_consts_all()
    nc = build_program(C)
    x = np.asarray(inputs["x"], dtype=np.float32)
    in_maps = []
    for b in range(N_CORES):
        m = {"xT": np.ascontiguousarray(x[b].T)}
        for k in _WSPEC:
            a = np.asarray(inputs[k], dtype=np.float32)
            if k == "r_k":
                a = a.reshape(DEPTH, 1024)
            m[k] = np.ascontiguousarray(a)
        for k, v in C.items():
            m["c_" + k] = v
        in_maps.append(m)
    res = run_bass_kernel_spmd(nc, in_maps, core_ids=list(range(N_CORES)))
    out = np.stack([np.ascontiguousarray(np.asarray(res.results[b]["outT"]).T) for b in range(N_CORES)], axis=0)
    return out.astype(np.float32)
```

```python
from contextlib import ExitStack
import concourse.bass as bass
import concourse.mybir as mybir

F32 = mybir.dt.float32
F32R = mybir.dt.float32r
BF16 = mybir.dt.bfloat16
I32 = mybir.dt.int32
U32 = mybir.dt.uint32
AF = mybir.ActivationFunctionType
ALU = mybir.AluOpType
AX = mybir.AxisListType

ENGS = ("pe", "dve", "act", "pool", "sp")
N_DMA_SEMS = 48


class Res:
    __slots__ = ("name", "w", "r")

    def __init__(self, name):
        self.name = name
        self.w = None
        self.r = []


class Instr:
    __slots__ = ("eng", "fn", "deps", "stream", "seq", "waited", "val", "is_dma", "known")

    def __init__(self, eng, fn):
        self.eng = eng
        self.fn = fn
        self.deps = []
        self.stream = None
        self.seq = 0
        self.waited = False
        self.val = 0
        self.is_dma = False
        self.known = None


class KB:
    _count = 0

    def __init__(self, nc, same_engine_sync=True):
        KB._count += 1
        self.tag = f"k{KB._count}_"
        self.nc = nc
        self.es = ExitStack()
        self.q = {e: [] for e in ENGS}
        self.stream_last = {}
        self.stream_cnt = {}
        self.dma_rr = 0
        self.same_engine_sync = same_engine_sync
        self.n_res = 0
        self.out_dmas = []

    def sb(self, name, shape, dtype=F32):
        return self.es.enter_context(self.nc.sbuf_tensor(self.tag + name, list(shape), dtype))

    def ps(self, name, shape, dtype=F32):
        return self.es.enter_context(self.nc.psum_tensor(self.tag + name, list(shape), dtype))

    def res(self, name=None):
        self.n_res += 1
        return Res(name or f"r{self.n_res}")

    def _known_after(self, d):
        k = dict(d.known)
        if k.get(d.stream, 0) < d.seq:
            k[d.stream] = d.seq
        return k

    def _add(self, ins, reads, writes, pe_accum=False):
        eng = ins.eng
        deps = []
        for r in reads:
            if r.w is not None:
                deps.append(r.w)
        for w in writes:
            if w.w is not None:
                if not (pe_accum and w.w.eng == "pe" and not w.w.is_dma):
                    deps.append(w.w)
            deps.extend(w.r)
        prev = self.q[eng][-1] if self.q[eng] else None
        known = dict(prev.known) if prev is not None else {}
        if ins.is_dma:
            p = self.stream_last.get(ins.stream)
            if p is not None:
                deps.append(p)
        final = []
        deps = sorted(set(deps), key=lambda d: -d.seq)
        for d in deps:
            if d is ins:
                continue
            if (not d.is_dma) and d.eng == eng and not self.same_engine_sync:
                continue
            if known.get(d.stream, 0) >= d.seq:
                continue
            final.append(d)
            d.waited = True
            ka = self._known_after(d)
            for s, v in ka.items():
                if known.get(s, 0) < v:
                    known[s] = v
        ins.deps = final
        ins.known = known
        self.q[eng].append(ins)
        for r in reads:
            r.r.append(ins)
        for w in writes:
            w.w = ins
            w.r = []
        return ins

    def op(self, eng, fn, reads=(), writes=(), pe_accum=False):
        if eng == "pool":
            eng = "dve"
        ins = Instr(eng, fn)
        ins.stream = eng
        self.stream_cnt[eng] = self.stream_cnt.get(eng, 0) + 1
        ins.seq = self.stream_cnt[eng]
        self._add(ins, list(reads), list(writes), pe_accum=pe_accum)
        self.stream_last[eng] = ins
        return ins

    def dma(self, out, in_, reads=(), writes=(), eng="sp", is_output=False, **kw):
        if eng == "pool":
            eng = "sp"
        fn = lambda e: e.dma_start(out=out, in_=in_, **kw)
        ins = Instr(eng, fn)
        ins.is_dma = True
        slot = self.dma_rr % N_DMA_SEMS
        self.dma_rr += 1
        ins.stream = ("dma", slot)
        self.stream_cnt[ins.stream] = self.stream_cnt.get(ins.stream, 0) + 1
        ins.seq = self.stream_cnt[ins.stream]
        ins.waited = True
        self._add(ins, list(reads), list(writes))
        self.stream_last[ins.stream] = ins
        if is_output:
            self.out_dmas.append(ins)
        return ins

    def gen(self, eng, fn, reads=(), writes=()):
        return self.op(eng, fn, reads, writes)

    def emit(self):
        nc = self.nc
        lastd = [v for k, v in self.stream_last.items() if isinstance(k, tuple)]
        if lastd:
            self.out_dmas = lastd
            fin = Instr("sp", None)
            fin.stream = "sp"
            self.stream_cnt["sp"] = self.stream_cnt.get("sp", 0) + 1
            fin.seq = self.stream_cnt["sp"]
            prev = self.q["sp"][-1] if self.q["sp"] else None
            known = dict(prev.known) if prev is not None else {}
            for d in self.out_dmas:
                if known.get(d.stream, 0) < d.seq:
                    fin.deps.append(d)
            fin.known = known
            self.q["sp"].append(fin)
        sems = {}
        for e in ENGS:
            sems[e] = self.es.enter_context(nc.semaphore(self.tag + f"s_{e}"))
            v = 0
            for ins in self.q[e]:
                if ins.is_dma:
                    continue
                if ins.waited:
                    v += 1
                    ins.val = v
        for s in range(N_DMA_SEMS):
            sems[("dma", s)] = self.es.enter_context(nc.semaphore(self.tag + f"s_dma{s}"))
        engmap = {"pe": "tensor", "dve": "vector", "act": "scalar", "pool": "gpsimd", "sp": "sync"}
        q = self.q

        def run(ename, e):
            for ins in q[ename]:
                for d in ins.deps:
                    if d.is_dma:
                        e.wait_ge(sems[d.stream], 16 * d.seq)
                    else:
                        e.wait_ge(sems[d.stream], d.val)
                if ins.fn is None:
                    continue
                r = ins.fn(e)
                if ins.is_dma:
                    r.then_inc(sems[ins.stream], 16)
                elif ins.waited:
                    r.then_inc(sems[ins.stream], 1)

        allsems = list(sems.values())
        with nc.Block() as cblock:
            @cblock.gpsimd
            def _(e):
                for sm in allsems:
                    e.sem_clear(sm)

        with nc.Block() as block:
            @block.tensor
            def _(e):
                run("pe", e)

            @block.vector
            def _(e):
                run("dve", e)

            @block.scalar
            def _(e):
                run("act", e)

            @block.gpsimd
            def _(e):
                run("pool", e)

            @block.sync
            def _(e):
                run("sp", e)
        self.es.close()

    def stats(self):
        return {e: len(self.q[e]) for e in ENGS}


import numpy as np
import concourse.bass as bass
import concourse.mybir as mybir

D = 2048
T = 4096
D_IN = 8608
D_SHIFT = 3488
EPS = 1e-6
NE = 16
FF = 2816
CAP = 512


class Tl:
    def __init__(self, kb, name, shape, dtype=F32):
        self.t = kb.sb(name, shape, dtype)
        self.r = kb.res(name)

    def __getitem__(self, k):
        return self.t[k]


class PS:
    def __init__(self, kb, n=8):
        self.kb = kb
        self.t = [kb.ps(f"ps{i}", [128, 512]) for i in range(n)]
        self.r = [kb.res(f"ps{i}") for i in range(n)]
        self.i = 0
        self.n = n

    def next(self):
        i = self.i % self.n
        self.i += 1
        return self.t[i], self.r[i]


def rs(*tiles):
    return [x.r if hasattr(x, "r") and not isinstance(x, Res) else x for x in tiles]


def col_chunks():
    ch = [(i * 128, 128) for i in range(27)] + [(3456, 32)] + [(3488 + i * 128, 128) for i in range(40)]
    return ch


def stage_inproj(nc, xsrc, g_ap, w_ap, projT):
    kb = KB(nc)
    KC = D // 128
    NT = 1024
    ut = kb.sb("ut", [128, KC, NT], F32R)
    ut_r = [kb.res(f"ut{k}") for k in range(KC)]
    xa = [Tl(kb, f"xa{i}", [128, NT]) for i in range(2)]
    gt = Tl(kb, "gt", [128, KC])
    onesf = Tl(kb, "onesf", [128, 128])
    ones = Tl(kb, "ones", [128, 128], F32R)
    sq = [Tl(kb, f"sq{i}", [128, NT], F32R) for i in range(2)]
    rstd = Tl(kb, "rstd", [128, NT])
    wt = [Tl(kb, f"wt{i}", [128, KC, 512], F32R) for i in range(2)]
    ot = [Tl(kb, f"ot{i}", [128, NT]) for i in range(2)]
    ps = PS(kb)
    kb.op("pool", lambda e: e.memset(onesf[:], 1.0), writes=rs(onesf))
    kb.op("dve", lambda e: e.tensor_copy(out=ones[:], in_=onesf[:]), reads=rs(onesf), writes=rs(ones))
    kb.dma(gt[:], g_ap.rearrange("(k p) -> p k", p=128), writes=rs(gt), eng="sp", allow_slow_non_contiguous=True)
    xv = xsrc.rearrange("(k p) n -> p k n", p=128)
    wv = w_ap.rearrange("(k p) n -> p k n", p=128)
    chunks = col_chunks()
    groups = [chunks[i:i + 4] for i in range(0, len(chunks), 4)]
    gi_glob = 0
    ci = 0
    for s in range(T // NT):
        tsl = slice(s * NT, (s + 1) * NT)
        p0t, p0r = ps.next()
        p1t, p1r = ps.next()
        pss = [(p0t, p0r), (p1t, p1r)]
        for k in range(KC):
            a = k % 2
            kb.dma(xa[a][:], xv[:, k, tsl], writes=rs(xa[a]), eng="sp")
            kb.op("act", lambda e, a=a: e.activation(out=sq[a][:], in_=xa[a][:], func=AF.Square),
                  reads=rs(xa[a]), writes=rs(sq[a]))
            for h in range(2):
                kb.op("pe", lambda e, k=k, a=a, h=h, pt=pss[h][0]: e.matmul(pt[:], lhsT=ones[:], rhs=sq[a][:, h * 512:(h + 1) * 512],
                                                                        start=(k == 0), stop=(k == KC - 1)),
                      reads=rs(ones, sq[a]), writes=[pss[h][1]], pe_accum=True)
        for h in range(2):
            sl = slice(h * 512, (h + 1) * 512)
            kb.op("act", lambda e, h=h, sl=sl, pt=pss[h][0]: e.activation(out=rstd[:, sl], in_=pt[:], func=AF.Sqrt, scale=1.0 / D, bias=EPS),
                  reads=[pss[h][1]], writes=rs(rstd))
        kb.op("dve", lambda e: e.reciprocal(out=rstd[:], in_=rstd[:]), writes=rs(rstd))
        for k in range(KC):
            a = k % 2
            kb.dma(xa[a][:], xv[:, k, tsl], writes=rs(xa[a]), eng="sp")
            kb.op("dve", lambda e, k=k, a=a: e.scalar_tensor_tensor(out=ut[:, k, :], in0=xa[a][:], scalar=gt[:, k:k + 1], in1=rstd[:],
                                                                op0=ALU.mult, op1=ALU.mult),
                  reads=rs(gt, rstd, xa[a]), writes=[ut_r[k]])
        for grp in groups:
            b = gi_glob % 2
            c0 = grp[0][0]
            c1 = grp[-1][0] + grp[-1][1]
            kb.dma(wt[b][:, :, 0:c1 - c0], wv[:, :, c0:c1], writes=rs(wt[b]), eng=("sp" if gi_glob % 2 == 0 else "act"))
            gi_glob += 1
            for (cs, cw) in grp:
                ob = ci % 2
                for h in range(2):
                    pt, pr = ps.next()
                    for k in range(KC):
                        if cw == 128:
                            lhsT = wt[b][:, k, cs - c0:cs - c0 + cw]
                            rhs = ut[:, k, h * 512:(h + 1) * 512]
                        else:
                            lhsT = wt[b][:, k, cs - c0:cs - c0 + cw].bitcast(F32)
                            rhs = ut[:, k, h * 512:(h + 1) * 512].bitcast(F32)
                        kb.op("pe", lambda e, pt=pt, lhsT=lhsT, rhs=rhs, k=k, cw=cw: e.matmul(pt[0:cw, :], lhsT=lhsT, rhs=rhs,
                                                                                       start=(k == 0), stop=(k == KC - 1)),
                              reads=[wt[b].r, ut_r[k]], writes=[pr], pe_accum=True)
                    sl = slice(h * 512, (h + 1) * 512)
                    if cs >= 4512:
                        kb.op("act", lambda e, ob=ob, pt=pt, sl=sl, cw=cw: e.activation(out=ot[ob][0:cw, sl], in_=pt[0:cw, :], func=AF.Sigmoid),
                              reads=[pr], writes=rs(ot[ob]))
                    elif h == 0:
                        kb.op("act", lambda e, ob=ob, pt=pt, sl=sl, cw=cw: e.copy(out=ot[ob][0:cw, sl], in_=pt[0:cw, :]),
                              reads=[pr], writes=rs(ot[ob]))
                    else:
                        kb.op("dve", lambda e, ob=ob, pt=pt, sl=sl, cw=cw: e.tensor_copy(out=ot[ob][0:cw, sl], in_=pt[0:cw, :]),
                              reads=[pr], writes=rs(ot[ob]))
                kb.dma(projT[cs:cs + cw, tsl], ot[ob][0:cw, :], reads=rs(ot[ob]), eng="pool", is_output=True)
                ci += 1
    kb.emit()


def load_col(kb, tile, ap1d, eng="sp"):
    n = ap1d.shape[0]
    kb.dma(tile[0:n, 0:1], ap1d.rearrange("(p o) -> p o", o=1), writes=rs(tile), eng=eng)


def load_halo(kb, P, src_rows, t0, S, eng="sp"):
    n = src_rows.shape[0]
    lo = max(t0 - 1, 0)
    hi = min(t0 + S + 1, T)
    kb.dma(P[0:n, lo - (t0 - 1):hi - (t0 - 1)], src_rows[:, lo:hi], writes=rs(P), eng=eng)
    if t0 == 0:
        kb.op("pool", lambda e: e.memset(P[0:n, 0:1], 0.0), writes=rs(P))
    if t0 + S == T:
        kb.op("pool", lambda e: e.memset(P[0:n, S + 1:S + 2], 0.0), writes=rs(P))


def shift_rows(kb, out_ap, P, tmp, hmu, omu, n, S, out_res):
    kb.op("pool", lambda e: e.tensor_tensor(out=tmp[0:n, :], in0=P[0:n, 0:S], in1=P[0:n, 2:S + 2], op=ALU.add),
          reads=rs(P), writes=rs(tmp))
    kb.op("pool", lambda e: e.tensor_scalar(out=tmp[0:n, :], in0=tmp[0:n, :], scalar1=hmu[0:n, 0:1], scalar2=None, op0=ALU.mult),
          reads=rs(hmu), writes=rs(tmp))
    kb.op("dve", lambda e: e.scalar_tensor_tensor(out=out_ap, in0=P[0:n, 1:S + 1], scalar=omu[0:n, 0:1], in1=tmp[0:n, :],
                                                  op0=ALU.mult, op1=ALU.add),
          reads=rs(P, omu, tmp), writes=[out_res])


def mu_cols(kb, mu, hmu, omu, ap1d, n):
    load_col(kb, mu, ap1d)
    kb.op("dve", lambda e: e.tensor_scalar(out=hmu[0:n, :], in0=mu[0:n, :], scalar1=0.5, scalar2=None, op0=ALU.mult),
          reads=rs(mu), writes=rs(hmu))
    kb.op("dve", lambda e: e.tensor_scalar(out=omu[0:n, :], in0=mu[0:n, :], scalar1=-1.0, scalar2=1.0, op0=ALU.mult, op1=ALU.add),
          reads=rs(mu), writes=rs(omu))


def stage_lora(nc, projT, mu_ap, loraA):
    kb = KB(nc)
    S = 1024
    P = [Tl(kb, f"P{i}", [128, S + 2]) for i in range(2)]
    tmp = Tl(kb, "tmp", [128, S])
    sh = Tl(kb, "sh", [128, S])
    o = [Tl(kb, f"o{i}", [128, S], F32R) for i in range(2)]
    mu = [Tl(kb, f"mu{i}", [128, 1]) for i in range(4)]
    hmu = [Tl(kb, f"hmu{i}", [128, 1]) for i in range(4)]
    omu = [Tl(kb, f"omu{i}", [128, 1]) for i in range(4)]
    blocks = [(0, 128, AF.Tanh), (128, 128, AF.Copy), (256, 128, AF.Sigmoid), (384, 32, AF.Sigmoid)]
    for bi, (r0, n, fn) in enumerate(blocks):
        mu_cols(kb, mu[bi], hmu[bi], omu[bi], mu_ap[3072 + r0:3072 + r0 + n], n)
    it = 0
    for s in range(T // S):
        t0 = s * S
        for bi, (r0, n, fn) in enumerate(blocks):
            p = P[it % 2]
            oo = o[it % 2]
            it += 1
            load_halo(kb, p, projT[3072 + r0:3072 + r0 + n, :], t0, S)
            shift_rows(kb, sh[0:n, :], p, tmp, hmu[bi], omu[bi], n, S, sh.r)
            if fn == AF.Copy:
                kb.op("act", lambda e, oo=oo, n=n: e.copy(out=oo[0:n, :], in_=sh[0:n, :]), reads=rs(sh), writes=rs(oo))
            else:
                kb.op("act", lambda e, oo=oo, n=n, fn=fn: e.activation(out=oo[0:n, :], in_=sh[0:n, :], func=fn), reads=rs(sh), writes=rs(oo))
            kb.dma(loraA[r0:r0 + n, t0:t0 + S], oo[0:n, :], reads=rs(oo), eng="act")
    kb.emit()


CEXP = float(np.exp(-0.5))


def stage_rwkv(nc, projT, loraA, yfT, yaT, W, cst, hp_list=range(8), dbg=None):
    kb = KB(nc)
    S = 1024
    NCH = S // 64
    ps = PS(kb)
    cf = {k: Tl(kb, "c_" + k, [128, 512], BF16) for k in ("m_up", "m_lo", "m_upi", "m_loi", "itile")}
    cstage = Tl(kb, "cstage", [128, 512])
    for k in cf:
        kb.dma(cstage[:], cst[k], writes=rs(cstage))
        kb.op("dve", lambda e, k=k: e.tensor_copy(out=cf[k][:], in_=cstage[:]), reads=rs(cstage), writes=rs(cf[k]))
    identf = Tl(kb, "identf", [128, 64])
    ident = Tl(kb, "ident", [128, 64], BF16)
    kb.dma(identf[:], cst["ident"], writes=rs(identf))
    kb.op("dve", lambda e: e.tensor_copy(out=ident[:], in_=identf[:]), reads=rs(identf), writes=rs(ident))
    bonesf = Tl(kb, "bonesf", [128, 128])
    bones = Tl(kb, "bones", [128, 128], F32R)
    kb.dma(bonesf[:], cst["bones"], writes=rs(bonesf))
    kb.op("dve", lambda e: e.tensor_copy(out=bones[:], in_=bonesf[:]), reads=rs(bonesf), writes=rs(bones))
    cmask = Tl(kb, "cmask", [128, S])
    kb.dma(cmask[:], cst["cmask"], writes=rs(cmask))
    colnames = ["mu_r", "mu_k", "mu_v", "w0f", "w0b", "a0f", "a0b", "k_k", "k_a", "r_k", "lnx_g", "lnx_b"]
    col = {n: Tl(kb, "col_" + n, [128, 1]) for n in colnames}
    hmu = {n: Tl(kb, "h" + n, [128, 1]) for n in ("mu_r", "mu_k", "mu_v")}
    omu = {n: Tl(kb, "o" + n, [128, 1]) for n in ("mu_r", "mu_k", "mu_v")}
    omka = Tl(kb, "omka", [128, 1])
    hka = Tl(kb, "hka", [128, 1])
    zf = Tl(kb, "zf", [128, 128])
    kb.op("dve", lambda e: e.memset(zf[:], 0.0), writes=rs(zf))
    wlp = [Tl(kb, f"wlp{i}", [128, 128], F32R) for i in range(2)]
    alp = [Tl(kb, f"alp{i}", [128, 128], F32R) for i in range(2)]
    gl0 = Tl(kb, "gl0", [128, 128], F32R)
    gl1 = Tl(kb, "gl1", [128, 128], F32R)
    for i in range(2):
        oh = slice(64, 128) if i == 0 else slice(0, 64)
        kb.op("dve", lambda e, i=i, oh=oh: e.tensor_copy(out=wlp[i][oh, :], in_=zf[oh, :]), reads=rs(zf), writes=rs(wlp[i]))
        kb.op("dve", lambda e, i=i, oh=oh: e.tensor_copy(out=alp[i][oh, :], in_=zf[oh, :]), reads=rs(zf), writes=rs(alp[i]))
    for (pa_, pb_) in ((32, 64), (64, 128)):
        kb.op("dve", lambda e, pa_=pa_, pb_=pb_: e.tensor_copy(out=gl1[pa_:pb_, :], in_=zf[pa_:pb_, :]), reads=rs(zf), writes=rs(gl1))
    P2 = [Tl(kb, n, [128, S + 2]) for n in ("P2a", "P2b")]
    Pr, Pk, Pv = P2[0], P2[1], P2[0]
    names32 = ["r_", "k_", "v_", "tA", "tB", "sgw", "a_", "kk", "kkn", "b_", "Pp", "E_", "R_", "Sf", "yc", "po1", "po2", "po3"]
    F = {n: Tl(kb, n, [128, S]) for n in names32}
    F["lw"] = F["sgw"]
    F["kdir"] = F["kk"]
    F["yf"] = F["Sf"]
    F["af"] = F["E_"]
    sqr = Tl(kb, "sqr", [128, S], F32R)
    tw = Tl(kb, "tw", [128, S], F32R)
    adp = Tl(kb, "adp", [128, S], F32R)
    sg0 = Tl(kb, "sg0", [128, S], F32R)
    sg1 = Tl(kb, "sg1", [128, S], F32R)
    for q in range(S // 128):
        for (pa_, pb_) in ((32, 64), (64, 128)):
            kb.op("dve", lambda e, q=q, pa_=pa_, pb_=pb_: e.tensor_copy(out=sg1[pa_:pb_, q * 128:(q + 1) * 128], in_=zf[pa_:pb_, :]), reads=rs(zf), writes=rs(sg1))
    yout = sqr
    tot = Tl(kb, "tot", [128, NCH])
    wC = Tl(kb, "wC", [128, NCH])
    namesb = ["qt", "rt", "kt", "nbt", "Kh", "Bn", "vb"]
    Bf = {n: Tl(kb, n, [128, S], BF16) for n in namesb}
    pb_names = ["Vtm", "Qtm", "Khtm", "Bhtm", "Um", "Utm", "Pa", "Pb", "Pta", "Ptb", "Xa", "Xb", "AkkT", "ArkT", "nArbT", "AV", "Gtm", "Qh", "PhiT", "RhT"]
    PB = [{n: Tl(kb, f"{n}{bi}", [128, 512], BF16) for n in pb_names} for bi in range(2)]
    Psi = [Tl(kb, f"Psi{bi}", [128, 512]) for bi in range(2)]
    Hbf = Tl(kb, "Hbf", [128, 64], BF16)

    def ew(eng, fname, reads, writes, **kw):
        kb.op(eng, lambda e: getattr(e, fname)(**kw), reads=rs(*reads), writes=rs(*writes))

    def psbf(pt):
        return pt[:].bitcast(BF16)

    HS = [slice(0, 64), slice(64, 128)]

    for hp in hp_list:
        c0 = hp * 128
        csl = slice(c0, c0 + 128)
        load_col(kb, col["mu_r"], W["mu"][c0:c0 + 128])
        load_col(kb, col["mu_k"], W["mu"][1024 + c0:1024 + c0 + 128])
        load_col(kb, col["mu_v"], W["mu"][2048 + c0:2048 + c0 + 128])
        load_col(kb, col["w0f"], W["w0"][0, csl])
        load_col(kb, col["w0b"], W["w0"][1, csl])
        load_col(kb, col["a0f"], W["a0"][0, csl])
        load_col(kb, col["a0b"], W["a0"][1, csl])
        for n in ("k_k", "k_a", "r_k", "lnx_g", "lnx_b"):
            load_col(kb, col[n], W[n][csl])
        for n in ("mu_r", "mu_k", "mu_v"):
            ew("dve", "tensor_scalar", [col[n]], [hmu[n]], out=hmu[n][:], in0=col[n][:], scalar1=0.5, scalar2=None, op0=ALU.mult)
            ew("dve", "tensor_scalar", [col[n]], [omu[n]], out=omu[n][:], in0=col[n][:], scalar1=-1.0, scalar2=1.0, op0=ALU.mult, op1=ALU.add)
        ew("dve", "tensor_scalar", [col["k_a"]], [omka], out=omka[:], in0=col["k_a"][:], scalar1=-1.0, scalar2=1.0, op0=ALU.mult, op1=ALU.add)
        ew("dve", "tensor_scalar", [col["k_a"]], [hka], out=hka[:], in0=col["k_a"][:], scalar1=0.5, scalar2=None, op0=ALU.mult)
        for i in range(2):
            kb.dma(wlp[i][HS[i], :], W["wl"][i, :, csl], writes=rs(wlp[i]))
            kb.dma(alp[i][HS[i], :], W["al"][i, :, csl], writes=rs(alp[i]))
        kb.dma(gl0[:], W["gl"][0:128, csl], writes=rs(gl0))
        kb.dma(gl1[0:32, :], W["gl"][128:160, csl], writes=rs(gl1))
        for d in range(2):
            ds = HS[d]
            ew("pool", "memset", [], [Hbf], ap=Hbf[:], constant=0.0)
            slabs = list(range(T // S))
            if d == 1:
                slabs = slabs[::-1]
            for s in slabs:
                t0 = s * S
                tsl = slice(t0, t0 + S)
                load_halo(kb, Pr, projT[c0:c0 + 128, :], t0, S, eng="sp")
                load_halo(kb, Pk, projT[1024 + c0:1024 + c0 + 128, :], t0, S, eng="act")
                kb.dma(tw[:], loraA[0:128, tsl], writes=rs(tw), eng="act")
                kb.dma(adp[:], loraA[128:256, tsl], writes=rs(adp), eng="sp")
                shift_rows(kb, F["r_"][:], Pr, F["tA"], hmu["mu_r"], omu["mu_r"], 128, S, F["r_"].r)
                load_halo(kb, Pv, projT[2048 + c0:2048 + c0 + 128, :], t0, S, eng="sp")
                shift_rows(kb, F["k_"][:], Pk, F["tA"], hmu["mu_k"], omu["mu_k"], 128, S, F["k_"].r)
                shift_rows(kb, F["v_"][:], Pv, F["tA"], hmu["mu_v"], omu["mu_v"], 128, S, F["v_"].r)
                w0c = col["w0f"] if d == 0 else col["w0b"]
                a0c = col["a0f"] if d == 0 else col["a0b"]
                for hf in range(2):
                    sl = slice(hf * 512, (hf + 1) * 512)
                    pt, pr = ps.next()
                    kb.op("pe", lambda e, pt=pt, sl=sl, d=d: e.matmul(pt[:], lhsT=wlp[d][:], rhs=tw[:, sl], start=True, stop=True),
                          reads=rs(wlp[d], tw), writes=[pr])
                    kb.op("act", lambda e, pt=pt, sl=sl, w0c=w0c: e.activation(out=F["sgw"][:, sl], in_=pt[:], func=AF.Sigmoid, bias=w0c[:, 0:1]),
                          reads=[pr] + rs(w0c), writes=rs(F["sgw"]))
                    pt, pr = ps.next()
                    kb.op("pe", lambda e, pt=pt, sl=sl, d=d: e.matmul(pt[:], lhsT=alp[d][:], rhs=adp[:, sl], start=True, stop=True),
                          reads=rs(alp[d], adp), writes=[pr])
                    kb.op("act", lambda e, pt=pt, sl=sl, a0c=a0c: e.activation(out=F["a_"][:, sl], in_=pt[:], func=AF.Sigmoid, bias=a0c[:, 0:1]),
                          reads=[pr] + rs(a0c), writes=rs(F["a_"]))
                ew("dve", "tensor_scalar", [F["k_"], col["k_k"]], [F["kk"]], out=F["kk"][:], in0=F["k_"][:], scalar1=col["k_k"][:, 0:1], scalar2=None, op0=ALU.mult)
                ew("act", "activation", [F["kk"]], [sqr], out=sqr[:], in_=F["kk"][:], func=AF.Square)
                for hf in range(2):
                    sl = slice(hf * 512, (hf + 1) * 512)
                    pt, pr = ps.next()
                    kb.op("pe", lambda e, pt=pt, sl=sl: e.matmul(pt[:], lhsT=bones[:], rhs=sqr[:, sl], start=True, stop=True),
                          reads=rs(bones, sqr), writes=[pr])
                    kb.op("act", lambda e, pt=pt, sl=sl: e.activation(out=F["tB"][:, sl], in_=pt[:], func=AF.Sqrt),
                          reads=[pr], writes=rs(F["tB"]))
                ew("dve", "tensor_scalar", [F["tB"]], [F["tB"]], out=F["tB"][:], in0=F["tB"][:], scalar1=1e-12, scalar2=None, op0=ALU.max)
                ew("dve", "reciprocal", [F["tB"]], [F["tB"]], out=F["tB"][:], in_=F["tB"][:])
                ew("dve", "tensor_tensor", [F["kk"], F["tB"]], [F["kkn"]], out=F["kkn"][:], in0=F["kk"][:], in1=F["tB"][:], op=ALU.mult)
                ew("dve", "tensor_scalar", [F["a_"], col["k_a"], omka], [F["tB"]], out=F["tB"][:], in0=F["a_"][:], scalar1=col["k_a"][:, 0:1], scalar2=omka[:, 0:1], op0=ALU.mult, op1=ALU.add)
                ew("dve", "tensor_tensor", [F["k_"], F["tB"]], [F["kdir"]], out=F["kdir"][:], in0=F["k_"][:], in1=F["tB"][:], op=ALU.mult)
                ew("pool", "tensor_tensor", [F["kkn"], F["a_"]], [F["b_"]], out=F["b_"][:], in0=F["kkn"][:], in1=F["a_"][:], op=ALU.mult)
                ew("pool", "tensor_scalar", [F["sgw"]], [F["lw"]], out=F["lw"][:], in0=F["sgw"][:], scalar1=-CEXP, scalar2=None, op0=ALU.mult)
                ew("dve", "tensor_tensor_scan", [cmask, F["lw"]], [F["Pp"]], out=F["Pp"][:], data0=cmask[:], data1=F["lw"][:], initial=0.0, op0=ALU.mult, op1=ALU.add)
                ew("pool", "tensor_tensor", [F["Pp"], F["lw"]], [F["E_"]], out=F["E_"][:], in0=F["Pp"][:], in1=F["lw"][:], op=ALU.subtract)
                Pv3 = F["Pp"][:].rearrange("p (c t) -> p c t", t=64)
                ew("dve", "tensor_copy", [F["Pp"]], [tot], out=tot[:], in_=Pv3[:, :, 63])
                ew("dve", "tensor_tensor", [tot, F["Pp"]], [F["R_"]], out=F["R_"][:].rearrange("p (c t) -> p c t", t=64),
                   in0=tot[:].unsqueeze(2).to_broadcast([128, NCH, 64]), in1=Pv3, op=ALU.subtract)
                ew("act", "activation", [tot], [wC], out=wC[:], in_=tot[:], func=AF.Exp)
                if d == 0:
                    Lc, Lp, Lr = F["Pp"], F["E_"], F["R_"]
                else:
                    ew("dve", "tensor_tensor", [tot, F["E_"]], [F["Sf"]], out=F["Sf"][:].rearrange("p (c t) -> p c t", t=64),
                       in0=tot[:].unsqueeze(2).to_broadcast([128, NCH, 64]), in1=F["E_"][:].rearrange("p (c t) -> p c t", t=64), op=ALU.subtract)
                    Lc, Lp, Lr = F["Sf"], F["R_"], F["E_"]
                ew("act", "activation", [Lc], [F["po1"]], out=F["po1"][:], in_=Lc[:], func=AF.Exp)
                ew("act", "activation", [Lp], [F["po2"]], out=F["po2"][:], in_=Lp[:], func=AF.Exp)
                ew("act", "activation", [Lc], [F["po3"]], out=F["po3"][:], in_=Lc[:], func=AF.Exp, scale=-1.0)
                ew("act", "activation", [Lr], [F["tA"]], out=F["tA"][:], in_=Lr[:], func=AF.Exp)
                ew("dve", "tensor_tensor", [F["kkn"], F["po2"]], [Bf["qt"]], out=Bf["qt"][:], in0=F["kkn"][:], in1=F["po2"][:], op=ALU.mult)
                ew("pool", "tensor_tensor", [F["r_"], F["po1"]], [Bf["rt"]], out=Bf["rt"][:], in0=F["r_"][:], in1=F["po1"][:], op=ALU.mult)
                ew("dve", "tensor_tensor", [F["kdir"], F["po3"]], [Bf["kt"]], out=Bf["kt"][:], in0=F["kdir"][:], in1=F["po3"][:], op=ALU.mult)
                ew("dve", "scalar_tensor_tensor", [F["b_"], F["po3"]], [Bf["nbt"]], out=Bf["nbt"][:], in0=F["b_"][:], scalar=-1.0, in1=F["po3"][:], op0=ALU.mult, op1=ALU.mult)
                ew("pool", "tensor_tensor", [F["kdir"], F["tA"]], [Bf["Kh"]], out=Bf["Kh"][:], in0=F["kdir"][:], in1=F["tA"][:], op=ALU.mult)
                ew("dve", "scalar_tensor_tensor", [F["b_"], F["tA"]], [Bf["Bn"]], out=Bf["Bn"][:], in0=F["b_"][:], scalar=-1.0, in1=F["tA"][:], op0=ALU.mult, op1=ALU.mult)
                ew("act", "copy", [F["v_"]], [Bf["vb"]], out=Bf["vb"][:], in_=F["v_"][:])
                if dbg is not None and d == 0 and s == 0:
                    for nm in ("r_", "k_", "v_", "a_", "kkn", "kdir", "b_", "lw", "Pp"):
                        kb.dma(dbg[nm], F[nm][:], reads=rs(F[nm]))
                m_s = cf["m_up"] if d == 0 else cf["m_lo"]
                m_st = cf["m_lo"] if d == 0 else cf["m_up"]
                m_i = cf["m_upi"] if d == 0 else cf["m_loi"]
                BI = [0, 1]

                def mm_batch(bi, out_fn, lhs_fn, rhs_fn, reads, n_acc=1, lhs2=None, rhs2=None):
                    pt, pr = ps.next()
                    for g in range(8):
                        for h in range(2):
                            hs = HS[h]
                            gs = slice(64 * g, 64 * g + 64)
                            if lhs2 is None:
                                kb.op("pe", lambda e, pt=pt, hs=hs, gs=gs, g=g: e.matmul(out_fn(pt, hs, gs), lhsT=lhs_fn(hs, g), rhs=rhs_fn(hs, g), start=True, stop=True),
                                      reads=reads, writes=[pr], pe_accum=True)
                            else:
                                kb.op("pe", lambda e, pt=pt, hs=hs, gs=gs, g=g: e.matmul(out_fn(pt, hs, gs), lhsT=lhs_fn(hs, g), rhs=rhs_fn(hs, g), start=True, stop=False),
                                      reads=reads, writes=[pr], pe_accum=True)
                                kb.op("pe", lambda e, pt=pt, hs=hs, gs=gs, g=g: e.matmul(out_fn(pt, hs, gs), lhsT=lhs2(hs, g), rhs=rhs2(hs, g), start=False, stop=True),
                                      reads=reads, writes=[pr], pe_accum=True)
                    return pt, pr

                def f32out(pt, hs, gs):
                    return pt[hs, gs]

                def cm(tile, bi):
                    return lambda hs, g: tile[hs, bi * 512 + 64 * g: bi * 512 + 64 * g + 64]

                def tmj(tile):
                    return lambda hs, g: tile[hs, 64 * g:64 * g + 64]

                for (src, dst) in (("vb", "Vtm"), ("qt", "Qtm"), ("Kh", "Khtm"), ("Bn", "Bhtm")):
                    for bi in BI:
                        pt, pr = ps.next()
                        pv = psbf(pt)
                        for g in range(8):
                            for h in range(2):
                                hs = HS[h]
                                kb.op("pe", lambda e, pv=pv, hs=hs, g=g, src=src, bi=bi: e.transpose(out=pv[hs, 64 * g:64 * g + 64], in_=Bf[src][hs, bi * 512 + 64 * g: bi * 512 + 64 * g + 64], identity=ident[hs, :]),
                                      reads=rs(Bf[src], ident), writes=[pr], pe_accum=True)
                        kb.op("act", lambda e, pv=pv, dst=dst, bi=bi: e.copy(out=PB[bi][dst][:], in_=pv[:, 0:512]), reads=[pr], writes=rs(PB[bi][dst]))
                for bi in BI:
                    pt, pr = mm_batch(bi, f32out, cm(Bf["nbt"], bi), cm(Bf["qt"], bi), rs(Bf["nbt"], Bf["qt"]))
                    ew("dve", "tensor_tensor", [pr, m_s], [PB[bi]["Um"]], out=PB[bi]["Um"][:], in0=pt[:], in1=m_s[:], op=ALU.mult)
                    ew("pool", "tensor_tensor", [PB[bi]["Um"], cf["itile"]], [PB[bi]["Xa"]], out=PB[bi]["Xa"][:], in0=PB[bi]["Um"][:], in1=cf["itile"][:], op=ALU.add)
                for bi in BI:
                    pt, pr = mm_batch(bi, f32out, cm(Bf["qt"], bi), cm(Bf["nbt"], bi), rs(Bf["nbt"], Bf["qt"]))
                    ew("dve", "tensor_tensor", [pr, m_st], [PB[bi]["Utm"]], out=PB[bi]["Utm"][:], in0=pt[:], in1=m_st[:], op=ALU.mult)
                for (lname, rname, mask, dst) in (("kt", "qt", m_s, "AkkT"), ("kt", "rt", m_i, "ArkT"), ("nbt", "rt", m_i, "nArbT")):
                    for bi in BI:
                        pt, pr = mm_batch(bi, f32out, cm(Bf[lname], bi), cm(Bf[rname], bi), rs(Bf[lname], Bf[rname]))
                        ew("dve", "tensor_tensor", [pr, mask], [PB[bi][dst]], out=PB[bi][dst][:], in0=pt[:], in1=mask[:], op=ALU.mult)
                for bi in BI:
                    pt, pr = mm_batch(bi, f32out, tmj(PB[bi]["AkkT"]), tmj(PB[bi]["Vtm"]), rs(PB[bi]["AkkT"], PB[bi]["Vtm"]))
                    ew("act", "copy", [pr], [PB[bi]["AV"]], out=PB[bi]["AV"][:], in_=pt[:])
                Pc = {bi: ("Um", "Utm") for bi in BI}
                Xc = {bi: "Xa" for bi in BI}
                for lev in range(1, 6):
                    pn, ptn = ("Pa", "Pta") if lev % 2 == 1 else ("Pb", "Ptb")
                    for bi in BI:
                        P_, Pt_ = Pc[bi]
                        if lev < 5:
                            pt, pr = mm_batch(bi, f32out, tmj(PB[bi][Pt_]), tmj(PB[bi][P_]), rs(PB[bi][Pt_], PB[bi][P_]))
                            ew("act", "copy", [pr], [PB[bi][pn]], out=PB[bi][pn][:], in_=pt[:])
                        pt, pr = mm_batch(bi, f32out, tmj(PB[bi][P_]), tmj(PB[bi][Pt_]), rs(PB[bi][Pt_], PB[bi][P_]))
                        ew("act", "copy", [pr], [PB[bi][ptn]], out=PB[bi][ptn][:], in_=pt[:])
                    for bi in BI:
                        xo = Xc[bi]
                        xn = "Xb" if xo == "Xa" else "Xa"
                        pt, pr = mm_batch(bi, f32out, tmj(PB[bi][ptn]), tmj(PB[bi][xo]), rs(PB[bi][ptn], PB[bi][xo]))
                        ew("dve", "tensor_tensor", [pr, PB[bi][xo]], [PB[bi][xn]], out=PB[bi][xn][:], in0=pt[:], in1=PB[bi][xo][:], op=ALU.add)
                        Xc[bi] = xn
                        Pc[bi] = (pn, ptn)
                for bi in BI:
                    X5 = PB[bi][Xc[bi]]
                    pt, pr = mm_batch(bi, f32out, tmj(X5), tmj(PB[bi]["AV"]), rs(X5, PB[bi]["AV"]))
                    ew("act", "copy", [pr], [PB[bi]["Gtm"]], out=PB[bi]["Gtm"][:], in_=pt[:])
                    pt, pr = mm_batch(bi, f32out, tmj(X5), tmj(PB[bi]["Qtm"]), rs(X5, PB[bi]["Qtm"]))
                    ew("act", "copy", [pr], [PB[bi]["Qh"]], out=PB[bi]["Qh"][:], in_=pt[:])
                for bi in BI:
                    pt, pr = mm_batch(bi, f32out, tmj(PB[bi]["Qh"]), tmj(PB[bi]["Bhtm"]), rs(PB[bi]["Qh"], PB[bi]["Bhtm"]))
                    for g in range(8):
                        cidx = bi * 8 + g
                        gs = slice(64 * g, 64 * g + 64)
                        ew("dve", "scalar_tensor_tensor", [pr, cf["itile"], wC], [PB[bi]["PhiT"]], out=PB[bi]["PhiT"][:, gs], in0=cf["itile"][:, gs],
                           scalar=wC[:, cidx:cidx + 1], in1=pt[:, gs], op0=ALU.mult, op1=ALU.add)
                    pt, pr = mm_batch(bi, f32out, tmj(PB[bi]["Qh"]), tmj(PB[bi]["nArbT"]), rs(PB[bi]["Qh"], PB[bi]["nArbT"]))
                    ew("dve", "tensor_tensor", [pr, Bf["rt"]], [PB[bi]["RhT"]], out=PB[bi]["RhT"][:], in0=pt[:], in1=Bf["rt"][:, bi * 512:(bi + 1) * 512], op=ALU.add)
                    pt, pr = mm_batch(bi, f32out, tmj(PB[bi]["Khtm"]), tmj(PB[bi]["Vtm"]), rs(PB[bi]["Khtm"], PB[bi]["Vtm"], PB[bi]["Bhtm"], PB[bi]["Gtm"]),
                                      lhs2=tmj(PB[bi]["Bhtm"]), rhs2=tmj(PB[bi]["Gtm"]))
                    ew("act", "copy", [pr], [Psi[bi]], out=Psi[bi][:], in_=pt[:])
                order = list(range(NCH))
                if d == 1:
                    order = order[::-1]
                for cidx in order:
                    bi, g = divmod(cidx, 8)
                    gs = slice(64 * g, 64 * g + 64)
                    pt, pr = ps.next()
                    for h in range(2):
                        hs = HS[h]
                        kb.op("pe", lambda e, pt=pt, hs=hs, gs=gs, bi=bi: e.matmul(pt[hs, 0:64], lhsT=PB[bi]["Vtm"][hs, gs], rhs=PB[bi]["ArkT"][hs, gs], start=True, stop=False),
                              reads=rs(PB[bi]["Vtm"], PB[bi]["ArkT"]), writes=[pr], pe_accum=True)
                        kb.op("pe", lambda e, pt=pt, hs=hs, gs=gs, bi=bi: e.matmul(pt[hs, 0:64], lhsT=PB[bi]["Gtm"][hs, gs], rhs=PB[bi]["nArbT"][hs, gs], start=False, stop=False),
                              reads=rs(PB[bi]["Gtm"], PB[bi]["nArbT"]), writes=[pr], pe_accum=True)
                        kb.op("pe", lambda e, pt=pt, hs=hs, gs=gs, bi=bi: e.matmul(pt[hs, 0:64], lhsT=Hbf[hs, :], rhs=PB[bi]["RhT"][hs, gs], start=False, stop=True),
                              reads=rs(Hbf, PB[bi]["RhT"]), writes=[pr], pe_accum=True)
                    ew("act", "copy", [pr], [F["yc"]], out=F["yc"][:, cidx * 64:(cidx + 1) * 64], in_=pt[:, 0:64])
                    pt2, pr2 = ps.next()
                    for h in range(2):
                        hs = HS[h]
                        kb.op("pe", lambda e, pt2=pt2, hs=hs, gs=gs, bi=bi: e.matmul(pt2[hs, 0:64], lhsT=PB[bi]["PhiT"][hs, gs], rhs=Hbf[hs, :], start=True, stop=True),
                              reads=rs(PB[bi]["PhiT"], Hbf), writes=[pr2], pe_accum=True)
                    ew("dve", "tensor_tensor", [pr2, Psi[bi]], [Hbf], out=Hbf[:], in0=pt2[:, 0:64], in1=Psi[bi][:, gs], op=ALU.add)
                if d == 0:
                    kb.dma(yfT[c0:c0 + 128, tsl], F["yc"][:], reads=rs(F["yc"]), eng="pool")
                else:
                    kb.dma(F["yf"][:], yfT[c0:c0 + 128, tsl], writes=rs(F["yf"]), eng="sp")
                    kb.dma(sg0[:], loraA[256:384, tsl], writes=rs(sg0), eng="act")
                    kb.dma(sg1[0:32, :], loraA[384:416, tsl], writes=rs(sg1), eng="act")
                    for hf in range(2):
                        sl = slice(hf * 512, (hf + 1) * 512)
                        pt, pr = ps.next()
                        kb.op("pe", lambda e, pt=pt, sl=sl: e.matmul(pt[:], lhsT=alp[0][:], rhs=adp[:, sl], start=True, stop=True),
                              reads=rs(alp[0], adp), writes=[pr])
                        kb.op("act", lambda e, pt=pt, sl=sl: e.activation(out=F["af"][:, sl], in_=pt[:], func=AF.Sigmoid, bias=col["a0f"][:, 0:1]),
                              reads=[pr] + rs(col["a0f"]), writes=rs(F["af"]))
                    ew("dve", "tensor_tensor", [F["af"], F["a_"]], [F["af"]], out=F["af"][:], in0=F["af"][:], in1=F["a_"][:], op=ALU.add)
                    ew("dve", "tensor_scalar", [F["af"], hka, omka], [F["af"]], out=F["af"][:], in0=F["af"][:], scalar1=hka[:, 0:1], scalar2=omka[:, 0:1], op0=ALU.mult, op1=ALU.add)
                    ew("dve", "tensor_tensor", [F["af"], F["k_"]], [F["af"]], out=F["af"][:], in0=F["af"][:], in1=F["k_"][:], op=ALU.mult)
                    ew("dve", "scalar_tensor_tensor", [F["af"], col["r_k"], F["r_"]], [sqr], out=sqr[:], in0=F["af"][:], scalar=col["r_k"][:, 0:1], in1=F["r_"][:], op0=ALU.mult, op1=ALU.mult)
                    ew("dve", "tensor_tensor", [F["yc"], F["yf"]], [F["yc"]], out=F["yc"][:], in0=F["yc"][:], in1=F["yf"][:], op=ALU.add)
                    ew("act", "copy", [F["yc"]], [tw], out=tw[:], in_=F["yc"][:])
                    ew("act", "activation", [F["yc"]], [adp], out=adp[:], in_=F["yc"][:], func=AF.Square)
                    for hf in range(2):
                        sl = slice(hf * 512, (hf + 1) * 512)
                        for (src, dstn, scale) in ((tw, "po1", 1.0 / 64), (adp, "po2", 1.0 / 64), (sqr, "po3", 1.0)):
                            pt, pr = ps.next()
                            kb.op("pe", lambda e, pt=pt, sl=sl, src=src: e.matmul(pt[:], lhsT=bones[:], rhs=src[:, sl], start=True, stop=True),
                                  reads=rs(bones, src), writes=[pr])
                            kb.op("act", lambda e, pt=pt, sl=sl, dstn=dstn, scale=scale: e.mul(out=F[dstn][:, sl], in_=pt[:], mul=scale),
                                  reads=[pr], writes=rs(F[dstn]))
                        pt, pr = ps.next()
                        kb.op("pe", lambda e, pt=pt, sl=sl: e.matmul(pt[:], lhsT=gl0[:], rhs=sg0[:, sl], start=True, stop=False),
                              reads=rs(gl0, sg0), writes=[pr], pe_accum=True)
                        kb.op("pe", lambda e, pt=pt, sl=sl: e.matmul(pt[:], lhsT=gl1[:], rhs=sg1[:, sl], start=False, stop=True),
                              reads=rs(gl1, sg1), writes=[pr], pe_accum=True)
                        kb.op("act", lambda e, pt=pt, sl=sl: e.copy(out=F["tA"][:, sl], in_=pt[:]), reads=[pr], writes=rs(F["tA"]))
                    ew("dve", "tensor_tensor", [F["po1"]], [F["tB"]], out=F["tB"][:], in0=F["po1"][:], in1=F["po1"][:], op=ALU.mult)
                    ew("dve", "tensor_tensor", [F["po2"], F["tB"]], [F["po2"]], out=F["po2"][:], in0=F["po2"][:], in1=F["tB"][:], op=ALU.subtract)
                    ew("act", "activation", [F["po2"]], [F["po2"]], out=F["po2"][:], in_=F["po2"][:], func=AF.Sqrt, bias=64e-5)
                    ew("dve", "reciprocal", [F["po2"]], [F["po2"]], out=F["po2"][:], in_=F["po2"][:])
                    ew("dve", "tensor_tensor", [F["yc"], F["po1"]], [F["yc"]], out=F["yc"][:], in0=F["yc"][:], in1=F["po1"][:], op=ALU.subtract)
                    ew("dve", "tensor_tensor", [F["yc"], F["po2"]], [F["yc"]], out=F["yc"][:], in0=F["yc"][:], in1=F["po2"][:], op=ALU.mult)
                    ew("dve", "tensor_scalar", [F["yc"], col["lnx_g"], col["lnx_b"]], [F["yc"]], out=F["yc"][:], in0=F["yc"][:], scalar1=col["lnx_g"][:, 0:1], scalar2=col["lnx_b"][:, 0:1], op0=ALU.mult, op1=ALU.add)
                    ew("dve", "tensor_tensor", [F["po3"], F["v_"]], [F["po3"]], out=F["po3"][:], in0=F["po3"][:], in1=F["v_"][:], op=ALU.mult)
                    ew("dve", "tensor_tensor", [F["yc"], F["po3"]], [F["yc"]], out=F["yc"][:], in0=F["yc"][:], in1=F["po3"][:], op=ALU.add)
                    ew("dve", "tensor_tensor", [F["yc"], F["tA"]], [yout], out=yout[:], in0=F["yc"][:], in1=F["tA"][:], op=ALU.mult)
                    kb.dma(yaT[c0:c0 + 128, tsl], yout[:], reads=rs(yout), eng="pool")
    kb.emit()


def stage_pool(nc, projT, pool_w, pool_scale, ybT, cnt_inv):
    kb = KB(nc)
    S = 1024
    HL = 8
    ps = PS(kb)
    Pp = [Tl(kb, f"Pp{i}", [128, S + 2 * HL]) for i in range(2)]
    acc = Tl(kb, "acc", [128, S + 2 * HL])
    acc2 = Tl(kb, "acc2", [128, S + 2 * HL])
    z = [Tl(kb, f"z{i}", [128, S], F32R) for i in range(2)]
    ci = Tl(kb, "ci", [128, S])
    pw = [Tl(kb, f"pw{i}", [128, 256], F32R) for i in range(2)]
    sc = Tl(kb, "sc", [128, 1])
    ot = [Tl(kb, f"ot{i}", [128, S], F32R) for i in range(2)]
    oi = 0
    for g, win in enumerate((2, 4, 8, 16)):
        half = win // 2
        for kc in range(2):
            kb.dma(pw[kc][:], pool_w[g, kc * 128:(kc + 1) * 128, :], writes=rs(pw[kc]))
        for s in range(T // S):
            t0 = s * S
            kb.dma(ci[:], cnt_inv[g:g + 1, t0:t0 + S].partition_broadcast(128), writes=rs(ci), eng="act")
            for kc in range(2):
                r0 = 3488 + g * 256 + kc * 128
                P = Pp[kc]
                lo = max(t0 - HL, 0)
                hi = min(t0 + S + HL, T)
                kb.dma(P[:, lo - (t0 - HL):hi - (t0 - HL)], projT[r0:r0 + 128, lo:hi], writes=rs(P), eng="sp")
                if t0 == 0:
                    kb.op("pool", lambda e, P=P: e.memset(P[:, 0:HL], 0.0), writes=rs(P))
                if t0 + S == T:
                    kb.op("pool", lambda e, P=P: e.memset(P[:, S + HL:S + 2 * HL], 0.0), writes=rs(P))
                W_ = S + 2 * HL
                kb.op("dve", lambda e, P=P: e.tensor_tensor(out=acc[:, 1:W_], in0=P[:, 0:W_ - 1], in1=P[:, 1:W_], op=ALU.add),
                      reads=rs(P), writes=rs(acc))
                cur, oth = acc, acc2
                wlen = 2
                while wlen < win:
                    kb.op("dve", lambda e, cur=cur, oth=oth, wlen=wlen: e.tensor_tensor(out=oth[:, 2 * wlen - 1:W_], in0=cur[:, 2 * wlen - 1:W_], in1=cur[:, wlen - 1:W_ - wlen], op=ALU.add),
                          reads=rs(cur), writes=rs(oth))
                    cur, oth = oth, cur
                    wlen *= 2
                off = HL + half - 1
                kb.op("dve", lambda e, cur=cur, off=off: e.tensor_tensor(out=acc2[:, 0:S] if cur is acc else acc[:, 0:S], in0=cur[:, off:off + S], in1=ci[:], op=ALU.mult),
                      reads=rs(cur, ci), writes=rs(acc2 if cur is acc else acc))
                mt = acc2 if cur is acc else acc
                kb.op("dve", lambda e, mt=mt, P=P, kc=kc: e.tensor_tensor(out=z[kc][:], in0=mt[:, 0:S], in1=P[:, HL:HL + S], op=ALU.subtract),
                      reads=rs(mt, P), writes=rs(z[kc]))
            for oc in range(2):
                c0 = g * 256 + oc * 128
                load_col(kb, sc, pool_scale[c0:c0 + 128])
                o = ot[oi % 2]
                oi += 1
                for hf in range(2):
                    sl = slice(hf * 512, (hf + 1) * 512)
                    pt, pr = ps.next()
                    for kc in range(2):
                        kb.op("pe", lambda e, pt=pt, kc=kc, oc=oc, sl=sl: e.matmul(pt[:], lhsT=pw[kc][:, oc * 128:(oc + 1) * 128], rhs=z[kc][:, sl], start=(kc == 0), stop=(kc == 1)),
                              reads=rs(pw[kc], z[kc]), writes=[pr], pe_accum=True)
                    kb.op("act", lambda e, pt=pt, o=o, sl=sl: e.activation(out=o[:, sl], in_=pt[:], func=AF.Identity, scale=sc[:, 0:1]),
                          reads=[pr] + rs(sc), writes=rs(o))
                kb.dma(ybT[c0:c0 + 128, t0:t0 + S], o[:], reads=rs(o), eng="pool")
    kb.emit()


def stage_mix(nc, xin, projT, yaT, ybT, w_up_a, w_up_b, w_o, g2, w_router, xmT, v_tm, affT, identf_ap):
    kb = KB(nc)
    N = 512
    KC = D // 128
    ps = PS(kb)
    ya = Tl(kb, "ya", [128, 8, N], F32R)
    yb = Tl(kb, "yb", [128, 8, N], F32R)
    mg = kb.sb("mg", [128, KC, N], F32R)
    mg_r = [kb.res(f"mg{k}") for k in range(KC)]
    xm = kb.sb("xm", [128, KC, N])
    xm_r = [kb.res(f"xm{k}") for k in range(KC)]
    vt = kb.sb("vt", [128, KC, N])
    vt_r = [kb.res(f"vt{k}") for k in range(KC)]
    wa = [Tl(kb, f"wa{i}", [128, 8, 128], F32R) for i in range(2)]
    wb = [Tl(kb, f"wb{i}", [128, 8, 128], F32R) for i in range(2)]
    wo = [Tl(kb, f"wo{i}", [128, KC, 128], F32R) for i in range(2)]
    sgA = [Tl(kb, f"sgA{i}", [128, N]) for i in range(2)]
    sgB = [Tl(kb, f"sgB{i}", [128, N]) for i in range(2)]
    xi = [Tl(kb, f"xi{i}", [128, N]) for i in range(2)]
    t1 = Tl(kb, "t1", [128, N])
    t2 = Tl(kb, "t2", [128, N])
    sq = [Tl(kb, f"sq{i}", [128, N], F32R) for i in range(2)]
    rstd = Tl(kb, "rstd", [128, N])
    gt = Tl(kb, "gt", [128, KC])
    onesf = Tl(kb, "onesf", [128, 128])
    ones = Tl(kb, "ones", [128, 128], F32R)
    idf = Tl(kb, "idf", [128, 128])
    wr = Tl(kb, "wr", [128, KC, NE])
    ex = Tl(kb, "ex", [NE, N])
    rsum = Tl(kb, "rsum", [NE, N])
    vo = [Tl(kb, f"vo{i}", [128, D], BF16) for i in range(2)]
    kb.op("pool", lambda e: e.memset(onesf[:], 1.0), writes=rs(onesf))
    kb.op("dve", lambda e: e.tensor_copy(out=ones[:], in_=onesf[:]), reads=rs(onesf), writes=rs(ones))
    kb.dma(idf[:], identf_ap, writes=rs(idf))
    kb.dma(gt[:], g2.rearrange("(k p) -> p k", p=128), writes=rs(gt), allow_slow_non_contiguous=True)
    kb.dma(wr[:], w_router.rearrange("(k p) e -> p k e", p=128), writes=rs(wr))
    wav = w_up_a.rearrange("(k p) n -> p k n", p=128)
    wbv = w_up_b.rearrange("(k p) n -> p k n", p=128)
    wov = w_o.rearrange("(k p) n -> p k n", p=128)
    it = 0
    voi = 0
    for s in range(T // N):
        tsl = slice(s * N, (s + 1) * N)
        kb.dma(ya[:], yaT[:, tsl].rearrange("(k p) n -> p k n", p=128), writes=rs(ya), eng="sp")
        kb.dma(yb[:], ybT[:, tsl].rearrange("(k p) n -> p k n", p=128), writes=rs(yb), eng="act")
        for oc in range(KC):
            b = it % 2
            it += 1
            osl = slice(oc * 128, (oc + 1) * 128)
            kb.dma(wa[b][:], wav[:, :, osl], writes=rs(wa[b]), eng="sp")
            kb.dma(wb[b][:], wbv[:, :, osl], writes=rs(wb[b]), eng="act")
            kb.dma(sgA[b][:], projT[4512 + oc * 128:4512 + (oc + 1) * 128, tsl], writes=rs(sgA[b]), eng="sp")
            kb.dma(sgB[b][:], projT[6560 + oc * 128:6560 + (oc + 1) * 128, tsl], writes=rs(sgB[b]), eng="act")
            pa, par = ps.next()
            for kc in range(8):
                kb.op("pe", lambda e, pa=pa, b=b, kc=kc: e.matmul(pa[:], lhsT=wa[b][:, kc, :], rhs=ya[:, kc, :], start=(kc == 0), stop=(kc == 7)),
                      reads=rs(wa[b], ya), writes=[par], pe_accum=True)
            pb_, pbr = ps.next()
            for kc in range(8):
                kb.op("pe", lambda e, pb_=pb_, b=b, kc=kc: e.matmul(pb_[:], lhsT=wb[b][:, kc, :], rhs=yb[:, kc, :], start=(kc == 0), stop=(kc == 7)),
                      reads=rs(wb[b], yb), writes=[pbr], pe_accum=True)
            kb.op("dve", lambda e, pa=pa, b=b: e.tensor_tensor(out=t1[:], in0=pa[:], in1=sgA[b][:], op=ALU.mult), reads=[par] + rs(sgA[b]), writes=rs(t1))
            kb.op("dve", lambda e, pb_=pb_, b=b: e.tensor_tensor(out=t2[:], in0=pb_[:], in1=sgB[b][:], op=ALU.mult), reads=[pbr] + rs(sgB[b]), writes=rs(t2))
            kb.op("pool", lambda e, oc=oc: e.tensor_tensor(out=mg[:, oc, :], in0=t1[:], in1=t2[:], op=ALU.add), reads=rs(t1, t2), writes=[mg_r[oc]])
        pss, pssr = ps.next()
        for oc in range(KC):
            b = it % 2
            it += 1
            osl = slice(oc * 128, (oc + 1) * 128)
            kb.dma(wo[b][:], wov[:, :, osl], writes=rs(wo[b]), eng="sp")
            kb.dma(xi[b][:], xin[osl, tsl], writes=rs(xi[b]), eng="act")
            po, por = ps.next()
            if po is pss:
                po, por = ps.next()
            for kc in range(KC):
                kb.op("pe", lambda e, po=po, b=b, kc=kc: e.matmul(po[:], lhsT=wo[b][:, kc, :], rhs=mg[:, kc, :], start=(kc == 0), stop=(kc == KC - 1)),
                      reads=[wo[b].r, mg_r[kc]], writes=[por], pe_accum=True)
            kb.op("dve", lambda e, po=po, b=b, oc=oc: e.tensor_tensor(out=xm[:, oc, :], in0=po[:], in1=xi[b][:], op=ALU.add), reads=[por] + rs(xi[b]), writes=[xm_r[oc]])
            kb.dma(xmT[osl, tsl], xm[:, oc, :], reads=[xm_r[oc]], eng="pool")
            a = oc % 2
            kb.op("act", lambda e, a=a, oc=oc: e.activation(out=sq[a][:], in_=xm[:, oc, :], func=AF.Square), reads=[xm_r[oc]], writes=rs(sq[a]))
            kb.op("pe", lambda e, a=a, oc=oc, pss=pss: e.matmul(pss[:], lhsT=ones[:], rhs=sq[a][:], start=(oc == 0), stop=(oc == KC - 1)),
                  reads=rs(ones, sq[a]), writes=[pssr], pe_accum=True)
        kb.op("act", lambda e, pss=pss: e.activation(out=rstd[:], in_=pss[:], func=AF.Sqrt, scale=1.0 / D, bias=EPS), reads=[pssr], writes=rs(rstd))
        kb.op("dve", lambda e: e.reciprocal(out=rstd[:], in_=rstd[:]), writes=rs(rstd))
        for oc in range(KC):
            kb.op("dve", lambda e, oc=oc: e.scalar_tensor_tensor(out=vt[:, oc, :], in0=xm[:, oc, :], scalar=gt[:, oc:oc + 1], in1=rstd[:], op0=ALU.mult, op1=ALU.mult),
                  reads=[xm_r[oc]] + rs(gt, rstd), writes=[vt_r[oc]])
        pl, plr = ps.next()
        for kc in range(KC):
            kb.op("pe", lambda e, pl=pl, kc=kc: e.matmul(pl[0:NE, :], lhsT=wr[:, kc, :], rhs=vt[:, kc, :], start=(kc == 0), stop=(kc == KC - 1)),
                  reads=[wr.r, vt_r[kc]], writes=[plr], pe_accum=True)
        kb.op("act", lambda e, pl=pl: e.activation(out=ex[:], in_=pl[0:NE, :], func=AF.Exp), reads=[plr], writes=rs(ex))
        pq, pqr = ps.next()
        kb.op("pe", lambda e, pq=pq: e.matmul(pq[0:NE, :], lhsT=onesf[0:NE, 0:NE], rhs=ex[:], start=True, stop=True), reads=rs(onesf, ex), writes=[pqr])
        kb.op("dve", lambda e, pq=pq: e.reciprocal(out=rsum[:], in_=pq[0:NE, :]), reads=[pqr], writes=rs(rsum))
        kb.op("dve", lambda e: e.tensor_tensor(out=ex[:], in0=ex[:], in1=rsum[:], op=ALU.mult), reads=rs(rsum), writes=rs(ex))
        kb.dma(affT[:, tsl], ex[:], reads=rs(ex), eng="sp")
        for tb in range(N // 128):
            o = vo[voi % 2]
            voi += 1
            for q in range(4):
                ptt, ptr = ps.next()
                for j in range(4):
                    oc = q * 4 + j
                    kb.op("pe", lambda e, ptt=ptt, oc=oc, tb=tb, j=j: e.transpose(out=ptt[:, j * 128:(j + 1) * 128], in_=vt[:, oc, tb * 128:(tb + 1) * 128], identity=idf[:]),
                          reads=[vt_r[oc]] + rs(idf), writes=[ptr], pe_accum=True)
                eng = "act" if q % 2 == 0 else "dve"
                if eng == "act":
                    kb.op("act", lambda e, ptt=ptt, o=o, q=q: e.copy(out=o[:, q * 512:(q + 1) * 512], in_=ptt[:]), reads=[ptr], writes=rs(o))
                else:
                    kb.op("dve", lambda e, ptt=ptt, o=o, q=q: e.tensor_copy(out=o[:, q * 512:(q + 1) * 512], in_=ptt[:]), reads=[ptr], writes=rs(o))
            kb.dma(v_tm[s * N + tb * 128:s * N + (tb + 1) * 128, :], o[:], reads=rs(o), eng="pool")
    kb.emit()


def stage_route(nc, affT, posD, gateD):
    kb = KB(nc)
    aff = Tl(kb, "aff", [NE, T])
    junk = Tl(kb, "junk", [NE, T])
    onesr = Tl(kb, "onesr", [NE, T])
    cs = Tl(kb, "cs", [NE, T])
    lo = Tl(kb, "lo", [NE, 1])
    hi = Tl(kb, "hi", [NE, 1])
    mid = Tl(kb, "mid", [NE, 1])
    cnt = Tl(kb, "cnt", [NE, 1])
    ge = Tl(kb, "ge", [NE, 1])
    d1 = Tl(kb, "d1", [NE, 1])
    d2 = Tl(kb, "d2", [NE, 1])

    def ew(eng, fname, reads, writes, **kw):
        kb.op(eng, lambda e: getattr(e, fname)(**kw), reads=rs(*reads), writes=rs(*writes))

    kb.dma(aff[:], affT, writes=rs(aff))
    ew("pool", "memset", [], [onesr], ap=onesr[:], constant=1.0)
    ew("pool", "memset", [], [lo], ap=lo[:], constant=0.0)
    ew("pool", "memset", [], [hi], ap=hi[:], constant=1.0)
    for it in range(30):
        ew("dve", "tensor_tensor", [lo, hi], [mid], out=mid[:], in0=lo[:], in1=hi[:], op=ALU.add)
        ew("dve", "tensor_scalar", [mid], [mid], out=mid[:], in0=mid[:], scalar1=0.5, scalar2=None, op0=ALU.mult)
        ew("dve", "tensor_scalar", [aff, mid], [junk, cnt], out=junk[:], in0=aff[:], scalar1=mid[:, 0:1], scalar2=None, op0=ALU.is_ge, op1=ALU.add, accum_out=cnt[:])
        ew("dve", "tensor_scalar", [cnt], [ge], out=ge[:], in0=cnt[:], scalar1=float(CAP) - 0.5, scalar2=None, op0=ALU.is_ge)
        ew("dve", "tensor_tensor", [mid, lo], [d1], out=d1[:], in0=mid[:], in1=lo[:], op=ALU.subtract)
        ew("dve", "tensor_tensor", [hi, mid], [d2], out=d2[:], in0=hi[:], in1=mid[:], op=ALU.subtract)
        ew("dve", "scalar_tensor_tensor", [d1, ge, lo], [lo], out=lo[:], in0=d1[:], scalar=ge[:, 0:1], in1=lo[:], op0=ALU.mult, op1=ALU.add)
        ew("dve", "scalar_tensor_tensor", [d2, ge, mid], [hi], out=hi[:], in0=d2[:], scalar=ge[:, 0:1], in1=mid[:], op0=ALU.mult, op1=ALU.add)
    ew("dve", "tensor_scalar", [aff, lo], [junk], out=junk[:], in0=aff[:], scalar1=lo[:, 0:1], scalar2=None, op0=ALU.is_ge)
    ew("dve", "tensor_tensor_scan", [onesr, junk], [cs], out=cs[:], data0=onesr[:], data1=junk[:], initial=0.0, op0=ALU.mult, op1=ALU.add)
    ew("dve", "tensor_tensor", [cs, junk], [cs], out=cs[:], in0=cs[:], in1=junk[:], op=ALU.mult)
    ew("dve", "tensor_scalar", [cs], [cs], out=cs[:], in0=cs[:], scalar1=-1.0, scalar2=None, op0=ALU.add)
    kb.dma(posD, cs[:], reads=rs(cs))
    ew("dve", "tensor_tensor", [aff, junk], [aff], out=aff[:], in0=aff[:], in1=junk[:], op=ALU.mult)
    kb.dma(gateD, aff[:], reads=rs(aff), eng="act")
    kb.emit()


def stage_experts(nc, posD, v_tm, w_gate, w_up, w_down, yD, iota_row, experts=range(NE)):
    kb = KB(nc)
    KC = D // 128
    NF = FF // 128
    NTI = T // 128
    ps = PS(kb)
    iot = Tl(kb, "iot", [128, 512])
    kb.dma(iot[:], iota_row, writes=rs(iot))
    ptm = Tl(kb, "ptm", [128, NTI])
    sel = [Tl(kb, f"sel{i}", [128, 512], BF16) for i in range(2)]
    vt = [Tl(kb, f"vt{i}", [128, D], BF16) for i in range(2)]
    xs = kb.sb("xs", [128, KC, 512], F32R)
    xs_r = [kb.res(f"xs{k}") for k in range(KC)]
    hT = kb.sb("hT", [128, NF, 512], F32R)
    hT_r = [kb.res(f"hT{k}") for k in range(NF)]
    wbuf = [Tl(kb, f"wb{i}", [128, KC * 256], F32R) for i in range(4)]
    sil = Tl(kb, "sil", [128, 512])
    yo = [Tl(kb, f"yo{i}", [128, D], F32R) for i in range(2)]
    vv = v_tm.rearrange("(p n) d -> p n d", n=NTI)
    wi = 0
    yi = 0
    si = 0
    for e_ in experts:
        kb.dma(ptm[:], posD[e_, :].rearrange("(p n) -> p n", n=NTI), writes=rs(ptm))
        for half in range(2):
            banks = [ps.next() for _ in range(8)]
            for n in range(NTI):
                b = si % 2
                si += 1
                kb.dma(vt[b][:, half * 1024:(half + 1) * 1024], vv[:, n, half * 1024:(half + 1) * 1024], writes=rs(vt[b]), eng=("sp" if n % 2 == 0 else "act"))
                kb.op("dve", lambda e, b=b, n=n: e.tensor_scalar(out=sel[b][:], in0=iot[:], scalar1=ptm[:, n:n + 1], scalar2=None, op0=ALU.is_equal),
                      reads=rs(iot, ptm), writes=rs(sel[b]))
                for ci in range(8):
                    c = half * 8 + ci
                    pt, pr = banks[ci]
                    kb.op("pe", lambda e, pt=pt, b=b, c=c, n=n: e.matmul(pt[:], lhsT=vt[b][:, c * 128:(c + 1) * 128], rhs=sel[b][:], start=(n == 0), stop=(n == NTI - 1)),
                          reads=rs(vt[b], sel[b]), writes=[pr], pe_accum=True)
            for ci in range(8):
                c = half * 8 + ci
                pt, pr = banks[ci]
                if ci % 2 == 0:
                    kb.op("act", lambda e, pt=pt, c=c: e.copy(out=xs[:, c, :], in_=pt[:]), reads=[pr], writes=[xs_r[c]])
                else:
                    kb.op("dve", lambda e, pt=pt, c=c: e.tensor_copy(out=xs[:, c, :], in_=pt[:]), reads=[pr], writes=[xs_r[c]])
        wgv = w_gate[e_].rearrange("(k p) f -> p k f", p=128)
        wuv = w_up[e_].rearrange("(k p) f -> p k f", p=128)
        for fp in range(NF // 2):
            bg = wbuf[(wi * 2) % 4]
            bu = wbuf[(wi * 2 + 1) % 4]
            wi += 1
            fsl = slice(fp * 256, (fp + 1) * 256)
            kb.dma(bg[:].rearrange("p (k f) -> p k f", f=256), wgv[:, :, fsl], writes=rs(bg), eng="sp")
            kb.dma(bu[:].rearrange("p (k f) -> p k f", f=256), wuv[:, :, fsl], writes=rs(bu), eng="act")
            bg3 = bg[:].rearrange("p (k f) -> p k f", f=256)
            bu3 = bu[:].rearrange("p (k f) -> p k f", f=256)
            for j in range(2):
                f = fp * 2 + j
                pg, pgr = ps.next()
                for k in range(KC):
                    kb.op("pe", lambda e, pg=pg, bg3=bg3, j=j, k=k: e.matmul(pg[:], lhsT=bg3[:, k, j * 128:(j + 1) * 128], rhs=xs[:, k, :], start=(k == 0), stop=(k == KC - 1)),
                          reads=[bg.r, xs_r[k]], writes=[pgr], pe_accum=True)
                pu, pur = ps.next()
                for k in range(KC):
                    kb.op("pe", lambda e, pu=pu, bu3=bu3, j=j, k=k: e.matmul(pu[:], lhsT=bu3[:, k, j * 128:(j + 1) * 128], rhs=xs[:, k, :], start=(k == 0), stop=(k == KC - 1)),
                          reads=[bu.r, xs_r[k]], writes=[pur], pe_accum=True)
                kb.op("act", lambda e, pg=pg: e.activation(out=sil[:], in_=pg[:], func=AF.Silu), reads=[pgr], writes=rs(sil))
                kb.op("dve", lambda e, pu=pu, f=f: e.tensor_tensor(out=hT[:, f, :], in0=pu[:], in1=sil[:], op=ALU.mult), reads=[pur] + rs(sil), writes=[hT_r[f]])
        wdv = w_down[e_].rearrange("(f p) d -> p f d", p=128)
        for qh in range(2):
            banks = [ps.next() for _ in range(8)]
            for f in range(NF):
                wb_ = wbuf[wi % 4]
                wi += 1
                kb.dma(wb_[:, 0:1024], wdv[:, f, qh * 1024:(qh + 1) * 1024], writes=rs(wb_), eng=("sp" if f % 2 == 0 else "act"))
                for j in range(4):
                    for qq in range(2):
                        pt, pr = banks[j * 2 + qq]
                        kb.op("pe", lambda e, pt=pt, wb_=wb_, f=f, j=j, qq=qq: e.matmul(pt[:], lhsT=hT[:, f, j * 128:(j + 1) * 128], rhs=wb_[:, qq * 512:(qq + 1) * 512], start=(f == 0), stop=(f == NF - 1)),
                              reads=[hT_r[f], wb_.r], writes=[pr], pe_accum=True)
            for j in range(4):
                o = yo[yi % 2]
                yi += 1
                for qq in range(2):
                    pt, pr = banks[j * 2 + qq]
                    if qq == 0:
                        kb.op("act", lambda e, pt=pt, o=o, qq=qq: e.copy(out=o[:, qq * 512:(qq + 1) * 512], in_=pt[:]), reads=[pr], writes=rs(o))
                    else:
                        kb.op("dve", lambda e, pt=pt, o=o, qq=qq: e.tensor_copy(out=o[:, qq * 512:(qq + 1) * 512], in_=pt[:]), reads=[pr], writes=rs(o))
                kb.dma(yD[e_, j * 128:(j + 1) * 128, qh * 1024:(qh + 1) * 1024], o[:, 0:1024], reads=rs(o), eng="sp")
    kb.emit()


def stage_combine(nc, posD, gateD, yD, xmT, xoutT, slot_col):
    kb = KB(nc)
    N = 512
    ps = PS(kb)
    scol = Tl(kb, "scol", [128, 4])
    kb.dma(scol[:], slot_col, writes=rs(scol))
    posb = [Tl(kb, f"posb{i}", [128, N]) for i in range(2)]
    gateb = [Tl(kb, f"gateb{i}", [128, N]) for i in range(2)]
    selg = [[Tl(kb, f"selg{i}_{j}", [128, N], F32R) for j in range(4)] for i in range(2)]
    ye = [Tl(kb, f"ye{i}", [128, 4, 1024], F32R) for i in range(2)]
    xi = [Tl(kb, f"xi{i}", [128, N]) for i in range(2)]
    xo = [Tl(kb, f"xo{i}", [128, N]) for i in range(2)]
    it = 0
    oi = 0
    for tb in range(T // N):
        tsl = slice(tb * N, (tb + 1) * N)
        for chalf in range(2):
            banks = [ps.next() for _ in range(8)]
            for e_ in range(NE):
                b = it % 2
                it += 1
                kb.dma(posb[b][:], posD[e_:e_ + 1, tsl].partition_broadcast(128), writes=rs(posb[b]), eng="sp")
                kb.dma(gateb[b][:], gateD[e_:e_ + 1, tsl].partition_broadcast(128), writes=rs(gateb[b]), eng="act")
                kb.dma(ye[b][:], yD[e_, :, chalf * 1024:(chalf + 1) * 1024].rearrange("(j p) d -> p j d", p=128), writes=rs(ye[b]), eng="sp")
                for j in range(4):
                    kb.op("dve", lambda e, b=b, j=j: e.scalar_tensor_tensor(out=selg[b][j][:], in0=posb[b][:], scalar=scol[:, j:j + 1], in1=gateb[b][:], op0=ALU.is_equal, op1=ALU.mult),
                          reads=rs(posb[b], gateb[b], scol), writes=rs(selg[b][j]))
                for ci in range(8):
                    pt, pr = banks[ci]
                    for j in range(4):
                        kb.op("pe", lambda e, pt=pt, b=b, j=j, ci=ci, e_=e_: e.matmul(pt[:], lhsT=ye[b][:, j, ci * 128:(ci + 1) * 128], rhs=selg[b][j][:], start=(e_ == 0 and j == 0), stop=(e_ == NE - 1 and j == 3)),
                              reads=rs(ye[b], selg[b][j]), writes=[pr], pe_accum=True)
            for ci in range(8):
                c = chalf * 8 + ci
                pt, pr = banks[ci]
                ob = oi % 2
                oi += 1
                kb.dma(xi[ob][:], xmT[c * 128:(c + 1) * 128, tsl], writes=rs(xi[ob]), eng="act")
                kb.op("dve", lambda e, pt=pt, ob=ob: e.tensor_tensor(out=xo[ob][:], in0=pt[:], in1=xi[ob][:], op=ALU.add), reads=[pr] + rs(xi[ob]), writes=rs(xo[ob]))
                kb.dma(xoutT[c * 128:(c + 1) * 128, tsl], xo[ob][:], reads=rs(xo[ob]), eng="pool")
    kb.emit()


def stage_final(nc, xT, g_ap, outT):
    kb = KB(nc)
    KC = D // 128
    N = 512
    ps = PS(kb)
    x = kb.sb("x", [128, KC, N])
    x_r = [kb.res(f"x{k}") for k in range(KC)]
    sq = [Tl(kb, f"sq{i}", [128, N], F32R) for i in range(2)]
    rstd = Tl(kb, "rstd", [128, N])
    gt = Tl(kb, "gt", [128, KC])
    onesf = Tl(kb, "onesf", [128, 128])
    ones = Tl(kb, "ones", [128, 128], F32R)
    o = [Tl(kb, f"o{i}", [128, N]) for i in range(2)]
    kb.op("pool", lambda e: e.memset(onesf[:], 1.0), writes=rs(onesf))
    kb.op("dve", lambda e: e.tensor_copy(out=ones[:], in_=onesf[:]), reads=rs(onesf), writes=rs(ones))
    kb.dma(gt[:], g_ap.rearrange("(k p) -> p k", p=128), writes=rs(gt), allow_slow_non_contiguous=True)
    xv = xT.rearrange("(k p) n -> p k n", p=128)
    ov = outT.rearrange("(k p) n -> p k n", p=128)
    oi = 0
    for s in range(T // N):
        tsl = slice(s * N, (s + 1) * N)
        pt, pr = ps.next()
        for k in range(KC):
            kb.dma(x[:, k, :], xv[:, k, tsl], writes=[x_r[k]], eng=("sp" if k % 2 == 0 else "act"))
            a = k % 2
            kb.op("act", lambda e, a=a, k=k: e.activation(out=sq[a][:], in_=x[:, k, :], func=AF.Square), reads=[x_r[k]], writes=rs(sq[a]))
            kb.op("pe", lambda e, pt=pt, a=a, k=k: e.matmul(pt[:], lhsT=ones[:], rhs=sq[a][:], start=(k == 0), stop=(k == KC - 1)),
                  reads=rs(ones, sq[a]), writes=[pr], pe_accum=True)
        kb.op("act", lambda e, pt=pt: e.activation(out=rstd[:], in_=pt[:], func=AF.Sqrt, scale=1.0 / D, bias=EPS), reads=[pr], writes=rs(rstd))
        kb.op("dve", lambda e: e.reciprocal(out=rstd[:], in_=rstd[:]), writes=rs(rstd))
        for k in range(KC):
            ob = oi % 2
            oi += 1
            kb.op("dve", lambda e, k=k, ob=ob: e.scalar_tensor_tensor(out=o[ob][:], in0=x[:, k, :], scalar=gt[:, k:k + 1], in1=rstd[:], op0=ALU.mult, op1=ALU.mult),
                  reads=[x_r[k]] + rs(gt, rstd), writes=rs(o[ob]))
            kb.dma(ov[:, k, tsl], o[ob][:], reads=rs(o[ob]), eng="pool", is_output=True)
    kb.emit()


def make_consts():
    p = np.arange(128)[:, None] % 64
    t = np.arange(512)[None, :] % 64
    c = {}
    c["m_up"] = (p < t).astype(np.float32)
    c["m_lo"] = (p > t).astype(np.float32)
    c["m_upi"] = (p <= t).astype(np.float32)
    c["m_loi"] = (p >= t).astype(np.float32)
    c["itile"] = (p == t).astype(np.float32)
    c["ident"] = (p == np.arange(64)[None, :]).astype(np.float32)
    c["bones"] = ((np.arange(128)[:, None] // 64) == (np.arange(128)[None, :] // 64)).astype(np.float32)
    cm = np.ones((128, 1024), np.float32)
    cm[:, ::64] = 0.0
    c["cmask"] = cm
    return c


from concourse.bass_utils import run_bass_kernel_spmd

N_CORES = 2
DEPTH = 2


def make_consts_all():
    c = make_consts()
    t = np.arange(T)
    ci = np.zeros((4, T), np.float32)
    for g, w in enumerate((2, 4, 8, 16)):
        h = w // 2
        lo = np.clip(t - h, 0, T)
        hi = np.clip(t + h, 0, T)
        ci[g] = 1.0 / (hi - lo).astype(np.float32)
    c["cnt_inv"] = ci
    c["iota_row"] = np.tile(np.arange(512, dtype=np.float32)[None, :], (128, 1))
    c["slot_col"] = (np.arange(4)[None, :] * 128 + np.arange(128)[:, None]).astype(np.float32)
    c["identf"] = np.eye(128, dtype=np.float32)
    return c


_WSPEC = {
    "norm_mix_g": ([DEPTH, D], F32), "w_in": ([DEPTH, D, D_IN], F32R), "mu_shift": ([DEPTH, 3488], F32),
    "w0": ([DEPTH, 2, 1024], F32), "w_lora_up": ([DEPTH, 2, 64, 1024], F32R), "a0": ([DEPTH, 2, 1024], F32),
    "a_lora_up": ([DEPTH, 2, 64, 1024], F32R), "g_lora_up": ([DEPTH, 160, 1024], F32R), "k_k": ([DEPTH, 1024], F32),
    "k_a": ([DEPTH, 1024], F32), "r_k": ([DEPTH, 1024], F32), "lnx_g": ([DEPTH, 1024], F32), "lnx_b": ([DEPTH, 1024], F32),
    "pool_w": ([DEPTH, 4, 256, 256], F32R), "pool_scale": ([DEPTH, 1024], F32), "w_up_a": ([DEPTH, 1024, D], F32R),
    "w_up_b": ([DEPTH, 1024, D], F32R), "w_o": ([DEPTH, D, D], F32R), "norm_moe_g": ([DEPTH, D], F32),
    "w_router": ([DEPTH, D, NE], F32), "w_gate_e": ([DEPTH, NE, D, FF], F32R), "w_up_e": ([DEPTH, NE, D, FF], F32R),
    "w_down_e": ([DEPTH, NE, FF, D], F32R), "final_g": ([D], F32),
}


def build_program(C):
    nc = bass.Bass("TRN2", target_bir_lowering=False)
    nc.dge_precook = False

    def din(name, shape, dt=F32):
        return nc.dram_tensor(name, list(shape), dt, kind="ExternalInput").ap()

    def dint(name, shape, dt=F32):
        return nc.dram_tensor(name, list(shape), dt, kind="Internal").ap()

    xT = din("xT", [D, T])
    Wt = {k: din(k, shp, dt) for k, (shp, dt) in _WSPEC.items()}
    cst = {k: din("c_" + k, v.shape) for k, v in C.items()}
    outT = nc.dram_tensor("outT", [D, T], F32, kind="ExternalOutput").ap()
    projT = dint("projT", [D_IN, T])
    loraA = dint("loraA", [416, T], F32R)
    yfT = dint("yfT", [1024, T])
    yaT = dint("yaT", [1024, T], F32R)
    ybT = dint("ybT", [1024, T], F32R)
    xmT = dint("xmT", [D, T])
    v_tm = dint("v_tm", [T, D], BF16)
    affT = dint("affT", [NE, T])
    posD = dint("posD", [NE, T])
    gateD = dint("gateD", [NE, T])
    yD = dint("yD", [NE, CAP, D], F32R)
    xs = [xT, dint("xA", [D, T]), dint("xB", [D, T])]
    for l in range(DEPTH):
        xin = xs[l]
        xout = xs[l + 1]
        stage_inproj(nc, xin, Wt["norm_mix_g"][l], Wt["w_in"][l], projT)
        stage_lora(nc, projT, Wt["mu_shift"][l], loraA)
        W = {"mu": Wt["mu_shift"][l], "w0": Wt["w0"][l], "wl": Wt["w_lora_up"][l], "a0": Wt["a0"][l], "al": Wt["a_lora_up"][l],
             "gl": Wt["g_lora_up"][l], "k_k": Wt["k_k"][l], "k_a": Wt["k_a"][l], "r_k": Wt["r_k"][l], "lnx_g": Wt["lnx_g"][l],
             "lnx_b": Wt["lnx_b"][l]}
        for hp in range(8):
            stage_rwkv(nc, projT, loraA, yfT, yaT, W, cst, hp_list=[hp])
        stage_pool(nc, projT, Wt["pool_w"][l], Wt["pool_scale"][l], ybT, cst["cnt_inv"])
        stage_mix(nc, xin, projT, yaT, ybT, Wt["w_up_a"][l], Wt["w_up_b"][l], Wt["w_o"][l], Wt["norm_moe_g"][l], Wt["w_router"][l],
                  xmT, v_tm, affT, cst["identf"])
        stage_route(nc, affT, posD, gateD)
        stage_experts(nc, posD, v_tm, Wt["w_gate_e"][l], Wt["w_up_e"][l], Wt["w_down_e"][l], yD, cst["iota_row"])
        stage_combine(nc, posD, gateD, yD, xmT, xout, cst["slot_col"])
    stage_final(nc, xs[DEPTH], Wt["final_g"], outT)
    return nc


def kernel(**inputs):
    C = make_consts_all()
    nc = build_program(C)
    x = np.asarray(inputs["x"], dtype=np.float32)
    in_maps = []
    for b in range(N_CORES):
        m = {"xT": np.ascontiguousarray(x[b].T)}
        for k in _WSPEC:
            a = np.asarray(inputs[k], dtype=np.float32)
            if k == "r_k":
                a = a.reshape(DEPTH, 1024)
            m[k] = np.ascontiguousarray(a)
        for k, v in C.items():
            m["c_" + k] = v
        in_maps.append(m)
    res = run_bass_kernel_spmd(nc, in_maps, core_ids=list(range(N_CORES)))
    out = np.stack([np.ascontiguousarray(np.asarray(res.results[b]["outT"]).T) for b in range(N_CORES)], axis=0)
    return out.astype(np.float32)
```

```python
from contextlib import ExitStack
import concourse.bass as bass
import concourse.mybir as mybir

F32 = mybir.dt.float32
F32R = mybir.dt.float32r
BF16 = mybir.dt.bfloat16
I32 = mybir.dt.int32
U32 = mybir.dt.uint32
AF = mybir.ActivationFunctionType
ALU = mybir.AluOpType
AX = mybir.AxisListType

ENGS = ("pe", "dve", "act", "pool", "sp")
N_DMA_SEMS = 48


class Res:
    __slots__ = ("name", "w", "r")

    def __init__(self, name):
        self.name = name
        self.w = None
        self.r = []


class Instr:
    __slots__ = ("eng", "fn", "deps", "stream", "seq", "waited", "val", "is_dma", "known")

    def __init__(self, eng, fn):
        self.eng = eng
        self.fn = fn
        self.deps = []
        self.stream = None
        self.seq = 0
        self.waited = False
        self.val = 0
        self.is_dma = False
        self.known = None


class KB:
    _count = 0

    def __init__(self, nc, same_engine_sync=True):
        KB._count += 1
        self.tag = f"k{KB._count}_"
        self.nc = nc
        self.es = ExitStack()
        self.q = {e: [] for e in ENGS}
        self.stream_last = {}
        self.stream_cnt = {}
        self.dma_rr = 0
        self.same_engine_sync = same_engine_sync
        self.n_res = 0
        self.out_dmas = []

    def sb(self, name, shape, dtype=F32):
        return self.es.enter_context(self.nc.sbuf_tensor(self.tag + name, list(shape), dtype))

    def ps(self, name, shape, dtype=F32):
        return self.es.enter_context(self.nc.psum_tensor(self.tag + name, list(shape), dtype))

    def res(self, name=None):
        self.n_res += 1
        return Res(name or f"r{self.n_res}")

    def _known_after(self, d):
        k = dict(d.known)
        if k.get(d.stream, 0) < d.seq:
            k[d.stream] = d.seq
        return k

    def _add(self, ins, reads, writes, pe_accum=False):
        eng = ins.eng
        deps = []
        for r in reads:
            if r.w is not None:
                deps.append(r.w)
        for w in writes:
            if w.w is not None:
                if not (pe_accum and w.w.eng == "pe" and not w.w.is_dma):
                    deps.append(w.w)
            deps.extend(w.r)
        prev = self.q[eng][-1] if self.q[eng] else None
        known = dict(prev.known) if prev is not None else {}
        if ins.is_dma:
            p = self.stream_last.get(ins.stream)
            if p is not None:
                deps.append(p)
        final = []
        deps = sorted(set(deps), key=lambda d: -d.seq)
        for d in deps:
            if d is ins:
                continue
            if (not d.is_dma) and d.eng == eng and not self.same_engine_sync:
                continue
            if known.get(d.stream, 0) >= d.seq:
                continue
            final.append(d)
            d.waited = True
            ka = self._known_after(d)
            for s, v in ka.items():
                if known.get(s, 0) < v:
                    known[s] = v
        ins.deps = final
        ins.known = known
        self.q[eng].append(ins)
        for r in reads:
            r.r.append(ins)
        for w in writes:
            w.w = ins
            w.r = []
        return ins

    def op(self, eng, fn, reads=(), writes=(), pe_accum=False):
        if eng == "pool":
            eng = "dve"
        ins = Instr(eng, fn)
        ins.stream = eng
        self.stream_cnt[eng] = self.stream_cnt.get(eng, 0) + 1
        ins.seq = self.stream_cnt[eng]
        self._add(ins, list(reads), list(writes), pe_accum=pe_accum)
        self.stream_last[eng] = ins
        return ins

    def dma(self, out, in_, reads=(), writes=(), eng="sp", is_output=False, **kw):
        if eng == "pool":
            eng = "sp"
        fn = lambda e: e.dma_start(out=out, in_=in_, **kw)
        ins = Instr(eng, fn)
        ins.is_dma = True
        slot = self.dma_rr % N_DMA_SEMS
        self.dma_rr += 1
        ins.stream = ("dma", slot)
        self.stream_cnt[ins.stream] = self.stream_cnt.get(ins.stream, 0) + 1
        ins.seq = self.stream_cnt[ins.stream]
        ins.waited = True
        self._add(ins, list(reads), list(writes))
        self.stream_last[ins.stream] = ins
        if is_output:
            self.out_dmas.append(ins)
        return ins

    def gen(self, eng, fn, reads=(), writes=()):
        return self.op(eng, fn, reads, writes)

    def emit(self):
        nc = self.nc
        lastd = [v for k, v in self.stream_last.items() if isinstance(k, tuple)]
        if lastd:
            self.out_dmas = lastd
            fin = Instr("sp", None)
            fin.stream = "sp"
            self.stream_cnt["sp"] = self.stream_cnt.get("sp", 0) + 1
            fin.seq = self.stream_cnt["sp"]
            prev = self.q["sp"][-1] if self.q["sp"] else None
            known = dict(prev.known) if prev is not None else {}
            for d in self.out_dmas:
                if known.get(d.stream, 0) < d.seq:
                    fin.deps.append(d)
            fin.known = known
            self.q["sp"].append(fin)
        sems = {}
        for e in ENGS:
            sems[e] = self.es.enter_context(nc.semaphore(self.tag + f"s_{e}"))
            v = 0
            for ins in self.q[e]:
                if ins.is_dma:
                    continue
                if ins.waited:
                    v += 1
                    ins.val = v
        for s in range(N_DMA_SEMS):
            sems[("dma", s)] = self.es.enter_context(nc.semaphore(self.tag + f"s_dma{s}"))
        engmap = {"pe": "tensor", "dve": "vector", "act": "scalar", "pool": "gpsimd", "sp": "sync"}
        q = self.q

        def run(ename, e):
            for ins in q[ename]:
                for d in ins.deps:
                    if d.is_dma:
                        e.wait_ge(sems[d.stream], 16 * d.seq)
                    else:
                        e.wait_ge(sems[d.stream], d.val)
                if ins.fn is None:
                    continue
                r = ins.fn(e)
                if ins.is_dma:
                    r.then_inc(sems[ins.stream], 16)
                elif ins.waited:
                    r.then_inc(sems[ins.stream], 1)

        allsems = list(sems.values())
        with nc.Block() as cblock:
            @cblock.gpsimd
            def _(e):
                for sm in allsems:
                    e.sem_clear(sm)

        with nc.Block() as block:
            @block.tensor
            def _(e):
                run("pe", e)

            @block.vector
            def _(e):
                run("dve", e)

            @block.scalar
            def _(e):
                run("act", e)

            @block.gpsimd
            def _(e):
                run("pool", e)

            @block.sync
            def _(e):
                run("sp", e)
        self.es.close()

    def stats(self):
        return {e: len(self.q[e]) for e in ENGS}


import numpy as np
import concourse.bass as bass
import concourse.mybir as mybir

D = 2048
T = 4096
D_IN = 8608
D_SHIFT = 3488
EPS = 1e-6
NE = 16
FF = 2816
CAP = 512


class Tl:
    def __init__(self, kb, name, shape, dtype=F32):
        self.t = kb.sb(name, shape, dtype)
        self.r = kb.res(name)

    def __getitem__(self, k):
        return self.t[k]


class PS:
    def __init__(self, kb, n=8):
        self.kb = kb
        self.t = [kb.ps(f"ps{i}", [128, 512]) for i in range(n)]
        self.r = [kb.res(f"ps{i}") for i in range(n)]
        self.i = 0
        self.n = n

    def next(self):
        i = self.i % self.n
        self.i += 1
        return self.t[i], self.r[i]


def rs(*tiles):
    return [x.r if hasattr(x, "r") and not isinstance(x, Res) else x for x in tiles]


def col_chunks():
    ch = [(i * 128, 128) for i in range(27)] + [(3456, 32)] + [(3488 + i * 128, 128) for i in range(40)]
    return ch


def stage_inproj(nc, xsrc, g_ap, w_ap, projT):
    kb = KB(nc)
    KC = D // 128
    NT = 1024
    ut = kb.sb("ut", [128, KC, NT], F32R)
    ut_r = [kb.res(f"ut{k}") for k in range(KC)]
    xa = [Tl(kb, f"xa{i}", [128, NT]) for i in range(2)]
    gt = Tl(kb, "gt", [128, KC])
    onesf = Tl(kb, "onesf", [128, 128])
    ones = Tl(kb, "ones", [128, 128], F32R)
    sq = [Tl(kb, f"sq{i}", [128, NT], F32R) for i in range(2)]
    rstd = Tl(kb, "rstd", [128, NT])
    wt = [Tl(kb, f"wt{i}", [128, KC, 512], F32R) for i in range(2)]
    ot = [Tl(kb, f"ot{i}", [128, NT]) for i in range(2)]
    ps = PS(kb)
    kb.op("pool", lambda e: e.memset(onesf[:], 1.0), writes=rs(onesf))
    kb.op("dve", lambda e: e.tensor_copy(out=ones[:], in_=onesf[:]), reads=rs(onesf), writes=rs(ones))
    kb.dma(gt[:], g_ap.rearrange("(k p) -> p k", p=128), writes=rs(gt), eng="sp", allow_slow_non_contiguous=True)
    xv = xsrc.rearrange("(k p) n -> p k n", p=128)
    wv = w_ap.rearrange("(k p) n -> p k n", p=128)
    chunks = col_chunks()
    groups = [chunks[i:i + 4] for i in range(0, len(chunks), 4)]
    gi_glob = 0
    ci = 0
    for s in range(T // NT):
        tsl = slice(s * NT, (s + 1) * NT)
        p0t, p0r = ps.next()
        p1t, p1r = ps.next()
        pss = [(p0t, p0r), (p1t, p1r)]
        for k in range(KC):
            a = k % 2
            kb.dma(xa[a][:], xv[:, k, tsl], writes=rs(xa[a]), eng="sp")
            kb.op("act", lambda e, a=a: e.activation(out=sq[a][:], in_=xa[a][:], func=AF.Square),
                  reads=rs(xa[a]), writes=rs(sq[a]))
            for h in range(2):
                kb.op("pe", lambda e, k=k, a=a, h=h, pt=pss[h][0]: e.matmul(pt[:], lhsT=ones[:], rhs=sq[a][:, h * 512:(h + 1) * 512],
                                                                        start=(k == 0), stop=(k == KC - 1)),
                      reads=rs(ones, sq[a]), writes=[pss[h][1]], pe_accum=True)
        for h in range(2):
            sl = slice(h * 512, (h + 1) * 512)
            kb.op("act", lambda e, h=h, sl=sl, pt=pss[h][0]: e.activation(out=rstd[:, sl], in_=pt[:], func=AF.Sqrt, scale=1.0 / D, bias=EPS),
                  reads=[pss[h][1]], writes=rs(rstd))
        kb.op("dve", lambda e: e.reciprocal(out=rstd[:], in_=rstd[:]), writes=rs(rstd))
        for k in range(KC):
            a = k % 2
            kb.dma(xa[a][:], xv[:, k, tsl], writes=rs(xa[a]), eng="sp")
            kb.op("dve", lambda e, k=k, a=a: e.scalar_tensor_tensor(out=ut[:, k, :], in0=xa[a][:], scalar=gt[:, k:k + 1], in1=rstd[:],
                                                                op0=ALU.mult, op1=ALU.mult),
                  reads=rs(gt, rstd, xa[a]), writes=[ut_r[k]])
        for grp in groups:
            b = gi_glob % 2
            c0 = grp[0][0]
            c1 = grp[-1][0] + grp[-1][1]
            kb.dma(wt[b][:, :, 0:c1 - c0], wv[:, :, c0:c1], writes=rs(wt[b]), eng=("sp" if gi_glob % 2 == 0 else "act"))
            gi_glob += 1
            for (cs, cw) in grp:
                ob = ci % 2
                for h in range(2):
                    pt, pr = ps.next()
                    for k in range(KC):
                        if cw == 128:
                            lhsT = wt[b][:, k, cs - c0:cs - c0 + cw]
                            rhs = ut[:, k, h * 512:(h + 1) * 512]
                        else:
                            lhsT = wt[b][:, k, cs - c0:cs - c0 + cw].bitcast(F32)
                            rhs = ut[:, k, h * 512:(h + 1) * 512].bitcast(F32)
                        kb.op("pe", lambda e, pt=pt, lhsT=lhsT, rhs=rhs, k=k, cw=cw: e.matmul(pt[0:cw, :], lhsT=lhsT, rhs=rhs,
                                                                                       start=(k == 0), stop=(k == KC - 1)),
                              reads=[wt[b].r, ut_r[k]], writes=[pr], pe_accum=True)
                    sl = slice(h * 512, (h + 1) * 512)
                    if cs >= 4512:
                        kb.op("act", lambda e, ob=ob, pt=pt, sl=sl, cw=cw: e.activation(out=ot[ob][0:cw, sl], in_=pt[0:cw, :], func=AF.Sigmoid),
                              reads=[pr], writes=rs(ot[ob]))
                    elif h == 0:
                        kb.op("act", lambda e, ob=ob, pt=pt, sl=sl, cw=cw: e.copy(out=ot[ob][0:cw, sl], in_=pt[0:cw, :]),
                              reads=[pr], writes=rs(ot[ob]))
                    else:
                        kb.op("dve", lambda e, ob=ob, pt=pt, sl=sl, cw=cw: e.tensor_copy(out=ot[ob][0:cw, sl], in_=pt[0:cw, :]),
                              reads=[pr], writes=rs(ot[ob]))
                kb.dma(projT[cs:cs + cw, tsl], ot[ob][0:cw, :], reads=rs(ot[ob]), eng="pool", is_output=True)
                ci += 1
    kb.emit()


def load_col(kb, tile, ap1d, eng="sp"):
    n = ap1d.shape[0]
    kb.dma(tile[0:n, 0:1], ap1d.rearrange("(p o) -> p o", o=1), writes=rs(tile), eng=eng)


def load_halo(kb, P, src_rows, t0, S, eng="sp"):
    n = src_rows.shape[0]
    lo = max(t0 - 1, 0)
    hi = min(t0 + S + 1, T)
    kb.dma(P[0:n, lo - (t0 - 1):hi - (t0 - 1)], src_rows[:, lo:hi], writes=rs(P), eng=eng)
    if t0 == 0:
        kb.op("pool", lambda e: e.memset(P[0:n, 0:1], 0.0), writes=rs(P))
    if t0 + S == T:
        kb.op("pool", lambda e: e.memset(P[0:n, S + 1:S + 2], 0.0), writes=rs(P))


def shift_rows(kb, out_ap, P, tmp, hmu, omu, n, S, out_res):
    kb.op("pool", lambda e: e.tensor_tensor(out=tmp[0:n, :], in0=P[0:n, 0:S], in1=P[0:n, 2:S + 2], op=ALU.add),
          reads=rs(P), writes=rs(tmp))
    kb.op("pool", lambda e: e.tensor_scalar(out=tmp[0:n, :], in0=tmp[0:n, :], scalar1=hmu[0:n, 0:1], scalar2=None, op0=ALU.mult),
          reads=rs(hmu), writes=rs(tmp))
    kb.op("dve", lambda e: e.scalar_tensor_tensor(out=out_ap, in0=P[0:n, 1:S + 1], scalar=omu[0:n, 0:1], in1=tmp[0:n, :],
                                                  op0=ALU.mult, op1=ALU.add),
          reads=rs(P, omu, tmp), writes=[out_res])


def mu_cols(kb, mu, hmu, omu, ap1d, n):
    load_col(kb, mu, ap1d)
    kb.op("dve", lambda e: e.tensor_scalar(out=hmu[0:n, :], in0=mu[0:n, :], scalar1=0.5, scalar2=None, op0=ALU.mult),
          reads=rs(mu), writes=rs(hmu))
    kb.op("dve", lambda e: e.tensor_scalar(out=omu[0:n, :], in0=mu[0:n, :], scalar1=-1.0, scalar2=1.0, op0=ALU.mult, op1=ALU.add),
          reads=rs(mu), writes=rs(omu))


def stage_lora(nc, projT, mu_ap, loraA):
    kb = KB(nc)
    S = 1024
    P = [Tl(kb, f"P{i}", [128, S + 2]) for i in range(2)]
    tmp = Tl(kb, "tmp", [128, S])
    sh = Tl(kb, "sh", [128, S])
    o = [Tl(kb, f"o{i}", [128, S], F32R) for i in range(2)]
    mu = [Tl(kb, f"mu{i}", [128, 1]) for i in range(4)]
    hmu = [Tl(kb, f"hmu{i}", [128, 1]) for i in range(4)]
    omu = [Tl(kb, f"omu{i}", [128, 1]) for i in range(4)]
    blocks = [(0, 128, AF.Tanh), (128, 128, AF.Copy), (256, 128, AF.Sigmoid), (384, 32, AF.Sigmoid)]
    for bi, (r0, n, fn) in enumerate(blocks):
        mu_cols(kb, mu[bi], hmu[bi], omu[bi], mu_ap[3072 + r0:3072 + r0 + n], n)
    it = 0
    for s in range(T // S):
        t0 = s * S
        for bi, (r0, n, fn) in enumerate(blocks):
            p = P[it % 2]
            oo = o[it % 2]
            it += 1
            load_halo(kb, p, projT[3072 + r0:3072 + r0 + n, :], t0, S)
            shift_rows(kb, sh[0:n, :], p, tmp, hmu[bi], omu[bi], n, S, sh.r)
            if fn == AF.Copy:
                kb.op("act", lambda e, oo=oo, n=n: e.copy(out=oo[0:n, :], in_=sh[0:n, :]), reads=rs(sh), writes=rs(oo))
            else:
                kb.op("act", lambda e, oo=oo, n=n, fn=fn: e.activation(out=oo[0:n, :], in_=sh[0:n, :], func=fn), reads=rs(sh), writes=rs(oo))
            kb.dma(loraA[r0:r0 + n, t0:t0 + S], oo[0:n, :], reads=rs(oo), eng="act")
    kb.emit()


CEXP = float(np.exp(-0.5))


def stage_rwkv(nc, projT, loraA, yfT, yaT, W, cst, hp_list=range(8), dbg=None):
    kb = KB(nc)
    S = 1024
    NCH = S // 64
    ps = PS(kb)
    cf = {k: Tl(kb, "c_" + k, [128, 512], BF16) for k in ("m_up", "m_lo", "m_upi", "m_loi", "itile")}
    cstage = Tl(kb, "cstage", [128, 512])
    for k in cf:
        kb.dma(cstage[:], cst[k], writes=rs(cstage))
        kb.op("dve", lambda e, k=k: e.tensor_copy(out=cf[k][:], in_=cstage[:]), reads=rs(cstage), writes=rs(cf[k]))
    identf = Tl(kb, "identf", [128, 64])
    ident = Tl(kb, "ident", [128, 64], BF16)
    kb.dma(identf[:], cst["ident"], writes=rs(identf))
    kb.op("dve", lambda e: e.tensor_copy(out=ident[:], in_=identf[:]), reads=rs(identf), writes=rs(ident))
    bonesf = Tl(kb, "bonesf", [128, 128])
    bones = Tl(kb, "bones", [128, 128], F32R)
    kb.dma(bonesf[:], cst["bones"], writes=rs(bonesf))
    kb.op("dve", lambda e: e.tensor_copy(out=bones[:], in_=bonesf[:]), reads=rs(bonesf), writes=rs(bones))
    cmask = Tl(kb, "cmask", [128, S])
    kb.dma(cmask[:], cst["cmask"], writes=rs(cmask))
    colnames = ["mu_r", "mu_k", "mu_v", "w0f", "w0b", "a0f", "a0b", "k_k", "k_a", "r_k", "lnx_g", "lnx_b"]
    col = {n: Tl(kb, "col_" + n, [128, 1]) for n in colnames}
    hmu = {n: Tl(kb, "h" + n, [128, 1]) for n in ("mu_r", "mu_k", "mu_v")}
    omu = {n: Tl(kb, "o" + n, [128, 1]) for n in ("mu_r", "mu_k", "mu_v")}
    omka = Tl(kb, "omka", [128, 1])
    hka = Tl(kb, "hka", [128, 1])
    zf = Tl(kb, "zf", [128, 128])
    kb.op("dve", lambda e: e.memset(zf[:], 0.0), writes=rs(zf))
    wlp = [Tl(kb, f"wlp{i}", [128, 128], F32R) for i in range(2)]
    alp = [Tl(kb, f"alp{i}", [128, 128], F32R) for i in range(2)]
    gl0 = Tl(kb, "gl0", [128, 128], F32R)
    gl1 = Tl(kb, "gl1", [128, 128], F32R)
    for i in range(2):
        oh = slice(64, 128) if i == 0 else slice(0, 64)
        kb.op("dve", lambda e, i=i, oh=oh: e.tensor_copy(out=wlp[i][oh, :], in_=zf[oh, :]), reads=rs(zf), writes=rs(wlp[i]))
        kb.op("dve", lambda e, i=i, oh=oh: e.tensor_copy(out=alp[i][oh, :], in_=zf[oh, :]), reads=rs(zf), writes=rs(alp[i]))
    for (pa_, pb_) in ((32, 64), (64, 128)):
        kb.op("dve", lambda e, pa_=pa_, pb_=pb_: e.tensor_copy(out=gl1[pa_:pb_, :], in_=zf[pa_:pb_, :]), reads=rs(zf), writes=rs(gl1))
    P2 = [Tl(kb, n, [128, S + 2]) for n in ("P2a", "P2b")]
    Pr, Pk, Pv = P2[0], P2[1], P2[0]
    names32 = ["r_", "k_", "v_", "tA", "tB", "sgw", "a_", "kk", "kkn", "b_", "Pp", "E_", "R_", "Sf", "yc", "po1", "po2", "po3"]
    F = {n: Tl(kb, n, [128, S]) for n in names32}
    F["lw"] = F["sgw"]
    F["kdir"] = F["kk"]
    F["yf"] = F["Sf"]
    F["af"] = F["E_"]
    sqr = Tl(kb, "sqr", [128, S], F32R)
    tw = Tl(kb, "tw", [128, S], F32R)
    adp = Tl(kb, "adp", [128, S], F32R)
    sg0 = Tl(kb, "sg0", [128, S], F32R)
    sg1 = Tl(kb, "sg1", [128, S], F32R)
    for q in range(S // 128):
        for (pa_, pb_) in ((32, 64), (64, 128)):
            kb.op("dve", lambda e, q=q, pa_=pa_, pb_=pb_: e.tensor_copy(out=sg1[pa_:pb_, q * 128:(q + 1) * 128], in_=zf[pa_:pb_, :]), reads=rs(zf), writes=rs(sg1))
    yout = sqr
    tot = Tl(kb, "tot", [128, NCH])
    wC = Tl(kb, "wC", [128, NCH])
    namesb = ["qt", "rt", "kt", "nbt", "Kh", "Bn", "vb"]
    Bf = {n: Tl(kb, n, [128, S], BF16) for n in namesb}
    pb_names = ["Vtm", "Qtm", "Khtm", "Bhtm", "Um", "Utm", "Pa", "Pb", "Pta", "Ptb", "Xa", "Xb", "AkkT", "ArkT", "nArbT", "AV", "Gtm", "Qh", "PhiT", "RhT"]
    PB = [{n: Tl(kb, f"{n}{bi}", [128, 512], BF16) for n in pb_names} for bi in range(2)]
    Psi = [Tl(kb, f"Psi{bi}", [128, 512]) for bi in range(2)]
    Hbf = Tl(kb, "Hbf", [128, 64], BF16)

    def ew(eng, fname, reads, writes, **kw):
        kb.op(eng, lambda e: getattr(e, fname)(**kw), reads=rs(*reads), writes=rs(*writes))

    def psbf(pt):
        return pt[:].bitcast(BF16)

    HS = [slice(0, 64), slice(64, 128)]

    for hp in hp_list:
        c0 = hp * 128
        csl = slice(c0, c0 + 128)
        load_col(kb, col["mu_r"], W["mu"][c0:c0 + 128])
        load_col(kb, col["mu_k"], W["mu"][1024 + c0:1024 + c0 + 128])
        load_col(kb, col["mu_v"], W["mu"][2048 + c0:2048 + c0 + 128])
        load_col(kb, col["w0f"], W["w0"][0, csl])
        load_col(kb, col["w0b"], W["w0"][1, csl])
        load_col(kb, col["a0f"], W["a0"][0, csl])
        load_col(kb, col["a0b"], W["a0"][1, csl])
        for n in ("k_k", "k_a", "r_k", "lnx_g", "lnx_b"):
            load_col(kb, col[n], W[n][csl])
        for n in ("mu_r", "mu_k", "mu_v"):
            ew("dve", "tensor_scalar", [col[n]], [hmu[n]], out=hmu[n][:], in0=col[n][:], scalar1=0.5, scalar2=None, op0=ALU.mult)
            ew("dve", "tensor_scalar", [col[n]], [omu[n]], out=omu[n][:], in0=col[n][:], scalar1=-1.0, scalar2=1.0, op0=ALU.mult, op1=ALU.add)
        ew("dve", "tensor_scalar", [col["k_a"]], [omka], out=omka[:], in0=col["k_a"][:], scalar1=-1.0, scalar2=1.0, op0=ALU.mult, op1=ALU.add)
        ew("dve", "tensor_scalar", [col["k_a"]], [hka], out=hka[:], in0=col["k_a"][:], scalar1=0.5, scalar2=None, op0=ALU.mult)
        for i in range(2):
            kb.dma(wlp[i][HS[i], :], W["wl"][i, :, csl], writes=rs(wlp[i]))
            kb.dma(alp[i][HS[i], :], W["al"][i, :, csl], writes=rs(alp[i]))
        kb.dma(gl0[:], W["gl"][0:128, csl], writes=rs(gl0))
        kb.dma(gl1[0:32, :], W["gl"][128:160, csl], writes=rs(gl1))
        for d in range(2):
            ds = HS[d]
            ew("pool", "memset", [], [Hbf], ap=Hbf[:], constant=0.0)
            slabs = list(range(T // S))
            if d == 1:
                slabs = slabs[::-1]
            for s in slabs:
                t0 = s * S
                tsl = slice(t0, t0 + S)
                load_halo(kb, Pr, projT[c0:c0 + 128, :], t0, S, eng="sp")
                load_halo(kb, Pk, projT[1024 + c0:1024 + c0 + 128, :], t0, S, eng="act")
                kb.dma(tw[:], loraA[0:128, tsl], writes=rs(tw), eng="act")
                kb.dma(adp[:], loraA[128:256, tsl], writes=rs(adp), eng="sp")
                shift_rows(kb, F["r_"][:], Pr, F["tA"], hmu["mu_r"], omu["mu_r"], 128, S, F["r_"].r)
                load_halo(kb, Pv, projT[2048 + c0:2048 + c0 + 128, :], t0, S, eng="sp")
                shift_rows(kb, F["k_"][:], Pk, F["tA"], hmu["mu_k"], omu["mu_k"], 128, S, F["k_"].r)
                shift_rows(kb, F["v_"][:], Pv, F["tA"], hmu["mu_v"], omu["mu_v"], 128, S, F["v_"].r)
                w0c = col["w0f"] if d == 0 else col["w0b"]
                a0c = col["a0f"] if d == 0 else col["a0b"]
                for hf in range(2):
                    sl = slice(hf * 512, (hf + 1) * 512)
                    pt, pr = ps.next()
                    kb.op("pe", lambda e, pt=pt, sl=sl, d=d: e.matmul(pt[:], lhsT=wlp[d][:], rhs=tw[:, sl], start=True, stop=True),
                          reads=rs(wlp[d], tw), writes=[pr])
                    kb.op("act", lambda e, pt=pt, sl=sl, w0c=w0c: e.activation(out=F["sgw"][:, sl], in_=pt[:], func=AF.Sigmoid, bias=w0c[:, 0:1]),
                          reads=[pr] + rs(w0c), writes=rs(F["sgw"]))
                    pt, pr = ps.next()
                    kb.op("pe", lambda e, pt=pt, sl=sl, d=d: e.matmul(pt[:], lhsT=alp[d][:], rhs=adp[:, sl], start=True, stop=True),
                          reads=rs(alp[d], adp), writes=[pr])
                    kb.op("act", lambda e, pt=pt, sl=sl, a0c=a0c: e.activation(out=F["a_"][:, sl], in_=pt[:], func=AF.Sigmoid, bias=a0c[:, 0:1]),
                          reads=[pr] + rs(a0c), writes=rs(F["a_"]))
                ew("dve", "tensor_scalar", [F["k_"], col["k_k"]], [F["kk"]], out=F["kk"][:], in0=F["k_"][:], scalar1=col["k_k"][:, 0:1], scalar2=None, op0=ALU.mult)
                ew("act", "activation", [F["kk"]], [sqr], out=sqr[:], in_=F["kk"][:], func=AF.Square)
                for hf in range(2):
                    sl = slice(hf * 512, (hf + 1) * 512)
                    pt, pr = ps.next()
                    kb.op("pe", lambda e, pt=pt, sl=sl: e.matmul(pt[:], lhsT=bones[:], rhs=sqr[:, sl], start=True, stop=True),
                          reads=rs(bones, sqr), writes=[pr])
                    kb.op("act", lambda e, pt=pt, sl=sl: e.activation(out=F["tB"][:, sl], in_=pt[:], func=AF.Sqrt),
                          reads=[pr], writes=rs(F["tB"]))
                ew("dve", "tensor_scalar", [F["tB"]], [F["tB"]], out=F["tB"][:], in0=F["tB"][:], scalar1=1e-12, scalar2=None, op0=ALU.max)
                ew("dve", "reciprocal", [F["tB"]], [F["tB"]], out=F["tB"][:], in_=F["tB"][:])
                ew("dve", "tensor_tensor", [F["kk"], F["tB"]], [F["kkn"]], out=F["kkn"][:], in0=F["kk"][:], in1=F["tB"][:], op=ALU.mult)
                ew("dve", "tensor_scalar", [F["a_"], col["k_a"], omka], [F["tB"]], out=F["tB"][:], in0=F["a_"][:], scalar1=col["k_a"][:, 0:1], scalar2=omka[:, 0:1], op0=ALU.mult, op1=ALU.add)
                ew("dve", "tensor_tensor", [F["k_"], F["tB"]], [F["kdir"]], out=F["kdir"][:], in0=F["k_"][:], in1=F["tB"][:], op=ALU.mult)
                ew("pool", "tensor_tensor", [F["kkn"], F["a_"]], [F["b_"]], out=F["b_"][:], in0=F["kkn"][:], in1=F["a_"][:], op=ALU.mult)
                ew("pool", "tensor_scalar", [F["sgw"]], [F["lw"]], out=F["lw"][:], in0=F["sgw"][:], scalar1=-CEXP, scalar2=None, op0=ALU.mult)
                ew("dve", "tensor_tensor_scan", [cmask, F["lw"]], [F["Pp"]], out=F["Pp"][:], data0=cmask[:], data1=F["lw"][:], initial=0.0, op0=ALU.mult, op1=ALU.add)
                ew("pool", "tensor_tensor", [F["Pp"], F["lw"]], [F["E_"]], out=F["E_"][:], in0=F["Pp"][:], in1=F["lw"][:], op=ALU.subtract)
                Pv3 = F["Pp"][:].rearrange("p (c t) -> p c t", t=64)
                ew("dve", "tensor_copy", [F["Pp"]], [tot], out=tot[:], in_=Pv3[:, :, 63])
                ew("dve", "tensor_tensor", [tot, F["Pp"]], [F["R_"]], out=F["R_"][:].rearrange("p (c t) -> p c t", t=64),
                   in0=tot[:].unsqueeze(2).to_broadcast([128, NCH, 64]), in1=Pv3, op=ALU.subtract)
                ew("act", "activation", [tot], [wC], out=wC[:], in_=tot[:], func=AF.Exp)
                if d == 0:
                    Lc, Lp, Lr = F["Pp"], F["E_"], F["R_"]
                else:
                    ew("dve", "tensor_tensor", [tot, F["E_"]], [F["Sf"]], out=F["Sf"][:].rearrange("p (c t) -> p c t", t=64),
                       in0=tot[:].unsqueeze(2).to_broadcast([128, NCH, 64]), in1=F["E_"][:].rearrange("p (c t) -> p c t", t=64), op=ALU.subtract)
                    Lc, Lp, Lr = F["Sf"], F["R_"], F["E_"]
                ew("act", "activation", [Lc], [F["po1"]], out=F["po1"][:], in_=Lc[:], func=AF.Exp)
                ew("act", "activation", [Lp], [F["po2"]], out=F["po2"][:], in_=Lp[:], func=AF.Exp)
                ew("act", "activation", [Lc], [F["po3"]], out=F["po3"][:], in_=Lc[:], func=AF.Exp, scale=-1.0)
                ew("act", "activation", [Lr], [F["tA"]], out=F["tA"][:], in_=Lr[:], func=AF.Exp)
                ew("dve", "tensor_tensor", [F["kkn"], F["po2"]], [Bf["qt"]], out=Bf["qt"][:], in0=F["kkn"][:], in1=F["po2"][:], op=ALU.mult)
                ew("pool", "tensor_tensor", [F["r_"], F["po1"]], [Bf["rt"]], out=Bf["rt"][:], in0=F["r_"][:], in1=F["po1"][:], op=ALU.mult)
                ew("dve", "tensor_tensor", [F["kdir"], F["po3"]], [Bf["kt"]], out=Bf["kt"][:], in0=F["kdir"][:], in1=F["po3"][:], op=ALU.mult)
                ew("dve", "scalar_tensor_tensor", [F["b_"], F["po3"]], [Bf["nbt"]], out=Bf["nbt"][:], in0=F["b_"][:], scalar=-1.0, in1=F["po3"][:], op0=ALU.mult, op1=ALU.mult)
                ew("pool", "tensor_tensor", [F["kdir"], F["tA"]], [Bf["Kh"]], out=Bf["Kh"][:], in0=F["kdir"][:], in1=F["tA"][:], op=ALU.mult)
                ew("dve", "scalar_tensor_tensor", [F["b_"], F["tA"]], [Bf["Bn"]], out=Bf["Bn"][:], in0=F["b_"][:], scalar=-1.0, in1=F["tA"][:], op0=ALU.mult, op1=ALU.mult)
                ew("act", "copy", [F["v_"]], [Bf["vb"]], out=Bf["vb"][:], in_=F["v_"][:])
                if dbg is not None and d == 0 and s == 0:
                    for nm in ("r_", "k_", "v_", "a_", "kkn", "kdir", "b_", "lw", "Pp"):
                        kb.dma(dbg[nm], F[nm][:], reads=rs(F[nm]))
                m_s = cf["m_up"] if d == 0 else cf["m_lo"]
                m_st = cf["m_lo"] if d == 0 else cf["m_up"]
                m_i = cf["m_upi"] if d == 0 else cf["m_loi"]
                BI = [0, 1]

                def mm_batch(bi, out_fn, lhs_fn, rhs_fn, reads, n_acc=1, lhs2=None, rhs2=None):
                    pt, pr = ps.next()
                    for g in range(8):
                        for h in range(2):
                            hs = HS[h]
                            gs = slice(64 * g, 64 * g + 64)
                            if lhs2 is None:
                                kb.op("pe", lambda e, pt=pt, hs=hs, gs=gs, g=g: e.matmul(out_fn(pt, hs, gs), lhsT=lhs_fn(hs, g), rhs=rhs_fn(hs, g), start=True, stop=True),
                                      reads=reads, writes=[pr], pe_accum=True)
                            else:
                                kb.op("pe", lambda e, pt=pt, hs=hs, gs=gs, g=g: e.matmul(out_fn(pt, hs, gs), lhsT=lhs_fn(hs, g), rhs=rhs_fn(hs, g), start=True, stop=False),
                                      reads=reads, writes=[pr], pe_accum=True)
                                kb.op("pe", lambda e, pt=pt, hs=hs, gs=gs, g=g: e.matmul(out_fn(pt, hs, gs), lhsT=lhs2(hs, g), rhs=rhs2(hs, g), start=False, stop=True),
                                      reads=reads, writes=[pr], pe_accum=True)
                    return pt, pr

                def f32out(pt, hs, gs):
                    return pt[hs, gs]

                def cm(tile, bi):
                    return lambda hs, g: tile[hs, bi * 512 + 64 * g: bi * 512 + 64 * g + 64]

                def tmj(tile):
                    return lambda hs, g: tile[hs, 64 * g:64 * g + 64]

                for (src, dst) in (("vb", "Vtm"), ("qt", "Qtm"), ("Kh", "Khtm"), ("Bn", "Bhtm")):
                    for bi in BI:
                        pt, pr = ps.next()
                        pv = psbf(pt)
                        for g in range(8):
                            for h in range(2):
                                hs = HS[h]
                                kb.op("pe", lambda e, pv=pv, hs=hs, g=g, src=src, bi=bi: e.transpose(out=pv[hs, 64 * g:64 * g + 64], in_=Bf[src][hs, bi * 512 + 64 * g: bi * 512 + 64 * g + 64], identity=ident[hs, :]),
                                      reads=rs(Bf[src], ident), writes=[pr], pe_accum=True)
                        kb.op("act", lambda e, pv=pv, dst=dst, bi=bi: e.copy(out=PB[bi][dst][:], in_=pv[:, 0:512]), reads=[pr], writes=rs(PB[bi][dst]))
                for bi in BI:
                    pt, pr = mm_batch(bi, f32out, cm(Bf["nbt"], bi), cm(Bf["qt"], bi), rs(Bf["nbt"], Bf["qt"]))
                    ew("dve", "tensor_tensor", [pr, m_s], [PB[bi]["Um"]], out=PB[bi]["Um"][:], in0=pt[:], in1=m_s[:], op=ALU.mult)
                    ew("pool", "tensor_tensor", [PB[bi]["Um"], cf["itile"]], [PB[bi]["Xa"]], out=PB[bi]["Xa"][:], in0=PB[bi]["Um"][:], in1=cf["itile"][:], op=ALU.add)
                for bi in BI:
                    pt, pr = mm_batch(bi, f32out, cm(Bf["qt"], bi), cm(Bf["nbt"], bi), rs(Bf["nbt"], Bf["qt"]))
                    ew("dve", "tensor_tensor", [pr, m_st], [PB[bi]["Utm"]], out=PB[bi]["Utm"][:], in0=pt[:], in1=m_st[:], op=ALU.mult)
                for (lname, rname, mask, dst) in (("kt", "qt", m_s, "AkkT"), ("kt", "rt", m_i, "ArkT"), ("nbt", "rt", m_i, "nArbT")):
                    for bi in BI:
                        pt, pr = mm_batch(bi, f32out, cm(Bf[lname], bi), cm(Bf[rname], bi), rs(Bf[lname], Bf[rname]))
                        ew("dve", "tensor_tensor", [pr, mask], [PB[bi][dst]], out=PB[bi][dst][:], in0=pt[:], in1=mask[:], op=ALU.mult)
                for bi in BI:
                    pt, pr = mm_batch(bi, f32out, tmj(PB[bi]["AkkT"]), tmj(PB[bi]["Vtm"]), rs(PB[bi]["AkkT"], PB[bi]["Vtm"]))
                    ew("act", "copy", [pr], [PB[bi]["AV"]], out=PB[bi]["AV"][:], in_=pt[:])
                Pc = {bi: ("Um", "Utm") for bi in BI}
                Xc = {bi: "Xa" for bi in BI}
                for lev in range(1, 6):
                    pn, ptn = ("Pa", "Pta") if lev % 2 == 1 else ("Pb", "Ptb")
                    for bi in BI:
                        P_, Pt_ = Pc[bi]
                        if lev < 5:
                            pt, pr = mm_batch(bi, f32out, tmj(PB[bi][Pt_]), tmj(PB[bi][P_]), rs(PB[bi][Pt_], PB[bi][P_]))
                            ew("act", "copy", [pr], [PB[bi][pn]], out=PB[bi][pn][:], in_=pt[:])
                        pt, pr = mm_batch(bi, f32out, tmj(PB[bi][P_]), tmj(PB[bi][Pt_]), rs(PB[bi][Pt_], PB[bi][P_]))
                        ew("act", "copy", [pr], [PB[bi][ptn]], out=PB[bi][ptn][:], in_=pt[:])
                    for bi in BI:
                        xo = Xc[bi]
                        xn = "Xb" if xo == "Xa" else "Xa"
                        pt, pr = mm_batch(bi, f32out, tmj(PB[bi][ptn]), tmj(PB[bi][xo]), rs(PB[bi][ptn], PB[bi][xo]))
                        ew("dve", "tensor_tensor", [pr, PB[bi][xo]], [PB[bi][xn]], out=PB[bi][xn][:], in0=pt[:], in1=PB[bi][xo][:], op=ALU.add)
                        Xc[bi] = xn
                        Pc[bi] = (pn, ptn)
                for bi in BI:
                    X5 = PB[bi][Xc[bi]]
                    pt, pr = mm_batch(bi, f32out, tmj(X5), tmj(PB[bi]["AV"]), rs(X5, PB[bi]["AV"]))
                    ew("act", "copy", [pr], [PB[bi]["Gtm"]], out=PB[bi]["Gtm"][:], in_=pt[:])
                    pt, pr = mm_batch(bi, f32out, tmj(X5), tmj(PB[bi]["Qtm"]), rs(X5, PB[bi]["Qtm"]))
                    ew("act", "copy", [pr], [PB[bi]["Qh"]], out=PB[bi]["Qh"][:], in_=pt[:])
                for bi in BI:
                    pt, pr = mm_batch(bi, f32out, tmj(PB[bi]["Qh"]), tmj(PB[bi]["Bhtm"]), rs(PB[bi]["Qh"], PB[bi]["Bhtm"]))
                    for g in range(8):
                        cidx = bi * 8 + g
                        gs = slice(64 * g, 64 * g + 64)
                        ew("dve", "scalar_tensor_tensor", [pr, cf["itile"], wC], [PB[bi]["PhiT"]], out=PB[bi]["PhiT"][:, gs], in0=cf["itile"][:, gs],
                           scalar=wC[:, cidx:cidx + 1], in1=pt[:, gs], op0=ALU.mult, op1=ALU.add)
                    pt, pr = mm_batch(bi, f32out, tmj(PB[bi]["Qh"]), tmj(PB[bi]["nArbT"]), rs(PB[bi]["Qh"], PB[bi]["nArbT"]))
                    ew("dve", "tensor_tensor", [pr, Bf["rt"]], [PB[bi]["RhT"]], out=PB[bi]["RhT"][:], in0=pt[:], in1=Bf["rt"][:, bi * 512:(bi + 1) * 512], op=ALU.add)
                    pt, pr = mm_batch(bi, f32out, tmj(PB[bi]["Khtm"]), tmj(PB[bi]["Vtm"]), rs(PB[bi]["Khtm"], PB[bi]["Vtm"], PB[bi]["Bhtm"], PB[bi]["Gtm"]),
                                      lhs2=tmj(PB[bi]["Bhtm"]), rhs2=tmj(PB[bi]["Gtm"]))
                    ew("act", "copy", [pr], [Psi[bi]], out=Psi[bi][:], in_=pt[:])
                order = list(range(NCH))
                if d == 1:
                    order = order[::-1]
                for cidx in order:
                    bi, g = divmod(cidx, 8)
                    gs = slice(64 * g, 64 * g + 64)
                    pt, pr = ps.next()
                    for h in range(2):
                        hs = HS[h]
                        kb.op("pe", lambda e, pt=pt, hs=hs, gs=gs, bi=bi: e.matmul(pt[hs, 0:64], lhsT=PB[bi]["Vtm"][hs, gs], rhs=PB[bi]["ArkT"][hs, gs], start=True, stop=False),
                              reads=rs(PB[bi]["Vtm"], PB[bi]["ArkT"]), writes=[pr], pe_accum=True)
                        kb.op("pe", lambda e, pt=pt, hs=hs, gs=gs, bi=bi: e.matmul(pt[hs, 0:64], lhsT=PB[bi]["Gtm"][hs, gs], rhs=PB[bi]["nArbT"][hs, gs], start=False, stop=False),
                              reads=rs(PB[bi]["Gtm"], PB[bi]["nArbT"]), writes=[pr], pe_accum=True)
                        kb.op("pe", lambda e, pt=pt, hs=hs, gs=gs, bi=bi: e.matmul(pt[hs, 0:64], lhsT=Hbf[hs, :], rhs=PB[bi]["RhT"][hs, gs], start=False, stop=True),
                              reads=rs(Hbf, PB[bi]["RhT"]), writes=[pr], pe_accum=True)
                    ew("act", "copy", [pr], [F["yc"]], out=F["yc"][:, cidx * 64:(cidx + 1) * 64], in_=pt[:, 0:64])
                    pt2, pr2 = ps.next()
                    for h in range(2):
                        hs = HS[h]
                        kb.op("pe", lambda e, pt2=pt2, hs=hs, gs=gs, bi=bi: e.matmul(pt2[hs, 0:64], lhsT=PB[bi]["PhiT"][hs, gs], rhs=Hbf[hs, :], start=True, stop=True),
                              reads=rs(PB[bi]["PhiT"], Hbf), writes=[pr2], pe_accum=True)
                    ew("dve", "tensor_tensor", [pr2, Psi[bi]], [Hbf], out=Hbf[:], in0=pt2[:, 0:64], in1=Psi[bi][:, gs], op=ALU.add)
                if d == 0:
                    kb.dma(yfT[c0:c0 + 128, tsl], F["yc"][:], reads=rs(F["yc"]), eng="pool")
                else:
                    kb.dma(F["yf"][:], yfT[c0:c0 + 128, tsl], writes=rs(F["yf"]), eng="sp")
                    kb.dma(sg0[:], loraA[256:384, tsl], writes=rs(sg0), eng="act")
                    kb.dma(sg1[0:32, :], loraA[384:416, tsl], writes=rs(sg1), eng="act")
                    for hf in range(2):
                        sl = slice(hf * 512, (hf + 1) * 512)
                        pt, pr = ps.next()
                        kb.op("pe", lambda e, pt=pt, sl=sl: e.matmul(pt[:], lhsT=alp[0][:], rhs=adp[:, sl], start=True, stop=True),
                              reads=rs(alp[0], adp), writes=[pr])
                        kb.op("act", lambda e, pt=pt, sl=sl: e.activation(out=F["af"][:, sl], in_=pt[:], func=AF.Sigmoid, bias=col["a0f"][:, 0:1]),
                              reads=[pr] + rs(col["a0f"]), writes=rs(F["af"]))
                    ew("dve", "tensor_tensor", [F["af"], F["a_"]], [F["af"]], out=F["af"][:], in0=F["af"][:], in1=F["a_"][:], op=ALU.add)
                    ew("dve", "tensor_scalar", [F["af"], hka, omka], [F["af"]], out=F["af"][:], in0=F["af"][:], scalar1=hka[:, 0:1], scalar2=omka[:, 0:1], op0=ALU.mult, op1=ALU.add)
                    ew("dve", "tensor_tensor", [F["af"], F["k_"]], [F["af"]], out=F["af"][:], in0=F["af"][:], in1=F["k_"][:], op=ALU.mult)
                    ew("dve", "scalar_tensor_tensor", [F["af"], col["r_k"], F["r_"]], [sqr], out=sqr[:], in0=F["af"][:], scalar=col["r_k"][:, 0:1], in1=F["r_"][:], op0=ALU.mult, op1=ALU.mult)
                    ew("dve", "tensor_tensor", [F["yc"], F["yf"]], [F["yc"]], out=F["yc"][:], in0=F["yc"][:], in1=F["yf"][:], op=ALU.add)
                    ew("act", "copy", [F["yc"]], [tw], out=tw[:], in_=F["yc"][:])
                    ew("act", "activation", [F["yc"]], [adp], out=adp[:], in_=F["yc"][:], func=AF.Square)
                    for hf in range(2):
                        sl = slice(hf * 512, (hf + 1) * 512)
                        for (src, dstn, scale) in ((tw, "po1", 1.0 / 64), (adp, "po2", 1.0 / 64), (sqr, "po3", 1.0)):
                            pt, pr = ps.next()
                            kb.op("pe", lambda e, pt=pt, sl=sl, src=src: e.matmul(pt[:], lhsT=bones[:], rhs=src[:, sl], start=True, stop=True),
                                  reads=rs(bones, src), writes=[pr])
                            kb.op("act", lambda e, pt=pt, sl=sl, dstn=dstn, scale=scale: e.mul(out=F[dstn][:, sl], in_=pt[:], mul=scale),
                                  reads=[pr], writes=rs(F[dstn]))
                        pt, pr = ps.next()
                        kb.op("pe", lambda e, pt=pt, sl=sl: e.matmul(pt[:], lhsT=gl0[:], rhs=sg0[:, sl], start=True, stop=False),
                              reads=rs(gl0, sg0), writes=[pr], pe_accum=True)
                        kb.op("pe", lambda e, pt=pt, sl=sl: e.matmul(pt[:], lhsT=gl1[:], rhs=sg1[:, sl], start=False, stop=True),
                              reads=rs(gl1, sg1), writes=[pr], pe_accum=True)
                        kb.op("act", lambda e, pt=pt, sl=sl: e.copy(out=F["tA"][:, sl], in_=pt[:]), reads=[pr], writes=rs(F["tA"]))
                    ew("dve", "tensor_tensor", [F["po1"]], [F["tB"]], out=F["tB"][:], in0=F["po1"][:], in1=F["po1"][:], op=ALU.mult)
                    ew("dve", "tensor_tensor", [F["po2"], F["tB"]], [F["po2"]], out=F["po2"][:], in0=F["po2"][:], in1=F["tB"][:], op=ALU.subtract)
                    ew("act", "activation", [F["po2"]], [F["po2"]], out=F["po2"][:], in_=F["po2"][:], func=AF.Sqrt, bias=64e-5)
                    ew("dve", "reciprocal", [F["po2"]], [F["po2"]], out=F["po2"][:], in_=F["po2"][:])
                    ew("dve", "tensor_tensor", [F["yc"], F["po1"]], [F["yc"]], out=F["yc"][:], in0=F["yc"][:], in1=F["po1"][:], op=ALU.subtract)
                    ew("dve", "tensor_tensor", [F["yc"], F["po2"]], [F["yc"]], out=F["yc"][:], in0=F["yc"][:], in1=F["po2"][:], op=ALU.mult)
                    ew("dve", "tensor_scalar", [F["yc"], col["lnx_g"], col["lnx_b"]], [F["yc"]], out=F["yc"][:], in0=F["yc"][:], scalar1=col["lnx_g"][:, 0:1], scalar2=col["lnx_b"][:, 0:1], op0=ALU.mult, op1=ALU.add)
                    ew("dve", "tensor_tensor", [F["po3"], F["v_"]], [F["po3"]], out=F["po3"][:], in0=F["po3"][:], in1=F["v_"][:], op=ALU.mult)
                    ew("dve", "tensor_tensor", [F["yc"], F["po3"]], [F["yc"]], out=F["yc"][:], in0=F["yc"][:], in1=F["po3"][:], op=ALU.add)
                    ew("dve", "tensor_tensor", [F["yc"], F["tA"]], [yout], out=yout[:], in0=F["yc"][:], in1=F["tA"][:], op=ALU.mult)
                    kb.dma(yaT[c0:c0 + 128, tsl], yout[:], reads=rs(yout), eng="pool")
    kb.emit()


def stage_pool(nc, projT, pool_w, pool_scale, ybT, cnt_inv):
    kb = KB(nc)
    S = 1024
    HL = 8
    ps = PS(kb)
    Pp = [Tl(kb, f"Pp{i}", [128, S + 2 * HL]) for i in range(2)]
    acc = Tl(kb, "acc", [128, S + 2 * HL])
    acc2 = Tl(kb, "acc2", [128, S + 2 * HL])
    z = [Tl(kb, f"z{i}", [128, S], F32R) for i in range(2)]
    ci = Tl(kb, "ci", [128, S])
    pw = [Tl(kb, f"pw{i}", [128, 256], F32R) for i in range(2)]
    sc = Tl(kb, "sc", [128, 1])
    ot = [Tl(kb, f"ot{i}", [128, S], F32R) for i in range(2)]
    oi = 0
    for g, win in enumerate((2, 4, 8, 16)):
        half = win // 2
        for kc in range(2):
            kb.dma(pw[kc][:], pool_w[g, kc * 128:(kc + 1) * 128, :], writes=rs(pw[kc]))
        for s in range(T // S):
            t0 = s * S
            kb.dma(ci[:], cnt_inv[g:g + 1, t0:t0 + S].partition_broadcast(128), writes=rs(ci), eng="act")
            for kc in range(2):
                r0 = 3488 + g * 256 + kc * 128
                P = Pp[kc]
                lo = max(t0 - HL, 0)
                hi = min(t0 + S + HL, T)
                kb.dma(P[:, lo - (t0 - HL):hi - (t0 - HL)], projT[r0:r0 + 128, lo:hi], writes=rs(P), eng="sp")
                if t0 == 0:
                    kb.op("pool", lambda e, P=P: e.memset(P[:, 0:HL], 0.0), writes=rs(P))
                if t0 + S == T:
                    kb.op("pool", lambda e, P=P: e.memset(P[:, S + HL:S + 2 * HL], 0.0), writes=rs(P))
                W_ = S + 2 * HL
                kb.op("dve", lambda e, P=P: e.tensor_tensor(out=acc[:, 1:W_], in0=P[:, 0:W_ - 1], in1=P[:, 1:W_], op=ALU.add),
                      reads=rs(P), writes=rs(acc))
                cur, oth = acc, acc2
                wlen = 2
                while wlen < win:
                    kb.op("dve", lambda e, cur=cur, oth=oth, wlen=wlen: e.tensor_tensor(out=oth[:, 2 * wlen - 1:W_], in0=cur[:, 2 * wlen - 1:W_], in1=cur[:, wlen - 1:W_ - wlen], op=ALU.add),
                          reads=rs(cur), writes=rs(oth))
                    cur, oth = oth, cur
                    wlen *= 2
                off = HL + half - 1
                kb.op("dve", lambda e, cur=cur, off=off: e.tensor_tensor(out=acc2[:, 0:S] if cur is acc else acc[:, 0:S], in0=cur[:, off:off + S], in1=ci[:], op=ALU.mult),
                      reads=rs(cur, ci), writes=rs(acc2 if cur is acc else acc))
                mt = acc2 if cur is acc else acc
                kb.op("dve", lambda e, mt=mt, P=P, kc=kc: e.tensor_tensor(out=z[kc][:], in0=mt[:, 0:S], in1=P[:, HL:HL + S], op=ALU.subtract),
                      reads=rs(mt, P), writes=rs(z[kc]))
            for oc in range(2):
                c0 = g * 256 + oc * 128
                load_col(kb, sc, pool_scale[c0:c0 + 128])
                o = ot[oi % 2]
                oi += 1
                for hf in range(2):
                    sl = slice(hf * 512, (hf + 1) * 512)
                    pt, pr = ps.next()
                    for kc in range(2):
                        kb.op("pe", lambda e, pt=pt, kc=kc, oc=oc, sl=sl: e.matmul(pt[:], lhsT=pw[kc][:, oc * 128:(oc + 1) * 128], rhs=z[kc][:, sl], start=(kc == 0), stop=(kc == 1)),
                              reads=rs(pw[kc], z[kc]), writes=[pr], pe_accum=True)
                    kb.op("act", lambda e, pt=pt, o=o, sl=sl: e.activation(out=o[:, sl], in_=pt[:], func=AF.Identity, scale=sc[:, 0:1]),
                          reads=[pr] + rs(sc), writes=rs(o))
                kb.dma(ybT[c0:c0 + 128, t0:t0 + S], o[:], reads=rs(o), eng="pool")
    kb.emit()


def stage_mix(nc, xin, projT, yaT, ybT, w_up_a, w_up_b, w_o, g2, w_router, xmT, v_tm, affT, identf_ap):
    kb = KB(nc)
    N = 512
    KC = D // 128
    ps = PS(kb)
    ya = Tl(kb, "ya", [128, 8, N], F32R)
    yb = Tl(kb, "yb", [128, 8, N], F32R)
    mg = kb.sb("mg", [128, KC, N], F32R)
    mg_r = [kb.res(f"mg{k}") for k in range(KC)]
    xm = kb.sb("xm", [128, KC, N])
    xm_r = [kb.res(f"xm{k}") for k in range(KC)]
    vt = kb.sb("vt", [128, KC, N])
    vt_r = [kb.res(f"vt{k}") for k in range(KC)]
    wa = [Tl(kb, f"wa{i}", [128, 8, 128], F32R) for i in range(2)]
    wb = [Tl(kb, f"wb{i}", [128, 8, 128], F32R) for i in range(2)]
    wo = [Tl(kb, f"wo{i}", [128, KC, 128], F32R) for i in range(2)]
    sgA = [Tl(kb, f"sgA{i}", [128, N]) for i in range(2)]
    sgB = [Tl(kb, f"sgB{i}", [128, N]) for i in range(2)]
    xi = [Tl(kb, f"xi{i}", [128, N]) for i in range(2)]
    t1 = Tl(kb, "t1", [128, N])
    t2 = Tl(kb, "t2", [128, N])
    sq = [Tl(kb, f"sq{i}", [128, N], F32R) for i in range(2)]
    rstd = Tl(kb, "rstd", [128, N])
    gt = Tl(kb, "gt", [128, KC])
    onesf = Tl(kb, "onesf", [128, 128])
    ones = Tl(kb, "ones", [128, 128], F32R)
    idf = Tl(kb, "idf", [128, 128])
    wr = Tl(kb, "wr", [128, KC, NE])
    ex = Tl(kb, "ex", [NE, N])
    rsum = Tl(kb, "rsum", [NE, N])
    vo = [Tl(kb, f"vo{i}", [128, D], BF16) for i in range(2)]
    kb.op("pool", lambda e: e.memset(onesf[:], 1.0), writes=rs(onesf))
    kb.op("dve", lambda e: e.tensor_copy(out=ones[:], in_=onesf[:]), reads=rs(onesf), writes=rs(ones))
    kb.dma(idf[:], identf_ap, writes=rs(idf))
    kb.dma(gt[:], g2.rearrange("(k p) -> p k", p=128), writes=rs(gt), allow_slow_non_contiguous=True)
    kb.dma(wr[:], w_router.rearrange("(k p) e -> p k e", p=128), writes=rs(wr))
    wav = w_up_a.rearrange("(k p) n -> p k n", p=128)
    wbv = w_up_b.rearrange("(k p) n -> p k n", p=128)
    wov = w_o.rearrange("(k p) n -> p k n", p=128)
    it = 0
    voi = 0
    for s in range(T // N):
        tsl = slice(s * N, (s + 1) * N)
        kb.dma(ya[:], yaT[:, tsl].rearrange("(k p) n -> p k n", p=128), writes=rs(ya), eng="sp")
        kb.dma(yb[:], ybT[:, tsl].rearrange("(k p) n -> p k n", p=128), writes=rs(yb), eng="act")
        for oc in range(KC):
            b = it % 2
            it += 1
            osl = slice(oc * 128, (oc + 1) * 128)
            kb.dma(wa[b][:], wav[:, :, osl], writes=rs(wa[b]), eng="sp")
            kb.dma(wb[b][:], wbv[:, :, osl], writes=rs(wb[b]), eng="act")
            kb.dma(sgA[b][:], projT[4512 + oc * 128:4512 + (oc + 1) * 128, tsl], writes=rs(sgA[b]), eng="sp")
            kb.dma(sgB[b][:], projT[6560 + oc * 128:6560 + (oc + 1) * 128, tsl], writes=rs(sgB[b]), eng="act")
            pa, par = ps.next()
            for kc in range(8):
                kb.op("pe", lambda e, pa=pa, b=b, kc=kc: e.matmul(pa[:], lhsT=wa[b][:, kc, :], rhs=ya[:, kc, :], start=(kc == 0), stop=(kc == 7)),
                      reads=rs(wa[b], ya), writes=[par], pe_accum=True)
            pb_, pbr = ps.next()
            for kc in range(8):
                kb.op("pe", lambda e, pb_=pb_, b=b, kc=kc: e.matmul(pb_[:], lhsT=wb[b][:, kc, :], rhs=yb[:, kc, :], start=(kc == 0), stop=(kc == 7)),
                      reads=rs(wb[b], yb), writes=[pbr], pe_accum=True)
            kb.op("dve", lambda e, pa=pa, b=b: e.tensor_tensor(out=t1[:], in0=pa[:], in1=sgA[b][:], op=ALU.mult), reads=[par] + rs(sgA[b]), writes=rs(t1))
            kb.op("dve", lambda e, pb_=pb_, b=b: e.tensor_tensor(out=t2[:], in0=pb_[:], in1=sgB[b][:], op=ALU.mult), reads=[pbr] + rs(sgB[b]), writes=rs(t2))
            kb.op("pool", lambda e, oc=oc: e.tensor_tensor(out=mg[:, oc, :], in0=t1[:], in1=t2[:], op=ALU.add), reads=rs(t1, t2), writes=[mg_r[oc]])
        pss, pssr = ps.next()
        for oc in range(KC):
            b = it % 2
            it += 1
            osl = slice(oc * 128, (oc + 1) * 128)
            kb.dma(wo[b][:], wov[:, :, osl], writes=rs(wo[b]), eng="sp")
            kb.dma(xi[b][:], xin[osl, tsl], writes=rs(xi[b]), eng="act")
            po, por = ps.next()
            if po is pss:
                po, por = ps.next()
            for kc in range(KC):
                kb.op("pe", lambda e, po=po, b=b, kc=kc: e.matmul(po[:], lhsT=wo[b][:, kc, :], rhs=mg[:, kc, :], start=(kc == 0), stop=(kc == KC - 1)),
                      reads=[wo[b].r, mg_r[kc]], writes=[por], pe_accum=True)
            kb.op("dve", lambda e, po=po, b=b, oc=oc: e.tensor_tensor(out=xm[:, oc, :], in0=po[:], in1=xi[b][:], op=ALU.add), reads=[por] + rs(xi[b]), writes=[xm_r[oc]])
            kb.dma(xmT[osl, tsl], xm[:, oc, :], reads=[xm_r[oc]], eng="pool")
            a = oc % 2
            kb.op("act", lambda e, a=a, oc=oc: e.activation(out=sq[a][:], in_=xm[:, oc, :], func=AF.Square), reads=[xm_r[oc]], writes=rs(sq[a]))
            kb.op("pe", lambda e, a=a, oc=oc, pss=pss: e.matmul(pss[:], lhsT=ones[:], rhs=sq[a][:], start=(oc == 0), stop=(oc == KC - 1)),
                  reads=rs(ones, sq[a]), writes=[pssr], pe_accum=True)
        kb.op("act", lambda e, pss=pss: e.activation(out=rstd[:], in_=pss[:], func=AF.Sqrt, scale=1.0 / D, bias=EPS), reads=[pssr], writes=rs(rstd))
        kb.op("dve", lambda e: e.reciprocal(out=rstd[:], in_=rstd[:]), writes=rs(rstd))
        for oc in range(KC):
            kb.op("dve", lambda e, oc=oc: e.scalar_tensor_tensor(out=vt[:, oc, :], in0=xm[:, oc, :], scalar=gt[:, oc:oc + 1], in1=rstd[:], op0=ALU.mult, op1=ALU.mult),
                  reads=[xm_r[oc]] + rs(gt, rstd), writes=[vt_r[oc]])
        pl, plr = ps.next()
        for kc in range(KC):
            kb.op("pe", lambda e, pl=pl, kc=kc: e.matmul(pl[0:NE, :], lhsT=wr[:, kc, :], rhs=vt[:, kc, :], start=(kc == 0), stop=(kc == KC - 1)),
                  reads=[wr.r, vt_r[kc]], writes=[plr], pe_accum=True)
        kb.op("act", lambda e, pl=pl: e.activation(out=ex[:], in_=pl[0:NE, :], func=AF.Exp), reads=[plr], writes=rs(ex))
        pq, pqr = ps.next()
        kb.op("pe", lambda e, pq=pq: e.matmul(pq[0:NE, :], lhsT=onesf[0:NE, 0:NE], rhs=ex[:], start=True, stop=True), reads=rs(onesf, ex), writes=[pqr])
        kb.op("dve", lambda e, pq=pq: e.reciprocal(out=rsum[:], in_=pq[0:NE, :]), reads=[pqr], writes=rs(rsum))
        kb.op("dve", lambda e: e.tensor_tensor(out=ex[:], in0=ex[:], in1=rsum[:], op=ALU.mult), reads=rs(rsum), writes=rs(ex))
        kb.dma(affT[:, tsl], ex[:], reads=rs(ex), eng="sp")
        for tb in range(N // 128):
            o = vo[voi % 2]
            voi += 1
            for q in range(4):
                ptt, ptr = ps.next()
                for j in range(4):
                    oc = q * 4 + j
                    kb.op("pe", lambda e, ptt=ptt, oc=oc, tb=tb, j=j: e.transpose(out=ptt[:, j * 128:(j + 1) * 128], in_=vt[:, oc, tb * 128:(tb + 1) * 128], identity=idf[:]),
                          reads=[vt_r[oc]] + rs(idf), writes=[ptr], pe_accum=True)
                eng = "act" if q % 2 == 0 else "dve"
                if eng == "act":
                    kb.op("act", lambda e, ptt=ptt, o=o, q=q: e.copy(out=o[:, q * 512:(q + 1) * 512], in_=ptt[:]), reads=[ptr], writes=rs(o))
                else:
                    kb.op("dve", lambda e, ptt=ptt, o=o, q=q: e.tensor_copy(out=o[:, q * 512:(q + 1) * 512], in_=ptt[:]), reads=[ptr], writes=rs(o))
            kb.dma(v_tm[s * N + tb * 128:s * N + (tb + 1) * 128, :], o[:], reads=rs(o), eng="pool")
    kb.emit()


def stage_route(nc, affT, posD, gateD):
    kb = KB(nc)
    aff = Tl(kb, "aff", [NE, T])
    junk = Tl(kb, "junk", [NE, T])
    onesr = Tl(kb, "onesr", [NE, T])
    cs = Tl(kb, "cs", [NE, T])
    lo = Tl(kb, "lo", [NE, 1])
    hi = Tl(kb, "hi", [NE, 1])
    mid = Tl(kb, "mid", [NE, 1])
    cnt = Tl(kb, "cnt", [NE, 1])
    ge = Tl(kb, "ge", [NE, 1])
    d1 = Tl(kb, "d1", [NE, 1])
    d2 = Tl(kb, "d2", [NE, 1])

    def ew(eng, fname, reads, writes, **kw):
        kb.op(eng, lambda e: getattr(e, fname)(**kw), reads=rs(*reads), writes=rs(*writes))

    kb.dma(aff[:], affT, writes=rs(aff))
    ew("pool", "memset", [], [onesr], ap=onesr[:], constant=1.0)
    ew("pool", "memset", [], [lo], ap=lo[:], constant=0.0)
    ew("pool", "memset", [], [hi], ap=hi[:], constant=1.0)
    for it in range(30):
        w = 2.0 ** -(it + 1)
        ew("dve", "tensor_scalar", [lo], [mid], out=mid[:], in0=lo[:], scalar1=w, scalar2=None, op0=ALU.add)
        ew("dve", "tensor_scalar", [aff, mid], [junk, cnt], out=junk[:], in0=aff[:], scalar1=mid[:, 0:1], scalar2=None, op0=ALU.is_ge, op1=ALU.add, accum_out=cnt[:])
        ew("dve", "tensor_scalar", [cnt], [ge], out=ge[:], in0=cnt[:], scalar1=float(CAP) - 0.5, scalar2=w, op0=ALU.is_ge, op1=ALU.mult)
        ew("dve", "tensor_tensor", [lo, ge], [lo], out=lo[:], in0=lo[:], in1=ge[:], op=ALU.add)
    ew("dve", "tensor_scalar", [aff, lo], [junk], out=junk[:], in0=aff[:], scalar1=lo[:, 0:1], scalar2=None, op0=ALU.is_ge)
    ew("dve", "tensor_tensor_scan", [onesr, junk], [cs], out=cs[:], data0=onesr[:], data1=junk[:], initial=0.0, op0=ALU.mult, op1=ALU.add)
    ew("dve", "tensor_tensor", [cs, junk], [cs], out=cs[:], in0=cs[:], in1=junk[:], op=ALU.mult)
    ew("dve", "tensor_scalar", [cs], [cs], out=cs[:], in0=cs[:], scalar1=-1.0, scalar2=None, op0=ALU.add)
    kb.dma(posD, cs[:], reads=rs(cs))
    ew("dve", "tensor_tensor", [aff, junk], [aff], out=aff[:], in0=aff[:], in1=junk[:], op=ALU.mult)
    kb.dma(gateD, aff[:], reads=rs(aff), eng="act")
    kb.emit()


def stage_experts(nc, posD, v_tm, w_gate, w_up, w_down, yD, iota_row, experts=range(NE)):
    kb = KB(nc)
    KC = D // 128
    NF = FF // 128
    NTI = T // 128
    ps = PS(kb)
    iot = Tl(kb, "iot", [128, 512])
    kb.dma(iot[:], iota_row, writes=rs(iot))
    ptm = Tl(kb, "ptm", [128, NTI])
    sel = [Tl(kb, f"sel{i}", [128, 512], BF16) for i in range(2)]
    vt = [Tl(kb, f"vt{i}", [128, D], BF16) for i in range(2)]
    xs = kb.sb("xs", [128, KC, 512], F32R)
    xs_r = [kb.res(f"xs{k}") for k in range(KC)]
    hT = kb.sb("hT", [128, NF, 512], F32R)
    hT_r = [kb.res(f"hT{k}") for k in range(NF)]
    wbuf = [Tl(kb, f"wb{i}", [128, KC * 256], F32R) for i in range(4)]
    sil = Tl(kb, "sil", [128, 512])
    yo = [Tl(kb, f"yo{i}", [128, D], BF16) for i in range(2)]
    vv = v_tm.rearrange("(p n) d -> p n d", n=NTI)
    wi = 0
    yi = 0
    si = 0
    for e_ in experts:
        kb.dma(ptm[:], posD[e_, :].rearrange("(p n) -> p n", n=NTI), writes=rs(ptm))
        for half in range(2):
            banks = [ps.next() for _ in range(8)]
            for n in range(NTI):
                b = si % 2
                si += 1
                kb.dma(vt[b][:, half * 1024:(half + 1) * 1024], vv[:, n, half * 1024:(half + 1) * 1024], writes=rs(vt[b]), eng=("sp" if n % 2 == 0 else "act"))
                kb.op("dve", lambda e, b=b, n=n: e.tensor_scalar(out=sel[b][:], in0=iot[:], scalar1=ptm[:, n:n + 1], scalar2=None, op0=ALU.is_equal),
                      reads=rs(iot, ptm), writes=rs(sel[b]))
                for ci in range(8):
                    c = half * 8 + ci
                    pt, pr = banks[ci]
                    kb.op("pe", lambda e, pt=pt, b=b, c=c, n=n: e.matmul(pt[:], lhsT=vt[b][:, c * 128:(c + 1) * 128], rhs=sel[b][:], start=(n == 0), stop=(n == NTI - 1)),
                          reads=rs(vt[b], sel[b]), writes=[pr], pe_accum=True)
            for ci in range(8):
                c = half * 8 + ci
                pt, pr = banks[ci]
                if ci % 2 == 0:
                    kb.op("act", lambda e, pt=pt, c=c: e.copy(out=xs[:, c, :], in_=pt[:]), reads=[pr], writes=[xs_r[c]])
                else:
                    kb.op("dve", lambda e, pt=pt, c=c: e.tensor_copy(out=xs[:, c, :], in_=pt[:]), reads=[pr], writes=[xs_r[c]])
        wgv = w_gate[e_].rearrange("(k p) f -> p k f", p=128)
        wuv = w_up[e_].rearrange("(k p) f -> p k f", p=128)
        for fp in range(NF // 2):
            bg = wbuf[(wi * 2) % 4]
            bu = wbuf[(wi * 2 + 1) % 4]
            wi += 1
            fsl = slice(fp * 256, (fp + 1) * 256)
            kb.dma(bg[:].rearrange("p (k f) -> p k f", f=256), wgv[:, :, fsl], writes=rs(bg), eng="sp")
            kb.dma(bu[:].rearrange("p (k f) -> p k f", f=256), wuv[:, :, fsl], writes=rs(bu), eng="act")
            bg3 = bg[:].rearrange("p (k f) -> p k f", f=256)
            bu3 = bu[:].rearrange("p (k f) -> p k f", f=256)
            for j in range(2):
                f = fp * 2 + j
                pg, pgr = ps.next()
                for k in range(KC):
                    kb.op("pe", lambda e, pg=pg, bg3=bg3, j=j, k=k: e.matmul(pg[:], lhsT=bg3[:, k, j * 128:(j + 1) * 128], rhs=xs[:, k, :], start=(k == 0), stop=(k == KC - 1)),
                          reads=[bg.r, xs_r[k]], writes=[pgr], pe_accum=True)
                pu, pur = ps.next()
                for k in range(KC):
                    kb.op("pe", lambda e, pu=pu, bu3=bu3, j=j, k=k: e.matmul(pu[:], lhsT=bu3[:, k, j * 128:(j + 1) * 128], rhs=xs[:, k, :], start=(k == 0), stop=(k == KC - 1)),
                          reads=[bu.r, xs_r[k]], writes=[pur], pe_accum=True)
                kb.op("act", lambda e, pg=pg: e.activation(out=sil[:], in_=pg[:], func=AF.Silu), reads=[pgr], writes=rs(sil))
                kb.op("dve", lambda e, pu=pu, f=f: e.tensor_tensor(out=hT[:, f, :], in0=pu[:], in1=sil[:], op=ALU.mult), reads=[pur] + rs(sil), writes=[hT_r[f]])
        wdv = w_down[e_].rearrange("(f p) d -> p f d", p=128)
        for qh in range(2):
            banks = [ps.next() for _ in range(8)]
            for f in range(NF):
                wb_ = wbuf[wi % 4]
                wi += 1
                kb.dma(wb_[:, 0:1024], wdv[:, f, qh * 1024:(qh + 1) * 1024], writes=rs(wb_), eng=("sp" if f % 2 == 0 else "act"))
                for j in range(4):
                    for qq in range(2):
                        pt, pr = banks[j * 2 + qq]
                        kb.op("pe", lambda e, pt=pt, wb_=wb_, f=f, j=j, qq=qq: e.matmul(pt[:], lhsT=hT[:, f, j * 128:(j + 1) * 128], rhs=wb_[:, qq * 512:(qq + 1) * 512], start=(f == 0), stop=(f == NF - 1)),
                              reads=[hT_r[f], wb_.r], writes=[pr], pe_accum=True)
            for j in range(4):
                o = yo[yi % 2]
                yi += 1
                for qq in range(2):
                    pt, pr = banks[j * 2 + qq]
                    if qq == 0:
                        kb.op("act", lambda e, pt=pt, o=o, qq=qq: e.copy(out=o[:, qq * 512:(qq + 1) * 512], in_=pt[:]), reads=[pr], writes=rs(o))
                    else:
                        kb.op("dve", lambda e, pt=pt, o=o, qq=qq: e.tensor_copy(out=o[:, qq * 512:(qq + 1) * 512], in_=pt[:]), reads=[pr], writes=rs(o))
                kb.dma(yD[e_, j * 128:(j + 1) * 128, qh * 1024:(qh + 1) * 1024], o[:, 0:1024], reads=rs(o), eng="sp")
    kb.emit()


def stage_combine(nc, posD, gateD, yD, xmT, xoutT, slot_col):
    kb = KB(nc)
    N = 512
    ps = PS(kb)
    scol = Tl(kb, "scol", [128, 4])
    kb.dma(scol[:], slot_col, writes=rs(scol))
    posb = [Tl(kb, f"posb{i}", [128, N]) for i in range(2)]
    gateb = [Tl(kb, f"gateb{i}", [128, N]) for i in range(2)]
    selg = [[Tl(kb, f"selg{i}_{j}", [128, N], BF16) for j in range(4)] for i in range(2)]
    ye = [Tl(kb, f"ye{i}", [128, 4, 1024], BF16) for i in range(2)]
    xi = [Tl(kb, f"xi{i}", [128, N]) for i in range(2)]
    xo = [Tl(kb, f"xo{i}", [128, N]) for i in range(2)]
    it = 0
    oi = 0
    for tb in range(T // N):
        tsl = slice(tb * N, (tb + 1) * N)
        for chalf in range(2):
            banks = [ps.next() for _ in range(8)]
            for e_ in range(NE):
                b = it % 2
                it += 1
                kb.dma(posb[b][:], posD[e_:e_ + 1, tsl].partition_broadcast(128), writes=rs(posb[b]), eng="sp")
                kb.dma(gateb[b][:], gateD[e_:e_ + 1, tsl].partition_broadcast(128), writes=rs(gateb[b]), eng="act")
                kb.dma(ye[b][:], yD[e_, :, chalf * 1024:(chalf + 1) * 1024].rearrange("(j p) d -> p j d", p=128), writes=rs(ye[b]), eng="sp")
                for j in range(4):
                    kb.op("dve", lambda e, b=b, j=j: e.scalar_tensor_tensor(out=selg[b][j][:], in0=posb[b][:], scalar=scol[:, j:j + 1], in1=gateb[b][:], op0=ALU.is_equal, op1=ALU.mult),
                          reads=rs(posb[b], gateb[b], scol), writes=rs(selg[b][j]))
                for ci in range(8):
                    pt, pr = banks[ci]
                    for j in range(4):
                        kb.op("pe", lambda e, pt=pt, b=b, j=j, ci=ci, e_=e_: e.matmul(pt[:], lhsT=ye[b][:, j, ci * 128:(ci + 1) * 128], rhs=selg[b][j][:], start=(e_ == 0 and j == 0), stop=(e_ == NE - 1 and j == 3)),
                              reads=rs(ye[b], selg[b][j]), writes=[pr], pe_accum=True)
            for ci in range(8):
                c = chalf * 8 + ci
                pt, pr = banks[ci]
                ob = oi % 2
                oi += 1
                kb.dma(xi[ob][:], xmT[c * 128:(c + 1) * 128, tsl], writes=rs(xi[ob]), eng="act")
                kb.op("dve", lambda e, pt=pt, ob=ob: e.tensor_tensor(out=xo[ob][:], in0=pt[:], in1=xi[ob][:], op=ALU.add), reads=[pr] + rs(xi[ob]), writes=rs(xo[ob]))
                kb.dma(xoutT[c * 128:(c + 1) * 128, tsl], xo[ob][:], reads=rs(xo[ob]), eng="pool")
    kb.emit()


def stage_final(nc, xT, g_ap, outT):
    kb = KB(nc)
    KC = D // 128
    N = 512
    ps = PS(kb)
    x = kb.sb("x", [128, KC, N])
    x_r = [kb.res(f"x{k}") for k in range(KC)]
    sq = [Tl(kb, f"sq{i}", [128, N], F32R) for i in range(2)]
    rstd = Tl(kb, "rstd", [128, N])
    gt = Tl(kb, "gt", [128, KC])
    onesf = Tl(kb, "onesf", [128, 128])
    ones = Tl(kb, "ones", [128, 128], F32R)
    o = [Tl(kb, f"o{i}", [128, N]) for i in range(2)]
    kb.op("pool", lambda e: e.memset(onesf[:], 1.0), writes=rs(onesf))
    kb.op("dve", lambda e: e.tensor_copy(out=ones[:], in_=onesf[:]), reads=rs(onesf), writes=rs(ones))
    kb.dma(gt[:], g_ap.rearrange("(k p) -> p k", p=128), writes=rs(gt), allow_slow_non_contiguous=True)
    xv = xT.rearrange("(k p) n -> p k n", p=128)
    ov = outT.rearrange("(k p) n -> p k n", p=128)
    oi = 0
    for s in range(T // N):
        tsl = slice(s * N, (s + 1) * N)
        pt, pr = ps.next()
        for k in range(KC):
            kb.dma(x[:, k, :], xv[:, k, tsl], writes=[x_r[k]], eng=("sp" if k % 2 == 0 else "act"))
            a = k % 2
            kb.op("act", lambda e, a=a, k=k: e.activation(out=sq[a][:], in_=x[:, k, :], func=AF.Square), reads=[x_r[k]], writes=rs(sq[a]))
            kb.op("pe", lambda e, pt=pt, a=a, k=k: e.matmul(pt[:], lhsT=ones[:], rhs=sq[a][:], start=(k == 0), stop=(k == KC - 1)),
                  reads=rs(ones, sq[a]), writes=[pr], pe_accum=True)
        kb.op("act", lambda e, pt=pt: e.activation(out=rstd[:], in_=pt[:], func=AF.Sqrt, scale=1.0 / D, bias=EPS), reads=[pr], writes=rs(rstd))
        kb.op("dve", lambda e: e.reciprocal(out=rstd[:], in_=rstd[:]), writes=rs(rstd))
        for k in range(KC):
            ob = oi % 2
            oi += 1
            kb.op("dve", lambda e, k=k, ob=ob: e.scalar_tensor_tensor(out=o[ob][:], in0=x[:, k, :], scalar=gt[:, k:k + 1], in1=rstd[:], op0=ALU.mult, op1=ALU.mult),
                  reads=[x_r[k]] + rs(gt, rstd), writes=rs(o[ob]))
            kb.dma(ov[:, k, tsl], o[ob][:], reads=rs(o[ob]), eng="pool", is_output=True)
    kb.emit()


def make_consts():
    p = np.arange(128)[:, None] % 64
    t = np.arange(512)[None, :] % 64
    c = {}
    c["m_up"] = (p < t).astype(np.float32)
    c["m_lo"] = (p > t).astype(np.float32)
    c["m_upi"] = (p <= t).astype(np.float32)
    c["m_loi"] = (p >= t).astype(np.float32)
    c["itile"] = (p == t).astype(np.float32)
    c["ident"] = (p == np.arange(64)[None, :]).astype(np.float32)
    c["bones"] = ((np.arange(128)[:, None] // 64) == (np.arange(128)[None, :] // 64)).astype(np.float32)
    cm = np.ones((128, 1024), np.float32)
    cm[:, ::64] = 0.0
    c["cmask"] = cm
    return c


from concourse.bass_utils import run_bass_kernel_spmd

N_CORES = 2
DEPTH = 2


def make_consts_all():
    c = make_consts()
    t = np.arange(T)
    ci = np.zeros((4, T), np.float32)
    for g, w in enumerate((2, 4, 8, 16)):
        h = w // 2
        lo = np.clip(t - h, 0, T)
        hi = np.clip(t + h, 0, T)
        ci[g] = 1.0 / (hi - lo).astype(np.float32)
    c["cnt_inv"] = ci
    c["iota_row"] = np.tile(np.arange(512, dtype=np.float32)[None, :], (128, 1))
    c["slot_col"] = (np.arange(4)[None, :] * 128 + np.arange(128)[:, None]).astype(np.float32)
    c["identf"] = np.eye(128, dtype=np.float32)
    return c


_WSPEC = {
    "norm_mix_g": ([DEPTH, D], F32), "w_in": ([DEPTH, D, D_IN], F32R), "mu_shift": ([DEPTH, 3488], F32),
    "w0": ([DEPTH, 2, 1024], F32), "w_lora_up": ([DEPTH, 2, 64, 1024], F32R), "a0": ([DEPTH, 2, 1024], F32),
    "a_lora_up": ([DEPTH, 2, 64, 1024], F32R), "g_lora_up": ([DEPTH, 160, 1024], F32R), "k_k": ([DEPTH, 1024], F32),
    "k_a": ([DEPTH, 1024], F32), "r_k": ([DEPTH, 1024], F32), "lnx_g": ([DEPTH, 1024], F32), "lnx_b": ([DEPTH, 1024], F32),
    "pool_w": ([DEPTH, 4, 256, 256], F32R), "pool_scale": ([DEPTH, 1024], F32), "w_up_a": ([DEPTH, 1024, D], F32R),
    "w_up_b": ([DEPTH, 1024, D], F32R), "w_o": ([DEPTH, D, D], F32R), "norm_moe_g": ([DEPTH, D], F32),
    "w_router": ([DEPTH, D, NE], F32), "w_gate_e": ([DEPTH, NE, D, FF], F32R), "w_up_e": ([DEPTH, NE, D, FF], F32R),
    "w_down_e": ([DEPTH, NE, FF, D], F32R), "final_g": ([D], F32),
}


def build_program(C):
    nc = bass.Bass("TRN2", target_bir_lowering=False)
    nc.dge_precook = False

    def din(name, shape, dt=F32):
        return nc.dram_tensor(name, list(shape), dt, kind="ExternalInput").ap()

    def dint(name, shape, dt=F32):
        return nc.dram_tensor(name, list(shape), dt, kind="Internal").ap()

    xT = din("xT", [D, T])
    Wt = {k: din(k, shp, dt) for k, (shp, dt) in _WSPEC.items()}
    cst = {k: din("c_" + k, v.shape) for k, v in C.items()}
    outT = nc.dram_tensor("outT", [D, T], F32, kind="ExternalOutput").ap()
    projT = dint("projT", [D_IN, T])
    loraA = dint("loraA", [416, T], F32R)
    yfT = dint("yfT", [1024, T])
    yaT = dint("yaT", [1024, T], F32R)
    ybT = dint("ybT", [1024, T], F32R)
    xmT = dint("xmT", [D, T])
    v_tm = dint("v_tm", [T, D], BF16)
    affT = dint("affT", [NE, T])
    posD = dint("posD", [NE, T])
    gateD = dint("gateD", [NE, T])
    yD = dint("yD", [NE, CAP, D], BF16)
    xs = [xT, dint("xA", [D, T]), dint("xB", [D, T])]
    for l in range(DEPTH):
        xin = xs[l]
        xout = xs[l + 1]
        stage_inproj(nc, xin, Wt["norm_mix_g"][l], Wt["w_in"][l], projT)
        stage_lora(nc, projT, Wt["mu_shift"][l], loraA)
        W = {"mu": Wt["mu_shift"][l], "w0": Wt["w0"][l], "wl": Wt["w_lora_up"][l], "a0": Wt["a0"][l], "al": Wt["a_lora_up"][l],
             "gl": Wt["g_lora_up"][l], "k_k": Wt["k_k"][l], "k_a": Wt["k_a"][l], "r_k": Wt["r_k"][l], "lnx_g": Wt["lnx_g"][l],
             "lnx_b": Wt["lnx_b"][l]}
        for hp in range(8):
            stage_rwkv(nc, projT, loraA, yfT, yaT, W, cst, hp_list=[hp])
        stage_pool(nc, projT, Wt["pool_w"][l], Wt["pool_scale"][l], ybT, cst["cnt_inv"])
        stage_mix(nc, xin, projT, yaT, ybT, Wt["w_up_a"][l], Wt["w_up_b"][l], Wt["w_o"][l], Wt["norm_moe_g"][l], Wt["w_router"][l],
                  xmT, v_tm, affT, cst["identf"])
        stage_route(nc, affT, posD, gateD)
        stage_experts(nc, posD, v_tm, Wt["w_gate_e"][l], Wt["w_up_e"][l], Wt["w_down_e"][l], yD, cst["iota_row"])
        stage_combine(nc, posD, gateD, yD, xmT, xout, cst["slot_col"])
    stage_final(nc, xs[DEPTH], Wt["final_g"], outT)
    return nc


def kernel(**inputs):
    C = make_consts_all()
    nc = build_program(C)
    x = np.asarray(inputs["x"], dtype=np.float32)
    in_maps = []
    for b in range(N_CORES):
        m = {"xT": np.ascontiguousarray(x[b].T)}
        for k in _WSPEC:
            a = np.asarray(inputs[k], dtype=np.float32)
            if k == "r_k":
                a = a.reshape(DEPTH, 1024)
            m[k] = np.ascontiguousarray(a)
        for k, v in C.items():
            m["c_" + k] = v
        in_maps.append(m)
    res = run_bass_kernel_spmd(nc, in_maps, core_ids=list(range(N_CORES)))
    out = np.stack([np.ascontiguousarray(np.asarray(res.results[b]["outT"]).T) for b in range(N_CORES)], axis=0)
    return out.astype(np.float32)
```

```python
from contextlib import ExitStack
import concourse.bass as bass
import concourse.mybir as mybir

F32 = mybir.dt.float32
F32R = mybir.dt.float32r
BF16 = mybir.dt.bfloat16
I32 = mybir.dt.int32
U32 = mybir.dt.uint32
AF = mybir.ActivationFunctionType
ALU = mybir.AluOpType
AX = mybir.AxisListType

ENGS = ("pe", "dve", "act", "pool", "sp")
N_DMA_SEMS = 48


class Res:
    __slots__ = ("name", "w", "r")

    def __init__(self, name):
        self.name = name
        self.w = None
        self.r = []


class Instr:
    __slots__ = ("eng", "fn", "deps", "stream", "seq", "waited", "val", "is_dma", "known")

    def __init__(self, eng, fn):
        self.eng = eng
        self.fn = fn
        self.deps = []
        self.stream = None
        self.seq = 0
        self.waited = False
        self.val = 0
        self.is_dma = False
        self.known = None


class KB:
    _count = 0

    def __init__(self, nc, same_engine_sync=True):
        KB._count += 1
        self.tag = f"k{KB._count}_"
        self.nc = nc
        self.es = ExitStack()
        self.q = {e: [] for e in ENGS}
        self.stream_last = {}
        self.stream_cnt = {}
        self.dma_rr = 0
        self.same_engine_sync = same_engine_sync
        self.n_res = 0
        self.out_dmas = []

    def sb(self, name, shape, dtype=F32):
        return self.es.enter_context(self.nc.sbuf_tensor(self.tag + name, list(shape), dtype))

    def ps(self, name, shape, dtype=F32):
        return self.es.enter_context(self.nc.psum_tensor(self.tag + name, list(shape), dtype))

    def res(self, name=None):
        self.n_res += 1
        return Res(name or f"r{self.n_res}")

    def _known_after(self, d):
        k = dict(d.known)
        if k.get(d.stream, 0) < d.seq:
            k[d.stream] = d.seq
        return k

    def _add(self, ins, reads, writes, pe_accum=False):
        eng = ins.eng
        deps = []
        for r in reads:
            if r.w is not None:
                deps.append(r.w)
        for w in writes:
            if w.w is not None:
                if not (pe_accum and w.w.eng == "pe" and not w.w.is_dma):
                    deps.append(w.w)
            deps.extend(w.r)
        prev = self.q[eng][-1] if self.q[eng] else None
        known = dict(prev.known) if prev is not None else {}
        if ins.is_dma:
            p = self.stream_last.get(ins.stream)
            if p is not None:
                deps.append(p)
        final = []
        deps = sorted(set(deps), key=lambda d: -d.seq)
        for d in deps:
            if d is ins:
                continue
            if (not d.is_dma) and d.eng == eng and not self.same_engine_sync:
                continue
            if known.get(d.stream, 0) >= d.seq:
                continue
            final.append(d)
            d.waited = True
            ka = self._known_after(d)
            for s, v in ka.items():
                if known.get(s, 0) < v:
                    known[s] = v
        ins.deps = final
        ins.known = known
        self.q[eng].append(ins)
        for r in reads:
            r.r.append(ins)
        for w in writes:
            w.w = ins
            w.r = []
        return ins

    def op(self, eng, fn, reads=(), writes=(), pe_accum=False):
        if eng == "pool":
            eng = "dve"
        ins = Instr(eng, fn)
        ins.stream = eng
        self.stream_cnt[eng] = self.stream_cnt.get(eng, 0) + 1
        ins.seq = self.stream_cnt[eng]
        self._add(ins, list(reads), list(writes), pe_accum=pe_accum)
        self.stream_last[eng] = ins
        return ins

    def dma(self, out, in_, reads=(), writes=(), eng="sp", is_output=False, **kw):
        if eng == "pool" or getattr(self, "force_sp", False):
            eng = "sp"
        fn = lambda e: e.dma_start(out=out, in_=in_, **kw)
        ins = Instr(eng, fn)
        ins.is_dma = True
        slot = self.dma_rr % N_DMA_SEMS
        self.dma_rr += 1
        ins.stream = ("dma", slot)
        self.stream_cnt[ins.stream] = self.stream_cnt.get(ins.stream, 0) + 1
        ins.seq = self.stream_cnt[ins.stream]
        ins.waited = True
        self._add(ins, list(reads), list(writes))
        self.stream_last[ins.stream] = ins
        if is_output:
            self.out_dmas.append(ins)
        return ins

    def gen(self, eng, fn, reads=(), writes=()):
        return self.op(eng, fn, reads, writes)

    def emit(self):
        nc = self.nc
        lastd = [v for k, v in self.stream_last.items() if isinstance(k, tuple)]
        if lastd:
            self.out_dmas = lastd
            fin = Instr("sp", None)
            fin.stream = "sp"
            self.stream_cnt["sp"] = self.stream_cnt.get("sp", 0) + 1
            fin.seq = self.stream_cnt["sp"]
            prev = self.q["sp"][-1] if self.q["sp"] else None
            known = dict(prev.known) if prev is not None else {}
            for d in self.out_dmas:
                if known.get(d.stream, 0) < d.seq:
                    fin.deps.append(d)
            fin.known = known
            self.q["sp"].append(fin)
        sems = {}
        for e in ENGS:
            sems[e] = self.es.enter_context(nc.semaphore(self.tag + f"s_{e}"))
            v = 0
            for ins in self.q[e]:
                if ins.is_dma:
                    continue
                if ins.waited:
                    v += 1
                    ins.val = v
        for s in range(N_DMA_SEMS):
            sems[("dma", s)] = self.es.enter_context(nc.semaphore(self.tag + f"s_dma{s}"))
        engmap = {"pe": "tensor", "dve": "vector", "act": "scalar", "pool": "gpsimd", "sp": "sync"}
        q = self.q

        def run(ename, e):
            for ins in q[ename]:
                for d in ins.deps:
                    if d.is_dma:
                        e.wait_ge(sems[d.stream], 16 * d.seq)
                    else:
                        e.wait_ge(sems[d.stream], d.val)
                if ins.fn is None:
                    continue
                r = ins.fn(e)
                if ins.is_dma:
                    r.then_inc(sems[ins.stream], 16)
                elif ins.waited:
                    r.then_inc(sems[ins.stream], 1)

        allsems = list(sems.values())
        with nc.Block() as cblock:
            @cblock.gpsimd
            def _(e):
                for sm in allsems:
                    e.sem_clear(sm)

        with nc.Block() as block:
            @block.tensor
            def _(e):
                run("pe", e)

            @block.vector
            def _(e):
                run("dve", e)

            @block.scalar
            def _(e):
                run("act", e)

            @block.gpsimd
            def _(e):
                run("pool", e)

            @block.sync
            def _(e):
                run("sp", e)
        self.es.close()

    def stats(self):
        return {e: len(self.q[e]) for e in ENGS}


import numpy as np
import concourse.bass as bass
import concourse.mybir as mybir

D = 2048
T = 4096
D_IN = 8608
D_SHIFT = 3488
EPS = 1e-6
NE = 16
FF = 2816
CAP = 512


class Tl:
    def __init__(self, kb, name, shape, dtype=F32):
        self.t = kb.sb(name, shape, dtype)
        self.r = kb.res(name)

    def __getitem__(self, k):
        return self.t[k]


class PS:
    def __init__(self, kb, n=8):
        self.kb = kb
        self.t = [kb.ps(f"ps{i}", [128, 512]) for i in range(n)]
        self.r = [kb.res(f"ps{i}") for i in range(n)]
        self.i = 0
        self.n = n

    def next(self):
        i = self.i % self.n
        self.i += 1
        return self.t[i], self.r[i]


def rs(*tiles):
    return [x.r if hasattr(x, "r") and not isinstance(x, Res) else x for x in tiles]


def col_chunks():
    ch = [(i * 128, 128) for i in range(27)] + [(3456, 32)] + [(3488 + i * 128, 128) for i in range(40)]
    return ch


def stage_inproj(nc, xsrc, g_ap, w_ap, projT):
    kb = KB(nc)
    KC = D // 128
    NT = 1024
    ut = kb.sb("ut", [128, KC, NT], F32R)
    ut_r = [kb.res(f"ut{k}") for k in range(KC)]
    xa = [Tl(kb, f"xa{i}", [128, NT]) for i in range(2)]
    gt = Tl(kb, "gt", [128, KC])
    onesf = Tl(kb, "onesf", [128, 128])
    ones = Tl(kb, "ones", [128, 128], F32R)
    sq = [Tl(kb, f"sq{i}", [128, NT], F32R) for i in range(2)]
    rstd = Tl(kb, "rstd", [128, NT])
    wt = [Tl(kb, f"wt{i}", [128, KC, 512], F32R) for i in range(2)]
    ot = [Tl(kb, f"ot{i}", [128, NT]) for i in range(2)]
    ps = PS(kb)
    kb.op("pool", lambda e: e.memset(onesf[:], 1.0), writes=rs(onesf))
    kb.op("dve", lambda e: e.tensor_copy(out=ones[:], in_=onesf[:]), reads=rs(onesf), writes=rs(ones))
    kb.dma(gt[:], g_ap.rearrange("(k p) -> p k", p=128), writes=rs(gt), eng="sp", allow_slow_non_contiguous=True)
    xv = xsrc.rearrange("(k p) n -> p k n", p=128)
    wv = w_ap.rearrange("(k p) n -> p k n", p=128)
    chunks = col_chunks()
    groups = [chunks[i:i + 4] for i in range(0, len(chunks), 4)]
    gi_glob = 0
    ci = 0
    for s in range(T // NT):
        tsl = slice(s * NT, (s + 1) * NT)
        p0t, p0r = ps.next()
        p1t, p1r = ps.next()
        pss = [(p0t, p0r), (p1t, p1r)]
        for k in range(KC):
            a = k % 2
            kb.dma(xa[a][:], xv[:, k, tsl], writes=rs(xa[a]), eng="sp")
            kb.op("act", lambda e, a=a: e.activation(out=sq[a][:], in_=xa[a][:], func=AF.Square),
                  reads=rs(xa[a]), writes=rs(sq[a]))
            for h in range(2):
                kb.op("pe", lambda e, k=k, a=a, h=h, pt=pss[h][0]: e.matmul(pt[:], lhsT=ones[:], rhs=sq[a][:, h * 512:(h + 1) * 512],
                                                                        start=(k == 0), stop=(k == KC - 1)),
                      reads=rs(ones, sq[a]), writes=[pss[h][1]], pe_accum=True)
        for h in range(2):
            sl = slice(h * 512, (h + 1) * 512)
            kb.op("act", lambda e, h=h, sl=sl, pt=pss[h][0]: e.activation(out=rstd[:, sl], in_=pt[:], func=AF.Sqrt, scale=1.0 / D, bias=EPS),
                  reads=[pss[h][1]], writes=rs(rstd))
        kb.op("dve", lambda e: e.reciprocal(out=rstd[:], in_=rstd[:]), writes=rs(rstd))
        for k in range(KC):
            a = k % 2
            kb.dma(xa[a][:], xv[:, k, tsl], writes=rs(xa[a]), eng="sp")
            kb.op("dve", lambda e, k=k, a=a: e.scalar_tensor_tensor(out=ut[:, k, :], in0=xa[a][:], scalar=gt[:, k:k + 1], in1=rstd[:],
                                                                op0=ALU.mult, op1=ALU.mult),
                  reads=rs(gt, rstd, xa[a]), writes=[ut_r[k]])
        for grp in groups:
            b = gi_glob % 2
            c0 = grp[0][0]
            c1 = grp[-1][0] + grp[-1][1]
            kb.dma(wt[b][:, :, 0:c1 - c0], wv[:, :, c0:c1], writes=rs(wt[b]), eng=("sp" if gi_glob % 2 == 0 else "act"))
            gi_glob += 1
            for (cs, cw) in grp:
                ob = ci % 2
                for h in range(2):
                    pt, pr = ps.next()
                    for k in range(KC):
                        if cw == 128:
                            lhsT = wt[b][:, k, cs - c0:cs - c0 + cw]
                            rhs = ut[:, k, h * 512:(h + 1) * 512]
                        else:
                            lhsT = wt[b][:, k, cs - c0:cs - c0 + cw].bitcast(F32)
                            rhs = ut[:, k, h * 512:(h + 1) * 512].bitcast(F32)
                        kb.op("pe", lambda e, pt=pt, lhsT=lhsT, rhs=rhs, k=k, cw=cw: e.matmul(pt[0:cw, :], lhsT=lhsT, rhs=rhs,
                                                                                       start=(k == 0), stop=(k == KC - 1)),
                              reads=[wt[b].r, ut_r[k]], writes=[pr], pe_accum=True)
                    sl = slice(h * 512, (h + 1) * 512)
                    if cs >= 4512:
                        kb.op("act", lambda e, ob=ob, pt=pt, sl=sl, cw=cw: e.activation(out=ot[ob][0:cw, sl], in_=pt[0:cw, :], func=AF.Sigmoid),
                              reads=[pr], writes=rs(ot[ob]))
                    elif h == 0:
                        kb.op("act", lambda e, ob=ob, pt=pt, sl=sl, cw=cw: e.copy(out=ot[ob][0:cw, sl], in_=pt[0:cw, :]),
                              reads=[pr], writes=rs(ot[ob]))
                    else:
                        kb.op("dve", lambda e, ob=ob, pt=pt, sl=sl, cw=cw: e.tensor_copy(out=ot[ob][0:cw, sl], in_=pt[0:cw, :]),
                              reads=[pr], writes=rs(ot[ob]))
                kb.dma(projT[cs:cs + cw, tsl], ot[ob][0:cw, :], reads=rs(ot[ob]), eng="pool", is_output=True)
                ci += 1
    kb.emit()


def load_col(kb, tile, ap1d, eng="sp"):
    n = ap1d.shape[0]
    kb.dma(tile[0:n, 0:1], ap1d.rearrange("(p o) -> p o", o=1), writes=rs(tile), eng=eng)


def load_halo(kb, P, src_rows, t0, S, eng="sp"):
    n = src_rows.shape[0]
    lo = max(t0 - 1, 0)
    hi = min(t0 + S + 1, T)
    kb.dma(P[0:n, lo - (t0 - 1):hi - (t0 - 1)], src_rows[:, lo:hi], writes=rs(P), eng=eng)
    if t0 == 0:
        kb.op("pool", lambda e: e.memset(P[0:n, 0:1], 0.0), writes=rs(P))
    if t0 + S == T:
        kb.op("pool", lambda e: e.memset(P[0:n, S + 1:S + 2], 0.0), writes=rs(P))


def shift_rows(kb, out_ap, P, tmp, hmu, omu, n, S, out_res):
    kb.op("pool", lambda e: e.tensor_tensor(out=tmp[0:n, :], in0=P[0:n, 0:S], in1=P[0:n, 2:S + 2], op=ALU.add),
          reads=rs(P), writes=rs(tmp))
    kb.op("pool", lambda e: e.tensor_scalar(out=tmp[0:n, :], in0=tmp[0:n, :], scalar1=hmu[0:n, 0:1], scalar2=None, op0=ALU.mult),
          reads=rs(hmu), writes=rs(tmp))
    kb.op("dve", lambda e: e.scalar_tensor_tensor(out=out_ap, in0=P[0:n, 1:S + 1], scalar=omu[0:n, 0:1], in1=tmp[0:n, :],
                                                  op0=ALU.mult, op1=ALU.add),
          reads=rs(P, omu, tmp), writes=[out_res])


def mu_cols(kb, mu, hmu, omu, ap1d, n):
    load_col(kb, mu, ap1d)
    kb.op("dve", lambda e: e.tensor_scalar(out=hmu[0:n, :], in0=mu[0:n, :], scalar1=0.5, scalar2=None, op0=ALU.mult),
          reads=rs(mu), writes=rs(hmu))
    kb.op("dve", lambda e: e.tensor_scalar(out=omu[0:n, :], in0=mu[0:n, :], scalar1=-1.0, scalar2=1.0, op0=ALU.mult, op1=ALU.add),
          reads=rs(mu), writes=rs(omu))


def stage_lora(nc, projT, mu_ap, loraA):
    kb = KB(nc)
    S = 1024
    P = [Tl(kb, f"P{i}", [128, S + 2]) for i in range(2)]
    tmp = Tl(kb, "tmp", [128, S])
    sh = Tl(kb, "sh", [128, S])
    o = [Tl(kb, f"o{i}", [128, S], F32R) for i in range(2)]
    mu = [Tl(kb, f"mu{i}", [128, 1]) for i in range(4)]
    hmu = [Tl(kb, f"hmu{i}", [128, 1]) for i in range(4)]
    omu = [Tl(kb, f"omu{i}", [128, 1]) for i in range(4)]
    blocks = [(0, 128, AF.Tanh), (128, 128, AF.Copy), (256, 128, AF.Sigmoid), (384, 32, AF.Sigmoid)]
    for bi, (r0, n, fn) in enumerate(blocks):
        mu_cols(kb, mu[bi], hmu[bi], omu[bi], mu_ap[3072 + r0:3072 + r0 + n], n)
    it = 0
    for s in range(T // S):
        t0 = s * S
        for bi, (r0, n, fn) in enumerate(blocks):
            p = P[it % 2]
            oo = o[it % 2]
            it += 1
            load_halo(kb, p, projT[3072 + r0:3072 + r0 + n, :], t0, S)
            shift_rows(kb, sh[0:n, :], p, tmp, hmu[bi], omu[bi], n, S, sh.r)
            if fn == AF.Copy:
                kb.op("act", lambda e, oo=oo, n=n: e.copy(out=oo[0:n, :], in_=sh[0:n, :]), reads=rs(sh), writes=rs(oo))
            else:
                kb.op("act", lambda e, oo=oo, n=n, fn=fn: e.activation(out=oo[0:n, :], in_=sh[0:n, :], func=fn), reads=rs(sh), writes=rs(oo))
            kb.dma(loraA[r0:r0 + n, t0:t0 + S], oo[0:n, :], reads=rs(oo), eng="act")
    kb.emit()


CEXP = float(np.exp(-0.5))


def stage_rwkv(nc, projT, loraA, yfT, yaT, W, cst, hp_list=range(8), dbg=None):
    kb = KB(nc)
    S = 1024
    NCH = S // 64
    ps = PS(kb)
    cf = {k: Tl(kb, "c_" + k, [128, 512], BF16) for k in ("m_up", "m_lo", "m_upi", "m_loi", "itile")}
    cstage = Tl(kb, "cstage", [128, 512])
    for k in cf:
        kb.dma(cstage[:], cst[k], writes=rs(cstage))
        kb.op("dve", lambda e, k=k: e.tensor_copy(out=cf[k][:], in_=cstage[:]), reads=rs(cstage), writes=rs(cf[k]))
    identf = Tl(kb, "identf", [128, 64])
    ident = Tl(kb, "ident", [128, 64], BF16)
    kb.dma(identf[:], cst["ident"], writes=rs(identf))
    kb.op("dve", lambda e: e.tensor_copy(out=ident[:], in_=identf[:]), reads=rs(identf), writes=rs(ident))
    bonesf = Tl(kb, "bonesf", [128, 128])
    bones = Tl(kb, "bones", [128, 128], F32R)
    kb.dma(bonesf[:], cst["bones"], writes=rs(bonesf))
    kb.op("dve", lambda e: e.tensor_copy(out=bones[:], in_=bonesf[:]), reads=rs(bonesf), writes=rs(bones))
    cmask = Tl(kb, "cmask", [128, S])
    kb.dma(cmask[:], cst["cmask"], writes=rs(cmask))
    colnames = ["mu_r", "mu_k", "mu_v", "w0f", "w0b", "a0f", "a0b", "k_k", "k_a", "r_k", "lnx_g", "lnx_b"]
    col = {n: Tl(kb, "col_" + n, [128, 1]) for n in colnames}
    hmu = {n: Tl(kb, "h" + n, [128, 1]) for n in ("mu_r", "mu_k", "mu_v")}
    omu = {n: Tl(kb, "o" + n, [128, 1]) for n in ("mu_r", "mu_k", "mu_v")}
    omka = Tl(kb, "omka", [128, 1])
    hka = Tl(kb, "hka", [128, 1])
    zf = Tl(kb, "zf", [128, 128])
    kb.op("dve", lambda e: e.memset(zf[:], 0.0), writes=rs(zf))
    wlp = [Tl(kb, f"wlp{i}", [128, 128], F32R) for i in range(2)]
    alp = [Tl(kb, f"alp{i}", [128, 128], F32R) for i in range(2)]
    gl0 = Tl(kb, "gl0", [128, 128], F32R)
    gl1 = Tl(kb, "gl1", [128, 128], F32R)
    for i in range(2):
        oh = slice(64, 128) if i == 0 else slice(0, 64)
        kb.op("dve", lambda e, i=i, oh=oh: e.tensor_copy(out=wlp[i][oh, :], in_=zf[oh, :]), reads=rs(zf), writes=rs(wlp[i]))
        kb.op("dve", lambda e, i=i, oh=oh: e.tensor_copy(out=alp[i][oh, :], in_=zf[oh, :]), reads=rs(zf), writes=rs(alp[i]))
    for (pa_, pb_) in ((32, 64), (64, 128)):
        kb.op("dve", lambda e, pa_=pa_, pb_=pb_: e.tensor_copy(out=gl1[pa_:pb_, :], in_=zf[pa_:pb_, :]), reads=rs(zf), writes=rs(gl1))
    P2 = [Tl(kb, n, [128, S + 2]) for n in ("P2a", "P2b")]
    Pr, Pk, Pv = P2[0], P2[1], P2[0]
    names32 = ["r_", "k_", "v_", "tA", "tB", "sgw", "a_", "kk", "kkn", "b_", "Pp", "E_", "R_", "Sf", "yc", "po1", "po2", "po3"]
    F = {n: Tl(kb, n, [128, S]) for n in names32}
    F["lw"] = F["sgw"]
    F["kdir"] = F["kk"]
    F["yf"] = F["Sf"]
    F["af"] = F["E_"]
    sqr = Tl(kb, "sqr", [128, S], F32R)
    tw = Tl(kb, "tw", [128, S], F32R)
    adp = Tl(kb, "adp", [128, S], F32R)
    sg0 = Tl(kb, "sg0", [128, S], F32R)
    sg1 = Tl(kb, "sg1", [128, S], F32R)
    for q in range(S // 128):
        for (pa_, pb_) in ((32, 64), (64, 128)):
            kb.op("dve", lambda e, q=q, pa_=pa_, pb_=pb_: e.tensor_copy(out=sg1[pa_:pb_, q * 128:(q + 1) * 128], in_=zf[pa_:pb_, :]), reads=rs(zf), writes=rs(sg1))
    yout = sqr
    tot = Tl(kb, "tot", [128, NCH])
    wC = Tl(kb, "wC", [128, NCH])
    namesb = ["qt", "rt", "kt", "nbt", "Kh", "Bn", "vb"]
    Bf = {n: Tl(kb, n, [128, S], BF16) for n in namesb}
    pb_names = ["Vtm", "Qtm", "Khtm", "Bhtm", "Um", "Utm", "Pa", "Pb", "Pta", "Ptb", "Xa", "Xb", "AkkT", "ArkT", "nArbT", "AV", "Gtm", "Qh", "PhiT", "RhT"]
    PB = [{n: Tl(kb, f"{n}{bi}", [128, 512], BF16) for n in pb_names} for bi in range(2)]
    Psi = [Tl(kb, f"Psi{bi}", [128, 512]) for bi in range(2)]
    Hbf = Tl(kb, "Hbf", [128, 64], BF16)

    def ew(eng, fname, reads, writes, **kw):
        kb.op(eng, lambda e: getattr(e, fname)(**kw), reads=rs(*reads), writes=rs(*writes))

    def psbf(pt):
        return pt[:].bitcast(BF16)

    HS = [slice(0, 64), slice(64, 128)]

    for hp in hp_list:
        c0 = hp * 128
        csl = slice(c0, c0 + 128)
        load_col(kb, col["mu_r"], W["mu"][c0:c0 + 128])
        load_col(kb, col["mu_k"], W["mu"][1024 + c0:1024 + c0 + 128])
        load_col(kb, col["mu_v"], W["mu"][2048 + c0:2048 + c0 + 128])
        load_col(kb, col["w0f"], W["w0"][0, csl])
        load_col(kb, col["w0b"], W["w0"][1, csl])
        load_col(kb, col["a0f"], W["a0"][0, csl])
        load_col(kb, col["a0b"], W["a0"][1, csl])
        for n in ("k_k", "k_a", "r_k", "lnx_g", "lnx_b"):
            load_col(kb, col[n], W[n][csl])
        for n in ("mu_r", "mu_k", "mu_v"):
            ew("dve", "tensor_scalar", [col[n]], [hmu[n]], out=hmu[n][:], in0=col[n][:], scalar1=0.5, scalar2=None, op0=ALU.mult)
            ew("dve", "tensor_scalar", [col[n]], [omu[n]], out=omu[n][:], in0=col[n][:], scalar1=-1.0, scalar2=1.0, op0=ALU.mult, op1=ALU.add)
        ew("dve", "tensor_scalar", [col["k_a"]], [omka], out=omka[:], in0=col["k_a"][:], scalar1=-1.0, scalar2=1.0, op0=ALU.mult, op1=ALU.add)
        ew("dve", "tensor_scalar", [col["k_a"]], [hka], out=hka[:], in0=col["k_a"][:], scalar1=0.5, scalar2=None, op0=ALU.mult)
        for i in range(2):
            kb.dma(wlp[i][HS[i], :], W["wl"][i, :, csl], writes=rs(wlp[i]))
            kb.dma(alp[i][HS[i], :], W["al"][i, :, csl], writes=rs(alp[i]))
        kb.dma(gl0[:], W["gl"][0:128, csl], writes=rs(gl0))
        kb.dma(gl1[0:32, :], W["gl"][128:160, csl], writes=rs(gl1))
        for d in range(2):
            ds = HS[d]
            ew("pool", "memset", [], [Hbf], ap=Hbf[:], constant=0.0)
            slabs = list(range(T // S))
            if d == 1:
                slabs = slabs[::-1]
            for s in slabs:
                t0 = s * S
                tsl = slice(t0, t0 + S)
                load_halo(kb, Pr, projT[c0:c0 + 128, :], t0, S, eng="sp")
                load_halo(kb, Pk, projT[1024 + c0:1024 + c0 + 128, :], t0, S, eng="act")
                kb.dma(tw[:], loraA[0:128, tsl], writes=rs(tw), eng="act")
                kb.dma(adp[:], loraA[128:256, tsl], writes=rs(adp), eng="sp")
                shift_rows(kb, F["r_"][:], Pr, F["tA"], hmu["mu_r"], omu["mu_r"], 128, S, F["r_"].r)
                load_halo(kb, Pv, projT[2048 + c0:2048 + c0 + 128, :], t0, S, eng="sp")
                shift_rows(kb, F["k_"][:], Pk, F["tA"], hmu["mu_k"], omu["mu_k"], 128, S, F["k_"].r)
                shift_rows(kb, F["v_"][:], Pv, F["tA"], hmu["mu_v"], omu["mu_v"], 128, S, F["v_"].r)
                w0c = col["w0f"] if d == 0 else col["w0b"]
                a0c = col["a0f"] if d == 0 else col["a0b"]
                for hf in range(2):
                    sl = slice(hf * 512, (hf + 1) * 512)
                    pt, pr = ps.next()
                    kb.op("pe", lambda e, pt=pt, sl=sl, d=d: e.matmul(pt[:], lhsT=wlp[d][:], rhs=tw[:, sl], start=True, stop=True),
                          reads=rs(wlp[d], tw), writes=[pr])
                    kb.op("act", lambda e, pt=pt, sl=sl, w0c=w0c: e.activation(out=F["sgw"][:, sl], in_=pt[:], func=AF.Sigmoid, bias=w0c[:, 0:1]),
                          reads=[pr] + rs(w0c), writes=rs(F["sgw"]))
                    pt, pr = ps.next()
                    kb.op("pe", lambda e, pt=pt, sl=sl, d=d: e.matmul(pt[:], lhsT=alp[d][:], rhs=adp[:, sl], start=True, stop=True),
                          reads=rs(alp[d], adp), writes=[pr])
                    kb.op("act", lambda e, pt=pt, sl=sl, a0c=a0c: e.activation(out=F["a_"][:, sl], in_=pt[:], func=AF.Sigmoid, bias=a0c[:, 0:1]),
                          reads=[pr] + rs(a0c), writes=rs(F["a_"]))
                ew("dve", "tensor_scalar", [F["k_"], col["k_k"]], [F["kk"]], out=F["kk"][:], in0=F["k_"][:], scalar1=col["k_k"][:, 0:1], scalar2=None, op0=ALU.mult)
                ew("act", "activation", [F["kk"]], [sqr], out=sqr[:], in_=F["kk"][:], func=AF.Square)
                for hf in range(2):
                    sl = slice(hf * 512, (hf + 1) * 512)
                    pt, pr = ps.next()
                    kb.op("pe", lambda e, pt=pt, sl=sl: e.matmul(pt[:], lhsT=bones[:], rhs=sqr[:, sl], start=True, stop=True),
                          reads=rs(bones, sqr), writes=[pr])
                    kb.op("act", lambda e, pt=pt, sl=sl: e.activation(out=F["tB"][:, sl], in_=pt[:], func=AF.Sqrt),
                          reads=[pr], writes=rs(F["tB"]))
                ew("dve", "tensor_scalar", [F["tB"]], [F["tB"]], out=F["tB"][:], in0=F["tB"][:], scalar1=1e-12, scalar2=None, op0=ALU.max)
                ew("dve", "reciprocal", [F["tB"]], [F["tB"]], out=F["tB"][:], in_=F["tB"][:])
                ew("dve", "tensor_tensor", [F["kk"], F["tB"]], [F["kkn"]], out=F["kkn"][:], in0=F["kk"][:], in1=F["tB"][:], op=ALU.mult)
                ew("dve", "tensor_scalar", [F["a_"], col["k_a"], omka], [F["tB"]], out=F["tB"][:], in0=F["a_"][:], scalar1=col["k_a"][:, 0:1], scalar2=omka[:, 0:1], op0=ALU.mult, op1=ALU.add)
                ew("dve", "tensor_tensor", [F["k_"], F["tB"]], [F["kdir"]], out=F["kdir"][:], in0=F["k_"][:], in1=F["tB"][:], op=ALU.mult)
                ew("pool", "tensor_tensor", [F["kkn"], F["a_"]], [F["b_"]], out=F["b_"][:], in0=F["kkn"][:], in1=F["a_"][:], op=ALU.mult)
                ew("pool", "tensor_scalar", [F["sgw"]], [F["lw"]], out=F["lw"][:], in0=F["sgw"][:], scalar1=-CEXP, scalar2=None, op0=ALU.mult)
                ew("dve", "tensor_tensor_scan", [cmask, F["lw"]], [F["Pp"]], out=F["Pp"][:], data0=cmask[:], data1=F["lw"][:], initial=0.0, op0=ALU.mult, op1=ALU.add)
                ew("pool", "tensor_tensor", [F["Pp"], F["lw"]], [F["E_"]], out=F["E_"][:], in0=F["Pp"][:], in1=F["lw"][:], op=ALU.subtract)
                Pv3 = F["Pp"][:].rearrange("p (c t) -> p c t", t=64)
                ew("dve", "tensor_copy", [F["Pp"]], [tot], out=tot[:], in_=Pv3[:, :, 63])
                ew("dve", "tensor_tensor", [tot, F["Pp"]], [F["R_"]], out=F["R_"][:].rearrange("p (c t) -> p c t", t=64),
                   in0=tot[:].unsqueeze(2).to_broadcast([128, NCH, 64]), in1=Pv3, op=ALU.subtract)
                ew("act", "activation", [tot], [wC], out=wC[:], in_=tot[:], func=AF.Exp)
                if d == 0:
                    Lc, Lp, Lr = F["Pp"], F["E_"], F["R_"]
                else:
                    ew("dve", "tensor_tensor", [tot, F["E_"]], [F["Sf"]], out=F["Sf"][:].rearrange("p (c t) -> p c t", t=64),
                       in0=tot[:].unsqueeze(2).to_broadcast([128, NCH, 64]), in1=F["E_"][:].rearrange("p (c t) -> p c t", t=64), op=ALU.subtract)
                    Lc, Lp, Lr = F["Sf"], F["R_"], F["E_"]
                ew("act", "activation", [Lc], [F["po1"]], out=F["po1"][:], in_=Lc[:], func=AF.Exp)
                ew("act", "activation", [Lp], [F["po2"]], out=F["po2"][:], in_=Lp[:], func=AF.Exp)
                ew("act", "activation", [Lc], [F["po3"]], out=F["po3"][:], in_=Lc[:], func=AF.Exp, scale=-1.0)
                ew("act", "activation", [Lr], [F["tA"]], out=F["tA"][:], in_=Lr[:], func=AF.Exp)
                ew("dve", "tensor_tensor", [F["kkn"], F["po2"]], [Bf["qt"]], out=Bf["qt"][:], in0=F["kkn"][:], in1=F["po2"][:], op=ALU.mult)
                ew("pool", "tensor_tensor", [F["r_"], F["po1"]], [Bf["rt"]], out=Bf["rt"][:], in0=F["r_"][:], in1=F["po1"][:], op=ALU.mult)
                ew("dve", "tensor_tensor", [F["kdir"], F["po3"]], [Bf["kt"]], out=Bf["kt"][:], in0=F["kdir"][:], in1=F["po3"][:], op=ALU.mult)
                ew("dve", "scalar_tensor_tensor", [F["b_"], F["po3"]], [Bf["nbt"]], out=Bf["nbt"][:], in0=F["b_"][:], scalar=-1.0, in1=F["po3"][:], op0=ALU.mult, op1=ALU.mult)
                ew("pool", "tensor_tensor", [F["kdir"], F["tA"]], [Bf["Kh"]], out=Bf["Kh"][:], in0=F["kdir"][:], in1=F["tA"][:], op=ALU.mult)
                ew("dve", "scalar_tensor_tensor", [F["b_"], F["tA"]], [Bf["Bn"]], out=Bf["Bn"][:], in0=F["b_"][:], scalar=-1.0, in1=F["tA"][:], op0=ALU.mult, op1=ALU.mult)
                ew("act", "copy", [F["v_"]], [Bf["vb"]], out=Bf["vb"][:], in_=F["v_"][:])
                if dbg is not None and d == 0 and s == 0:
                    for nm in ("r_", "k_", "v_", "a_", "kkn", "kdir", "b_", "lw", "Pp"):
                        kb.dma(dbg[nm], F[nm][:], reads=rs(F[nm]))
                m_s = cf["m_up"] if d == 0 else cf["m_lo"]
                m_st = cf["m_lo"] if d == 0 else cf["m_up"]
                m_i = cf["m_upi"] if d == 0 else cf["m_loi"]
                BI = [0, 1]

                def mm_batch(bi, out_fn, lhs_fn, rhs_fn, reads, n_acc=1, lhs2=None, rhs2=None):
                    pt, pr = ps.next()
                    for g in range(8):
                        for h in range(2):
                            hs = HS[h]
                            gs = slice(64 * g, 64 * g + 64)
                            if lhs2 is None:
                                kb.op("pe", lambda e, pt=pt, hs=hs, gs=gs, g=g: e.matmul(out_fn(pt, hs, gs), lhsT=lhs_fn(hs, g), rhs=rhs_fn(hs, g), start=True, stop=True),
                                      reads=reads, writes=[pr], pe_accum=True)
                            else:
                                kb.op("pe", lambda e, pt=pt, hs=hs, gs=gs, g=g: e.matmul(out_fn(pt, hs, gs), lhsT=lhs_fn(hs, g), rhs=rhs_fn(hs, g), start=True, stop=False),
                                      reads=reads, writes=[pr], pe_accum=True)
                                kb.op("pe", lambda e, pt=pt, hs=hs, gs=gs, g=g: e.matmul(out_fn(pt, hs, gs), lhsT=lhs2(hs, g), rhs=rhs2(hs, g), start=False, stop=True),
                                      reads=reads, writes=[pr], pe_accum=True)
                    return pt, pr

                def f32out(pt, hs, gs):
                    return pt[hs, gs]

                def cm(tile, bi):
                    return lambda hs, g: tile[hs, bi * 512 + 64 * g: bi * 512 + 64 * g + 64]

                def tmj(tile):
                    return lambda hs, g: tile[hs, 64 * g:64 * g + 64]

                for (src, dst) in (("vb", "Vtm"), ("qt", "Qtm"), ("Kh", "Khtm"), ("Bn", "Bhtm")):
                    for bi in BI:
                        pt, pr = ps.next()
                        pv = psbf(pt)
                        for g in range(8):
                            for h in range(2):
                                hs = HS[h]
                                kb.op("pe", lambda e, pv=pv, hs=hs, g=g, src=src, bi=bi: e.transpose(out=pv[hs, 64 * g:64 * g + 64], in_=Bf[src][hs, bi * 512 + 64 * g: bi * 512 + 64 * g + 64], identity=ident[hs, :]),
                                      reads=rs(Bf[src], ident), writes=[pr], pe_accum=True)
                        kb.op("act", lambda e, pv=pv, dst=dst, bi=bi: e.copy(out=PB[bi][dst][:], in_=pv[:, 0:512]), reads=[pr], writes=rs(PB[bi][dst]))
                for bi in BI:
                    pt, pr = mm_batch(bi, f32out, cm(Bf["nbt"], bi), cm(Bf["qt"], bi), rs(Bf["nbt"], Bf["qt"]))
                    ew("dve", "tensor_tensor", [pr, m_s], [PB[bi]["Um"]], out=PB[bi]["Um"][:], in0=pt[:], in1=m_s[:], op=ALU.mult)
                    ew("pool", "tensor_tensor", [PB[bi]["Um"], cf["itile"]], [PB[bi]["Xa"]], out=PB[bi]["Xa"][:], in0=PB[bi]["Um"][:], in1=cf["itile"][:], op=ALU.add)
                for bi in BI:
                    pt, pr = mm_batch(bi, f32out, cm(Bf["qt"], bi), cm(Bf["nbt"], bi), rs(Bf["nbt"], Bf["qt"]))
                    ew("dve", "tensor_tensor", [pr, m_st], [PB[bi]["Utm"]], out=PB[bi]["Utm"][:], in0=pt[:], in1=m_st[:], op=ALU.mult)
                for (lname, rname, mask, dst) in (("kt", "qt", m_s, "AkkT"), ("kt", "rt", m_i, "ArkT"), ("nbt", "rt", m_i, "nArbT")):
                    for bi in BI:
                        pt, pr = mm_batch(bi, f32out, cm(Bf[lname], bi), cm(Bf[rname], bi), rs(Bf[lname], Bf[rname]))
                        ew("dve", "tensor_tensor", [pr, mask], [PB[bi][dst]], out=PB[bi][dst][:], in0=pt[:], in1=mask[:], op=ALU.mult)
                for bi in BI:
                    pt, pr = mm_batch(bi, f32out, tmj(PB[bi]["AkkT"]), tmj(PB[bi]["Vtm"]), rs(PB[bi]["AkkT"], PB[bi]["Vtm"]))
                    ew("act", "copy", [pr], [PB[bi]["AV"]], out=PB[bi]["AV"][:], in_=pt[:])
                Pc = {bi: ("Um", "Utm") for bi in BI}
                Xc = {bi: "Xa" for bi in BI}
                for lev in range(1, 6):
                    pn, ptn = ("Pa", "Pta") if lev % 2 == 1 else ("Pb", "Ptb")
                    for bi in BI:
                        P_, Pt_ = Pc[bi]
                        if lev < 5:
                            pt, pr = mm_batch(bi, f32out, tmj(PB[bi][Pt_]), tmj(PB[bi][P_]), rs(PB[bi][Pt_], PB[bi][P_]))
                            ew("act", "copy", [pr], [PB[bi][pn]], out=PB[bi][pn][:], in_=pt[:])
                        pt, pr = mm_batch(bi, f32out, tmj(PB[bi][P_]), tmj(PB[bi][Pt_]), rs(PB[bi][Pt_], PB[bi][P_]))
                        ew("act", "copy", [pr], [PB[bi][ptn]], out=PB[bi][ptn][:], in_=pt[:])
                    for bi in BI:
                        xo = Xc[bi]
                        xn = "Xb" if xo == "Xa" else "Xa"
                        pt, pr = mm_batch(bi, f32out, tmj(PB[bi][ptn]), tmj(PB[bi][xo]), rs(PB[bi][ptn], PB[bi][xo]))
                        ew("dve", "tensor_tensor", [pr, PB[bi][xo]], [PB[bi][xn]], out=PB[bi][xn][:], in0=pt[:], in1=PB[bi][xo][:], op=ALU.add)
                        Xc[bi] = xn
                        Pc[bi] = (pn, ptn)
                for bi in BI:
                    X5 = PB[bi][Xc[bi]]
                    pt, pr = mm_batch(bi, f32out, tmj(X5), tmj(PB[bi]["AV"]), rs(X5, PB[bi]["AV"]))
                    ew("act", "copy", [pr], [PB[bi]["Gtm"]], out=PB[bi]["Gtm"][:], in_=pt[:])
                    pt, pr = mm_batch(bi, f32out, tmj(X5), tmj(PB[bi]["Qtm"]), rs(X5, PB[bi]["Qtm"]))
                    ew("act", "copy", [pr], [PB[bi]["Qh"]], out=PB[bi]["Qh"][:], in_=pt[:])
                for bi in BI:
                    pt, pr = mm_batch(bi, f32out, tmj(PB[bi]["Qh"]), tmj(PB[bi]["Bhtm"]), rs(PB[bi]["Qh"], PB[bi]["Bhtm"]))
                    for g in range(8):
                        cidx = bi * 8 + g
                        gs = slice(64 * g, 64 * g + 64)
                        ew("dve", "scalar_tensor_tensor", [pr, cf["itile"], wC], [PB[bi]["PhiT"]], out=PB[bi]["PhiT"][:, gs], in0=cf["itile"][:, gs],
                           scalar=wC[:, cidx:cidx + 1], in1=pt[:, gs], op0=ALU.mult, op1=ALU.add)
                    pt, pr = mm_batch(bi, f32out, tmj(PB[bi]["Qh"]), tmj(PB[bi]["nArbT"]), rs(PB[bi]["Qh"], PB[bi]["nArbT"]))
                    ew("dve", "tensor_tensor", [pr, Bf["rt"]], [PB[bi]["RhT"]], out=PB[bi]["RhT"][:], in0=pt[:], in1=Bf["rt"][:, bi * 512:(bi + 1) * 512], op=ALU.add)
                    pt, pr = mm_batch(bi, f32out, tmj(PB[bi]["Khtm"]), tmj(PB[bi]["Vtm"]), rs(PB[bi]["Khtm"], PB[bi]["Vtm"], PB[bi]["Bhtm"], PB[bi]["Gtm"]),
                                      lhs2=tmj(PB[bi]["Bhtm"]), rhs2=tmj(PB[bi]["Gtm"]))
                    ew("act", "copy", [pr], [Psi[bi]], out=Psi[bi][:], in_=pt[:])
                order = list(range(NCH))
                if d == 1:
                    order = order[::-1]
                for cidx in order:
                    bi, g = divmod(cidx, 8)
                    gs = slice(64 * g, 64 * g + 64)
                    pt, pr = ps.next()
                    for h in range(2):
                        hs = HS[h]
                        kb.op("pe", lambda e, pt=pt, hs=hs, gs=gs, bi=bi: e.matmul(pt[hs, 0:64], lhsT=PB[bi]["Vtm"][hs, gs], rhs=PB[bi]["ArkT"][hs, gs], start=True, stop=False),
                              reads=rs(PB[bi]["Vtm"], PB[bi]["ArkT"]), writes=[pr], pe_accum=True)
                        kb.op("pe", lambda e, pt=pt, hs=hs, gs=gs, bi=bi: e.matmul(pt[hs, 0:64], lhsT=PB[bi]["Gtm"][hs, gs], rhs=PB[bi]["nArbT"][hs, gs], start=False, stop=False),
                              reads=rs(PB[bi]["Gtm"], PB[bi]["nArbT"]), writes=[pr], pe_accum=True)
                        kb.op("pe", lambda e, pt=pt, hs=hs, gs=gs, bi=bi: e.matmul(pt[hs, 0:64], lhsT=Hbf[hs, :], rhs=PB[bi]["RhT"][hs, gs], start=False, stop=True),
                              reads=rs(Hbf, PB[bi]["RhT"]), writes=[pr], pe_accum=True)
                    ew("act", "copy", [pr], [F["yc"]], out=F["yc"][:, cidx * 64:(cidx + 1) * 64], in_=pt[:, 0:64])
                    pt2, pr2 = ps.next()
                    for h in range(2):
                        hs = HS[h]
                        kb.op("pe", lambda e, pt2=pt2, hs=hs, gs=gs, bi=bi: e.matmul(pt2[hs, 0:64], lhsT=PB[bi]["PhiT"][hs, gs], rhs=Hbf[hs, :], start=True, stop=True),
                              reads=rs(PB[bi]["PhiT"], Hbf), writes=[pr2], pe_accum=True)
                    ew("dve", "tensor_tensor", [pr2, Psi[bi]], [Hbf], out=Hbf[:], in0=pt2[:, 0:64], in1=Psi[bi][:, gs], op=ALU.add)
                if d == 0:
                    kb.dma(yfT[c0:c0 + 128, tsl], F["yc"][:], reads=rs(F["yc"]), eng="pool")
                else:
                    kb.dma(F["yf"][:], yfT[c0:c0 + 128, tsl], writes=rs(F["yf"]), eng="sp")
                    kb.dma(sg0[:], loraA[256:384, tsl], writes=rs(sg0), eng="act")
                    kb.dma(sg1[0:32, :], loraA[384:416, tsl], writes=rs(sg1), eng="act")
                    for hf in range(2):
                        sl = slice(hf * 512, (hf + 1) * 512)
                        pt, pr = ps.next()
                        kb.op("pe", lambda e, pt=pt, sl=sl: e.matmul(pt[:], lhsT=alp[0][:], rhs=adp[:, sl], start=True, stop=True),
                              reads=rs(alp[0], adp), writes=[pr])
                        kb.op("act", lambda e, pt=pt, sl=sl: e.activation(out=F["af"][:, sl], in_=pt[:], func=AF.Sigmoid, bias=col["a0f"][:, 0:1]),
                              reads=[pr] + rs(col["a0f"]), writes=rs(F["af"]))
                    ew("dve", "tensor_tensor", [F["af"], F["a_"]], [F["af"]], out=F["af"][:], in0=F["af"][:], in1=F["a_"][:], op=ALU.add)
                    ew("dve", "tensor_scalar", [F["af"], hka, omka], [F["af"]], out=F["af"][:], in0=F["af"][:], scalar1=hka[:, 0:1], scalar2=omka[:, 0:1], op0=ALU.mult, op1=ALU.add)
                    ew("dve", "tensor_tensor", [F["af"], F["k_"]], [F["af"]], out=F["af"][:], in0=F["af"][:], in1=F["k_"][:], op=ALU.mult)
                    ew("dve", "scalar_tensor_tensor", [F["af"], col["r_k"], F["r_"]], [sqr], out=sqr[:], in0=F["af"][:], scalar=col["r_k"][:, 0:1], in1=F["r_"][:], op0=ALU.mult, op1=ALU.mult)
                    ew("dve", "tensor_tensor", [F["yc"], F["yf"]], [F["yc"]], out=F["yc"][:], in0=F["yc"][:], in1=F["yf"][:], op=ALU.add)
                    ew("act", "copy", [F["yc"]], [tw], out=tw[:], in_=F["yc"][:])
                    ew("act", "activation", [F["yc"]], [adp], out=adp[:], in_=F["yc"][:], func=AF.Square)
                    for hf in range(2):
                        sl = slice(hf * 512, (hf + 1) * 512)
                        for (src, dstn, scale) in ((tw, "po1", 1.0 / 64), (adp, "po2", 1.0 / 64), (sqr, "po3", 1.0)):
                            pt, pr = ps.next()
                            kb.op("pe", lambda e, pt=pt, sl=sl, src=src: e.matmul(pt[:], lhsT=bones[:], rhs=src[:, sl], start=True, stop=True),
                                  reads=rs(bones, src), writes=[pr])
                            kb.op("act", lambda e, pt=pt, sl=sl, dstn=dstn, scale=scale: e.mul(out=F[dstn][:, sl], in_=pt[:], mul=scale),
                                  reads=[pr], writes=rs(F[dstn]))
                        pt, pr = ps.next()
                        kb.op("pe", lambda e, pt=pt, sl=sl: e.matmul(pt[:], lhsT=gl0[:], rhs=sg0[:, sl], start=True, stop=False),
                              reads=rs(gl0, sg0), writes=[pr], pe_accum=True)
                        kb.op("pe", lambda e, pt=pt, sl=sl: e.matmul(pt[:], lhsT=gl1[:], rhs=sg1[:, sl], start=False, stop=True),
                              reads=rs(gl1, sg1), writes=[pr], pe_accum=True)
                        kb.op("act", lambda e, pt=pt, sl=sl: e.copy(out=F["tA"][:, sl], in_=pt[:]), reads=[pr], writes=rs(F["tA"]))
                    ew("dve", "tensor_tensor", [F["po1"]], [F["tB"]], out=F["tB"][:], in0=F["po1"][:], in1=F["po1"][:], op=ALU.mult)
                    ew("dve", "tensor_tensor", [F["po2"], F["tB"]], [F["po2"]], out=F["po2"][:], in0=F["po2"][:], in1=F["tB"][:], op=ALU.subtract)
                    ew("act", "activation", [F["po2"]], [F["po2"]], out=F["po2"][:], in_=F["po2"][:], func=AF.Sqrt, bias=64e-5)
                    ew("dve", "reciprocal", [F["po2"]], [F["po2"]], out=F["po2"][:], in_=F["po2"][:])
                    ew("dve", "tensor_tensor", [F["yc"], F["po1"]], [F["yc"]], out=F["yc"][:], in0=F["yc"][:], in1=F["po1"][:], op=ALU.subtract)
                    ew("dve", "tensor_tensor", [F["yc"], F["po2"]], [F["yc"]], out=F["yc"][:], in0=F["yc"][:], in1=F["po2"][:], op=ALU.mult)
                    ew("dve", "tensor_scalar", [F["yc"], col["lnx_g"], col["lnx_b"]], [F["yc"]], out=F["yc"][:], in0=F["yc"][:], scalar1=col["lnx_g"][:, 0:1], scalar2=col["lnx_b"][:, 0:1], op0=ALU.mult, op1=ALU.add)
                    ew("dve", "tensor_tensor", [F["po3"], F["v_"]], [F["po3"]], out=F["po3"][:], in0=F["po3"][:], in1=F["v_"][:], op=ALU.mult)
                    ew("dve", "tensor_tensor", [F["yc"], F["po3"]], [F["yc"]], out=F["yc"][:], in0=F["yc"][:], in1=F["po3"][:], op=ALU.add)
                    ew("dve", "tensor_tensor", [F["yc"], F["tA"]], [yout], out=yout[:], in0=F["yc"][:], in1=F["tA"][:], op=ALU.mult)
                    kb.dma(yaT[c0:c0 + 128, tsl], yout[:], reads=rs(yout), eng="pool")
    kb.emit()


def stage_pool(nc, projT, pool_w, pool_scale, ybT, cnt_inv):
    kb = KB(nc)
    S = 1024
    HL = 8
    ps = PS(kb)
    Pp = [Tl(kb, f"Pp{i}", [128, S + 2 * HL]) for i in range(2)]
    acc = Tl(kb, "acc", [128, S + 2 * HL])
    acc2 = Tl(kb, "acc2", [128, S + 2 * HL])
    z = [Tl(kb, f"z{i}", [128, S], F32R) for i in range(2)]
    ci = Tl(kb, "ci", [128, S])
    pw = [Tl(kb, f"pw{i}", [128, 256], F32R) for i in range(2)]
    sc = Tl(kb, "sc", [128, 1])
    ot = [Tl(kb, f"ot{i}", [128, S], F32R) for i in range(2)]
    oi = 0
    for g, win in enumerate((2, 4, 8, 16)):
        half = win // 2
        for kc in range(2):
            kb.dma(pw[kc][:], pool_w[g, kc * 128:(kc + 1) * 128, :], writes=rs(pw[kc]))
        for s in range(T // S):
            t0 = s * S
            kb.dma(ci[:], cnt_inv[g:g + 1, t0:t0 + S].partition_broadcast(128), writes=rs(ci), eng="act")
            for kc in range(2):
                r0 = 3488 + g * 256 + kc * 128
                P = Pp[kc]
                lo = max(t0 - HL, 0)
                hi = min(t0 + S + HL, T)
                kb.dma(P[:, lo - (t0 - HL):hi - (t0 - HL)], projT[r0:r0 + 128, lo:hi], writes=rs(P), eng="sp")
                if t0 == 0:
                    kb.op("pool", lambda e, P=P: e.memset(P[:, 0:HL], 0.0), writes=rs(P))
                if t0 + S == T:
                    kb.op("pool", lambda e, P=P: e.memset(P[:, S + HL:S + 2 * HL], 0.0), writes=rs(P))
                W_ = S + 2 * HL
                kb.op("dve", lambda e, P=P: e.tensor_tensor(out=acc[:, 1:W_], in0=P[:, 0:W_ - 1], in1=P[:, 1:W_], op=ALU.add),
                      reads=rs(P), writes=rs(acc))
                cur, oth = acc, acc2
                wlen = 2
                while wlen < win:
                    kb.op("dve", lambda e, cur=cur, oth=oth, wlen=wlen: e.tensor_tensor(out=oth[:, 2 * wlen - 1:W_], in0=cur[:, 2 * wlen - 1:W_], in1=cur[:, wlen - 1:W_ - wlen], op=ALU.add),
                          reads=rs(cur), writes=rs(oth))
                    cur, oth = oth, cur
                    wlen *= 2
                off = HL + half - 1
                kb.op("dve", lambda e, cur=cur, off=off: e.tensor_tensor(out=acc2[:, 0:S] if cur is acc else acc[:, 0:S], in0=cur[:, off:off + S], in1=ci[:], op=ALU.mult),
                      reads=rs(cur, ci), writes=rs(acc2 if cur is acc else acc))
                mt = acc2 if cur is acc else acc
                kb.op("dve", lambda e, mt=mt, P=P, kc=kc: e.tensor_tensor(out=z[kc][:], in0=mt[:, 0:S], in1=P[:, HL:HL + S], op=ALU.subtract),
                      reads=rs(mt, P), writes=rs(z[kc]))
            for oc in range(2):
                c0 = g * 256 + oc * 128
                load_col(kb, sc, pool_scale[c0:c0 + 128])
                o = ot[oi % 2]
                oi += 1
                for hf in range(2):
                    sl = slice(hf * 512, (hf + 1) * 512)
                    pt, pr = ps.next()
                    for kc in range(2):
                        kb.op("pe", lambda e, pt=pt, kc=kc, oc=oc, sl=sl: e.matmul(pt[:], lhsT=pw[kc][:, oc * 128:(oc + 1) * 128], rhs=z[kc][:, sl], start=(kc == 0), stop=(kc == 1)),
                              reads=rs(pw[kc], z[kc]), writes=[pr], pe_accum=True)
                    kb.op("act", lambda e, pt=pt, o=o, sl=sl: e.activation(out=o[:, sl], in_=pt[:], func=AF.Identity, scale=sc[:, 0:1]),
                          reads=[pr] + rs(sc), writes=rs(o))
                kb.dma(ybT[c0:c0 + 128, t0:t0 + S], o[:], reads=rs(o), eng="pool")
    kb.emit()


def stage_mix(nc, xin, projT, yaT, ybT, w_up_a, w_up_b, w_o, g2, w_router, xmT, v_tm, affT, identf_ap):
    kb = KB(nc)
    N = 512
    KC = D // 128
    ps = PS(kb)
    ya = Tl(kb, "ya", [128, 8, N], F32R)
    yb = Tl(kb, "yb", [128, 8, N], F32R)
    mg = kb.sb("mg", [128, KC, N], F32R)
    mg_r = [kb.res(f"mg{k}") for k in range(KC)]
    xm = kb.sb("xm", [128, KC, N])
    xm_r = [kb.res(f"xm{k}") for k in range(KC)]
    vt = kb.sb("vt", [128, KC, N])
    vt_r = [kb.res(f"vt{k}") for k in range(KC)]
    wa = [Tl(kb, f"wa{i}", [128, 8, 128], F32R) for i in range(2)]
    wb = [Tl(kb, f"wb{i}", [128, 8, 128], F32R) for i in range(2)]
    wo = [Tl(kb, f"wo{i}", [128, KC, 128], F32R) for i in range(2)]
    sgA = [Tl(kb, f"sgA{i}", [128, N]) for i in range(2)]
    sgB = [Tl(kb, f"sgB{i}", [128, N]) for i in range(2)]
    xi = [Tl(kb, f"xi{i}", [128, N]) for i in range(2)]
    t1 = Tl(kb, "t1", [128, N])
    t2 = Tl(kb, "t2", [128, N])
    sq = [Tl(kb, f"sq{i}", [128, N], F32R) for i in range(2)]
    rstd = Tl(kb, "rstd", [128, N])
    gt = Tl(kb, "gt", [128, KC])
    onesf = Tl(kb, "onesf", [128, 128])
    ones = Tl(kb, "ones", [128, 128], F32R)
    idf = Tl(kb, "idf", [128, 128])
    wr = Tl(kb, "wr", [128, KC, NE])
    ex = Tl(kb, "ex", [NE, N])
    rsum = Tl(kb, "rsum", [NE, N])
    vo = [Tl(kb, f"vo{i}", [128, D], BF16) for i in range(2)]
    kb.op("pool", lambda e: e.memset(onesf[:], 1.0), writes=rs(onesf))
    kb.op("dve", lambda e: e.tensor_copy(out=ones[:], in_=onesf[:]), reads=rs(onesf), writes=rs(ones))
    kb.dma(idf[:], identf_ap, writes=rs(idf))
    kb.dma(gt[:], g2.rearrange("(k p) -> p k", p=128), writes=rs(gt), allow_slow_non_contiguous=True)
    kb.dma(wr[:], w_router.rearrange("(k p) e -> p k e", p=128), writes=rs(wr))
    wav = w_up_a.rearrange("(k p) n -> p k n", p=128)
    wbv = w_up_b.rearrange("(k p) n -> p k n", p=128)
    wov = w_o.rearrange("(k p) n -> p k n", p=128)
    it = 0
    voi = 0
    for s in range(T // N):
        tsl = slice(s * N, (s + 1) * N)
        kb.dma(ya[:], yaT[:, tsl].rearrange("(k p) n -> p k n", p=128), writes=rs(ya), eng="sp")
        kb.dma(yb[:], ybT[:, tsl].rearrange("(k p) n -> p k n", p=128), writes=rs(yb), eng="act")
        for oc in range(KC):
            b = it % 2
            it += 1
            osl = slice(oc * 128, (oc + 1) * 128)
            kb.dma(wa[b][:], wav[:, :, osl], writes=rs(wa[b]), eng="sp")
            kb.dma(wb[b][:], wbv[:, :, osl], writes=rs(wb[b]), eng="act")
            kb.dma(sgA[b][:], projT[4512 + oc * 128:4512 + (oc + 1) * 128, tsl], writes=rs(sgA[b]), eng="sp")
            kb.dma(sgB[b][:], projT[6560 + oc * 128:6560 + (oc + 1) * 128, tsl], writes=rs(sgB[b]), eng="act")
            pa, par = ps.next()
            for kc in range(8):
                kb.op("pe", lambda e, pa=pa, b=b, kc=kc: e.matmul(pa[:], lhsT=wa[b][:, kc, :], rhs=ya[:, kc, :], start=(kc == 0), stop=(kc == 7)),
                      reads=rs(wa[b], ya), writes=[par], pe_accum=True)
            pb_, pbr = ps.next()
            for kc in range(8):
                kb.op("pe", lambda e, pb_=pb_, b=b, kc=kc: e.matmul(pb_[:], lhsT=wb[b][:, kc, :], rhs=yb[:, kc, :], start=(kc == 0), stop=(kc == 7)),
                      reads=rs(wb[b], yb), writes=[pbr], pe_accum=True)
            kb.op("dve", lambda e, pa=pa, b=b: e.tensor_tensor(out=t1[:], in0=pa[:], in1=sgA[b][:], op=ALU.mult), reads=[par] + rs(sgA[b]), writes=rs(t1))
            kb.op("dve", lambda e, pb_=pb_, b=b: e.tensor_tensor(out=t2[:], in0=pb_[:], in1=sgB[b][:], op=ALU.mult), reads=[pbr] + rs(sgB[b]), writes=rs(t2))
            kb.op("pool", lambda e, oc=oc: e.tensor_tensor(out=mg[:, oc, :], in0=t1[:], in1=t2[:], op=ALU.add), reads=rs(t1, t2), writes=[mg_r[oc]])
        pss, pssr = ps.next()
        for oc in range(KC):
            b = it % 2
            it += 1
            osl = slice(oc * 128, (oc + 1) * 128)
            kb.dma(wo[b][:], wov[:, :, osl], writes=rs(wo[b]), eng="sp")
            kb.dma(xi[b][:], xin[osl, tsl], writes=rs(xi[b]), eng="act")
            po, por = ps.next()
            if po is pss:
                po, por = ps.next()
            for kc in range(KC):
                kb.op("pe", lambda e, po=po, b=b, kc=kc: e.matmul(po[:], lhsT=wo[b][:, kc, :], rhs=mg[:, kc, :], start=(kc == 0), stop=(kc == KC - 1)),
                      reads=[wo[b].r, mg_r[kc]], writes=[por], pe_accum=True)
            kb.op("dve", lambda e, po=po, b=b, oc=oc: e.tensor_tensor(out=xm[:, oc, :], in0=po[:], in1=xi[b][:], op=ALU.add), reads=[por] + rs(xi[b]), writes=[xm_r[oc]])
            kb.dma(xmT[osl, tsl], xm[:, oc, :], reads=[xm_r[oc]], eng="pool")
            a = oc % 2
            kb.op("act", lambda e, a=a, oc=oc: e.activation(out=sq[a][:], in_=xm[:, oc, :], func=AF.Square), reads=[xm_r[oc]], writes=rs(sq[a]))
            kb.op("pe", lambda e, a=a, oc=oc, pss=pss: e.matmul(pss[:], lhsT=ones[:], rhs=sq[a][:], start=(oc == 0), stop=(oc == KC - 1)),
                  reads=rs(ones, sq[a]), writes=[pssr], pe_accum=True)
        kb.op("act", lambda e, pss=pss: e.activation(out=rstd[:], in_=pss[:], func=AF.Sqrt, scale=1.0 / D, bias=EPS), reads=[pssr], writes=rs(rstd))
        kb.op("dve", lambda e: e.reciprocal(out=rstd[:], in_=rstd[:]), writes=rs(rstd))
        for oc in range(KC):
            kb.op("dve", lambda e, oc=oc: e.scalar_tensor_tensor(out=vt[:, oc, :], in0=xm[:, oc, :], scalar=gt[:, oc:oc + 1], in1=rstd[:], op0=ALU.mult, op1=ALU.mult),
                  reads=[xm_r[oc]] + rs(gt, rstd), writes=[vt_r[oc]])
        pl, plr = ps.next()
        for kc in range(KC):
            kb.op("pe", lambda e, pl=pl, kc=kc: e.matmul(pl[0:NE, :], lhsT=wr[:, kc, :], rhs=vt[:, kc, :], start=(kc == 0), stop=(kc == KC - 1)),
                  reads=[wr.r, vt_r[kc]], writes=[plr], pe_accum=True)
        kb.op("act", lambda e, pl=pl: e.activation(out=ex[:], in_=pl[0:NE, :], func=AF.Exp), reads=[plr], writes=rs(ex))
        pq, pqr = ps.next()
        kb.op("pe", lambda e, pq=pq: e.matmul(pq[0:NE, :], lhsT=onesf[0:NE, 0:NE], rhs=ex[:], start=True, stop=True), reads=rs(onesf, ex), writes=[pqr])
        kb.op("dve", lambda e, pq=pq: e.reciprocal(out=rsum[:], in_=pq[0:NE, :]), reads=[pqr], writes=rs(rsum))
        kb.op("dve", lambda e: e.tensor_tensor(out=ex[:], in0=ex[:], in1=rsum[:], op=ALU.mult), reads=rs(rsum), writes=rs(ex))
        kb.dma(affT[:, tsl], ex[:], reads=rs(ex), eng="sp")
        for tb in range(N // 128):
            o = vo[voi % 2]
            voi += 1
            for q in range(4):
                ptt, ptr = ps.next()
                for j in range(4):
                    oc = q * 4 + j
                    kb.op("pe", lambda e, ptt=ptt, oc=oc, tb=tb, j=j: e.transpose(out=ptt[:, j * 128:(j + 1) * 128], in_=vt[:, oc, tb * 128:(tb + 1) * 128], identity=idf[:]),
                          reads=[vt_r[oc]] + rs(idf), writes=[ptr], pe_accum=True)
                eng = "act" if q % 2 == 0 else "dve"
                if eng == "act":
                    kb.op("act", lambda e, ptt=ptt, o=o, q=q: e.copy(out=o[:, q * 512:(q + 1) * 512], in_=ptt[:]), reads=[ptr], writes=rs(o))
                else:
                    kb.op("dve", lambda e, ptt=ptt, o=o, q=q: e.tensor_copy(out=o[:, q * 512:(q + 1) * 512], in_=ptt[:]), reads=[ptr], writes=rs(o))
            kb.dma(v_tm[s * N + tb * 128:s * N + (tb + 1) * 128, :], o[:], reads=rs(o), eng="pool")
    kb.emit()


def stage_route(nc, affT, posD, gateD):
    kb = KB(nc)
    kb.force_sp = True
    aff = Tl(kb, "aff", [NE, T])
    junk = Tl(kb, "junk", [NE, T])
    onesr = Tl(kb, "onesr", [NE, T])
    cs = Tl(kb, "cs", [NE, T])
    lo = Tl(kb, "lo", [NE, 1])
    hi = Tl(kb, "hi", [NE, 1])
    mid = Tl(kb, "mid", [NE, 1])
    cnt = Tl(kb, "cnt", [NE, 1])
    ge = Tl(kb, "ge", [NE, 1])
    d1 = Tl(kb, "d1", [NE, 1])
    d2 = Tl(kb, "d2", [NE, 1])

    def ew(eng, fname, reads, writes, **kw):
        kb.op(eng, lambda e: getattr(e, fname)(**kw), reads=rs(*reads), writes=rs(*writes))

    kb.dma(aff[:], affT, writes=rs(aff))
    ew("pool", "memset", [], [onesr], ap=onesr[:], constant=1.0)
    ew("pool", "memset", [], [lo], ap=lo[:], constant=0.0)
    ew("pool", "memset", [], [hi], ap=hi[:], constant=1.0)
    for it in range(30):
        w = 2.0 ** -(it + 1)
        ew("dve", "tensor_scalar", [lo], [mid], out=mid[:], in0=lo[:], scalar1=w, scalar2=None, op0=ALU.add)
        ew("dve", "tensor_scalar", [aff, mid], [junk, cnt], out=junk[:], in0=aff[:], scalar1=mid[:, 0:1], scalar2=None, op0=ALU.is_ge, op1=ALU.add, accum_out=cnt[:])
        ew("dve", "tensor_scalar", [cnt], [ge], out=ge[:], in0=cnt[:], scalar1=float(CAP) - 0.5, scalar2=w, op0=ALU.is_ge, op1=ALU.mult)
        ew("dve", "tensor_tensor", [lo, ge], [lo], out=lo[:], in0=lo[:], in1=ge[:], op=ALU.add)
    ew("dve", "tensor_scalar", [aff, lo], [junk], out=junk[:], in0=aff[:], scalar1=lo[:, 0:1], scalar2=None, op0=ALU.is_ge)
    ew("dve", "tensor_tensor_scan", [onesr, junk], [cs], out=cs[:], data0=onesr[:], data1=junk[:], initial=0.0, op0=ALU.mult, op1=ALU.add)
    ew("dve", "tensor_tensor", [cs, junk], [cs], out=cs[:], in0=cs[:], in1=junk[:], op=ALU.mult)
    ew("dve", "tensor_scalar", [cs], [cs], out=cs[:], in0=cs[:], scalar1=-1.0, scalar2=None, op0=ALU.add)
    kb.dma(posD, cs[:], reads=rs(cs))
    ew("dve", "tensor_tensor", [aff, junk], [aff], out=aff[:], in0=aff[:], in1=junk[:], op=ALU.mult)
    kb.dma(gateD, aff[:], reads=rs(aff), eng="act")
    kb.emit()


def stage_experts(nc, posD, v_tm, w_gate, w_up, w_down, yD, iota_row, experts=range(NE)):
    kb = KB(nc)
    kb.force_sp = True
    KC = D // 128
    NF = FF // 128
    NTI = T // 128
    ps = PS(kb)
    iot = Tl(kb, "iot", [128, 512])
    kb.dma(iot[:], iota_row, writes=rs(iot))
    ptm = Tl(kb, "ptm", [128, NTI])
    sel = [Tl(kb, f"sel{i}", [128, 512], BF16) for i in range(2)]
    vt = [Tl(kb, f"vt{i}", [128, D], BF16) for i in range(2)]
    xs = kb.sb("xs", [128, KC, 512], F32R)
    xs_r = [kb.res(f"xs{k}") for k in range(KC)]
    hT = kb.sb("hT", [128, NF, 512], F32R)
    hT_r = [kb.res(f"hT{k}") for k in range(NF)]
    wbuf = [Tl(kb, f"wb{i}", [128, KC * 256], F32R) for i in range(4)]
    sil = Tl(kb, "sil", [128, 512])
    yo = [Tl(kb, f"yo{i}", [128, D], BF16) for i in range(2)]
    vv = v_tm.rearrange("(p n) d -> p n d", n=NTI)
    wi = 0
    yi = 0
    si = 0
    for e_ in experts:
        kb.dma(ptm[:], posD[e_, :].rearrange("(p n) -> p n", n=NTI), writes=rs(ptm))
        for half in range(2):
            banks = [ps.next() for _ in range(8)]
            for n in range(NTI):
                b = si % 2
                si += 1
                kb.dma(vt[b][:, half * 1024:(half + 1) * 1024], vv[:, n, half * 1024:(half + 1) * 1024], writes=rs(vt[b]), eng=("sp" if n % 2 == 0 else "act"))
                kb.op("dve", lambda e, b=b, n=n: e.tensor_scalar(out=sel[b][:], in0=iot[:], scalar1=ptm[:, n:n + 1], scalar2=None, op0=ALU.is_equal),
                      reads=rs(iot, ptm), writes=rs(sel[b]))
                for ci in range(8):
                    c = half * 8 + ci
                    pt, pr = banks[ci]
                    kb.op("pe", lambda e, pt=pt, b=b, c=c, n=n: e.matmul(pt[:], lhsT=vt[b][:, c * 128:(c + 1) * 128], rhs=sel[b][:], start=(n == 0), stop=(n == NTI - 1)),
                          reads=rs(vt[b], sel[b]), writes=[pr], pe_accum=True)
            for ci in range(8):
                c = half * 8 + ci
                pt, pr = banks[ci]
                if ci % 2 == 0:
                    kb.op("act", lambda e, pt=pt, c=c: e.copy(out=xs[:, c, :], in_=pt[:]), reads=[pr], writes=[xs_r[c]])
                else:
                    kb.op("dve", lambda e, pt=pt, c=c: e.tensor_copy(out=xs[:, c, :], in_=pt[:]), reads=[pr], writes=[xs_r[c]])
        wgv = w_gate[e_].rearrange("(k p) f -> p k f", p=128)
        wuv = w_up[e_].rearrange("(k p) f -> p k f", p=128)
        for fp in range(NF // 2):
            bg = wbuf[(wi * 2) % 4]
            bu = wbuf[(wi * 2 + 1) % 4]
            wi += 1
            fsl = slice(fp * 256, (fp + 1) * 256)
            kb.dma(bg[:].rearrange("p (k f) -> p k f", f=256), wgv[:, :, fsl], writes=rs(bg), eng="sp")
            kb.dma(bu[:].rearrange("p (k f) -> p k f", f=256), wuv[:, :, fsl], writes=rs(bu), eng="act")
            bg3 = bg[:].rearrange("p (k f) -> p k f", f=256)
            bu3 = bu[:].rearrange("p (k f) -> p k f", f=256)
            for j in range(2):
                f = fp * 2 + j
                pg, pgr = ps.next()
                for k in range(KC):
                    kb.op("pe", lambda e, pg=pg, bg3=bg3, j=j, k=k: e.matmul(pg[:], lhsT=bg3[:, k, j * 128:(j + 1) * 128], rhs=xs[:, k, :], start=(k == 0), stop=(k == KC - 1)),
                          reads=[bg.r, xs_r[k]], writes=[pgr], pe_accum=True)
                pu, pur = ps.next()
                for k in range(KC):
                    kb.op("pe", lambda e, pu=pu, bu3=bu3, j=j, k=k: e.matmul(pu[:], lhsT=bu3[:, k, j * 128:(j + 1) * 128], rhs=xs[:, k, :], start=(k == 0), stop=(k == KC - 1)),
                          reads=[bu.r, xs_r[k]], writes=[pur], pe_accum=True)
                kb.op("act", lambda e, pg=pg: e.activation(out=sil[:], in_=pg[:], func=AF.Silu), reads=[pgr], writes=rs(sil))
                kb.op("dve", lambda e, pu=pu, f=f: e.tensor_tensor(out=hT[:, f, :], in0=pu[:], in1=sil[:], op=ALU.mult), reads=[pur] + rs(sil), writes=[hT_r[f]])
        wdv = w_down[e_].rearrange("(f p) d -> p f d", p=128)
        for qh in range(2):
            banks = [ps.next() for _ in range(8)]
            for f in range(NF):
                wb_ = wbuf[wi % 4]
                wi += 1
                kb.dma(wb_[:, 0:1024], wdv[:, f, qh * 1024:(qh + 1) * 1024], writes=rs(wb_), eng=("sp" if f % 2 == 0 else "act"))
                for j in range(4):
                    for qq in range(2):
                        pt, pr = banks[j * 2 + qq]
                        kb.op("pe", lambda e, pt=pt, wb_=wb_, f=f, j=j, qq=qq: e.matmul(pt[:], lhsT=hT[:, f, j * 128:(j + 1) * 128], rhs=wb_[:, qq * 512:(qq + 1) * 512], start=(f == 0), stop=(f == NF - 1)),
                              reads=[hT_r[f], wb_.r], writes=[pr], pe_accum=True)
            for j in range(4):
                o = yo[yi % 2]
                yi += 1
                for qq in range(2):
                    pt, pr = banks[j * 2 + qq]
                    if qq == 0:
                        kb.op("act", lambda e, pt=pt, o=o, qq=qq: e.copy(out=o[:, qq * 512:(qq + 1) * 512], in_=pt[:]), reads=[pr], writes=rs(o))
                    else:
                        kb.op("dve", lambda e, pt=pt, o=o, qq=qq: e.tensor_copy(out=o[:, qq * 512:(qq + 1) * 512], in_=pt[:]), reads=[pr], writes=rs(o))
                kb.dma(yD[e_, j * 128:(j + 1) * 128, qh * 1024:(qh + 1) * 1024], o[:, 0:1024], reads=rs(o), eng="sp")
    kb.emit()


def stage_combine(nc, posD, gateD, yD, xmT, xoutT, slot_col):
    kb = KB(nc)
    kb.force_sp = True
    N = 512
    ps = PS(kb)
    scol = Tl(kb, "scol", [128, 4])
    kb.dma(scol[:], slot_col, writes=rs(scol))
    posb = [Tl(kb, f"posb{i}", [128, N]) for i in range(2)]
    gateb = [Tl(kb, f"gateb{i}", [128, N]) for i in range(2)]
    selg = [[Tl(kb, f"selg{i}_{j}", [128, N], BF16) for j in range(4)] for i in range(2)]
    ye = [Tl(kb, f"ye{i}", [128, 4, 1024], BF16) for i in range(2)]
    xi = [Tl(kb, f"xi{i}", [128, N]) for i in range(2)]
    xo = [Tl(kb, f"xo{i}", [128, N]) for i in range(2)]
    it = 0
    oi = 0
    for tb in range(T // N):
        tsl = slice(tb * N, (tb + 1) * N)
        for chalf in range(2):
            banks = [ps.next() for _ in range(8)]
            for e_ in range(NE):
                b = it % 2
                it += 1
                kb.dma(posb[b][:], posD[e_:e_ + 1, tsl].partition_broadcast(128), writes=rs(posb[b]), eng="sp")
                kb.dma(gateb[b][:], gateD[e_:e_ + 1, tsl].partition_broadcast(128), writes=rs(gateb[b]), eng="act")
                kb.dma(ye[b][:], yD[e_, :, chalf * 1024:(chalf + 1) * 1024].rearrange("(j p) d -> p j d", p=128), writes=rs(ye[b]), eng="sp")
                for j in range(4):
                    kb.op("dve", lambda e, b=b, j=j: e.scalar_tensor_tensor(out=selg[b][j][:], in0=posb[b][:], scalar=scol[:, j:j + 1], in1=gateb[b][:], op0=ALU.is_equal, op1=ALU.mult),
                          reads=rs(posb[b], gateb[b], scol), writes=rs(selg[b][j]))
                for ci in range(8):
                    pt, pr = banks[ci]
                    for j in range(4):
                        kb.op("pe", lambda e, pt=pt, b=b, j=j, ci=ci, e_=e_: e.matmul(pt[:], lhsT=ye[b][:, j, ci * 128:(ci + 1) * 128], rhs=selg[b][j][:], start=(e_ == 0 and j == 0), stop=(e_ == NE - 1 and j == 3)),
                              reads=rs(ye[b], selg[b][j]), writes=[pr], pe_accum=True)
            for ci in range(8):
                c = chalf * 8 + ci
                pt, pr = banks[ci]
                ob = oi % 2
                oi += 1
                kb.dma(xi[ob][:], xmT[c * 128:(c + 1) * 128, tsl], writes=rs(xi[ob]), eng="act")
                kb.op("dve", lambda e, pt=pt, ob=ob: e.tensor_tensor(out=xo[ob][:], in0=pt[:], in1=xi[ob][:], op=ALU.add), reads=[pr] + rs(xi[ob]), writes=rs(xo[ob]))
                kb.dma(xoutT[c * 128:(c + 1) * 128, tsl], xo[ob][:], reads=rs(xo[ob]), eng="pool")
    kb.emit()


def stage_final(nc, xT, g_ap, outT):
    kb = KB(nc)
    KC = D // 128
    N = 512
    ps = PS(kb)
    x = kb.sb("x", [128, KC, N])
    x_r = [kb.res(f"x{k}") for k in range(KC)]
    sq = [Tl(kb, f"sq{i}", [128, N], F32R) for i in range(2)]
    rstd = Tl(kb, "rstd", [128, N])
    gt = Tl(kb, "gt", [128, KC])
    onesf = Tl(kb, "onesf", [128, 128])
    ones = Tl(kb, "ones", [128, 128], F32R)
    o = [Tl(kb, f"o{i}", [128, N]) for i in range(2)]
    kb.op("pool", lambda e: e.memset(onesf[:], 1.0), writes=rs(onesf))
    kb.op("dve", lambda e: e.tensor_copy(out=ones[:], in_=onesf[:]), reads=rs(onesf), writes=rs(ones))
    kb.dma(gt[:], g_ap.rearrange("(k p) -> p k", p=128), writes=rs(gt), allow_slow_non_contiguous=True)
    xv = xT.rearrange("(k p) n -> p k n", p=128)
    ov = outT.rearrange("(k p) n -> p k n", p=128)
    oi = 0
    for s in range(T // N):
        tsl = slice(s * N, (s + 1) * N)
        pt, pr = ps.next()
        for k in range(KC):
            kb.dma(x[:, k, :], xv[:, k, tsl], writes=[x_r[k]], eng=("sp" if k % 2 == 0 else "act"))
            a = k % 2
            kb.op("act", lambda e, a=a, k=k: e.activation(out=sq[a][:], in_=x[:, k, :], func=AF.Square), reads=[x_r[k]], writes=rs(sq[a]))
            kb.op("pe", lambda e, pt=pt, a=a, k=k: e.matmul(pt[:], lhsT=ones[:], rhs=sq[a][:], start=(k == 0), stop=(k == KC - 1)),
                  reads=rs(ones, sq[a]), writes=[pr], pe_accum=True)
        kb.op("act", lambda e, pt=pt: e.activation(out=rstd[:], in_=pt[:], func=AF.Sqrt, scale=1.0 / D, bias=EPS), reads=[pr], writes=rs(rstd))
        kb.op("dve", lambda e: e.reciprocal(out=rstd[:], in_=rstd[:]), writes=rs(rstd))
        for k in range(KC):
            ob = oi % 2
            oi += 1
            kb.op("dve", lambda e, k=k, ob=ob: e.scalar_tensor_tensor(out=o[ob][:], in0=x[:, k, :], scalar=gt[:, k:k + 1], in1=rstd[:], op0=ALU.mult, op1=ALU.mult),
                  reads=[x_r[k]] + rs(gt, rstd), writes=rs(o[ob]))
            kb.dma(ov[:, k, tsl], o[ob][:], reads=rs(o[ob]), eng="pool", is_output=True)
    kb.emit()


def make_consts():
    p = np.arange(128)[:, None] % 64
    t = np.arange(512)[None, :] % 64
    c = {}
    c["m_up"] = (p < t).astype(np.float32)
    c["m_lo"] = (p > t).astype(np.float32)
    c["m_upi"] = (p <= t).astype(np.float32)
    c["m_loi"] = (p >= t).astype(np.float32)
    c["itile"] = (p == t).astype(np.float32)
    c["ident"] = (p == np.arange(64)[None, :]).astype(np.float32)
    c["bones"] = ((np.arange(128)[:, None] // 64) == (np.arange(128)[None, :] // 64)).astype(np.float32)
    cm = np.ones((128, 1024), np.float32)
    cm[:, ::64] = 0.0
    c["cmask"] = cm
    return c


from concourse.bass_utils import run_bass_kernel_spmd

N_CORES = 2
DEPTH = 2


def make_consts_all():
    c = make_consts()
    t = np.arange(T)
    ci = np.zeros((4, T), np.float32)
    for g, w in enumerate((2, 4, 8, 16)):
        h = w // 2
        lo = np.clip(t - h, 0, T)
        hi = np.clip(t + h, 0, T)
        ci[g] = 1.0 / (hi - lo).astype(np.float32)
    c["cnt_inv"] = ci
    c["iota_row"] = np.tile(np.arange(512, dtype=np.float32)[None, :], (128, 1))
    c["slot_col"] = (np.arange(4)[None, :] * 128 + np.arange(128)[:, None]).astype(np.float32)
    c["identf"] = np.eye(128, dtype=np.float32)
    return c


_WSPEC = {
    "norm_mix_g": ([DEPTH, D], F32), "w_in": ([DEPTH, D, D_IN], F32R), "mu_shift": ([DEPTH, 3488], F32),
    "w0": ([DEPTH, 2, 1024], F32), "w_lora_up": ([DEPTH, 2, 64, 1024], F32R), "a0": ([DEPTH, 2, 1024], F32),
    "a_lora_up": ([DEPTH, 2, 64, 1024], F32R), "g_lora_up": ([DEPTH, 160, 1024], F32R), "k_k": ([DEPTH, 1024], F32),
    "k_a": ([DEPTH, 1024], F32), "r_k": ([DEPTH, 1024], F32), "lnx_g": ([DEPTH, 1024], F32), "lnx_b": ([DEPTH, 1024], F32),
    "pool_w": ([DEPTH, 4, 256, 256], F32R), "pool_scale": ([DEPTH, 1024], F32), "w_up_a": ([DEPTH, 1024, D], F32R),
    "w_up_b": ([DEPTH, 1024, D], F32R), "w_o": ([DEPTH, D, D], F32R), "norm_moe_g": ([DEPTH, D], F32),
    "w_router": ([DEPTH, D, NE], F32), "w_gate_e": ([DEPTH, NE, D, FF], F32R), "w_up_e": ([DEPTH, NE, D, FF], F32R),
    "w_down_e": ([DEPTH, NE, FF, D], F32R), "final_g": ([D], F32),
}


def build_program(C):
    nc = bass.Bass("TRN2", target_bir_lowering=False)
    nc.dge_precook = False

    def din(name, shape, dt=F32):
        return nc.dram_tensor(name, list(shape), dt, kind="ExternalInput").ap()

    def dint(name, shape, dt=F32):
        return nc.dram_tensor(name, list(shape), dt, kind="Internal").ap()

    xT = din("xT", [D, T])
    Wt = {k: din(k, shp, dt) for k, (shp, dt) in _WSPEC.items()}
    cst = {k: din("c_" + k, v.shape) for k, v in C.items()}
    outT = nc.dram_tensor("outT", [D, T], F32, kind="ExternalOutput").ap()
    projT = dint("projT", [D_IN, T])
    loraA = dint("loraA", [416, T], F32R)
    yfT = dint("yfT", [1024, T])
    yaT = dint("yaT", [1024, T], F32R)
    ybT = dint("ybT", [1024, T], F32R)
    xmT = dint("xmT", [D, T])
    v_tm = dint("v_tm", [T, D], BF16)
    affT = dint("affT", [NE, T])
    posD = dint("posD", [NE, T])
    gateD = dint("gateD", [NE, T])
    yD = dint("yD", [NE, CAP, D], BF16)
    xs = [xT, dint("xA", [D, T]), dint("xB", [D, T])]
    for l in range(DEPTH):
        xin = xs[l]
        xout = xs[l + 1]
        stage_inproj(nc, xin, Wt["norm_mix_g"][l], Wt["w_in"][l], projT)
        stage_lora(nc, projT, Wt["mu_shift"][l], loraA)
        W = {"mu": Wt["mu_shift"][l], "w0": Wt["w0"][l], "wl": Wt["w_lora_up"][l], "a0": Wt["a0"][l], "al": Wt["a_lora_up"][l],
             "gl": Wt["g_lora_up"][l], "k_k": Wt["k_k"][l], "k_a": Wt["k_a"][l], "r_k": Wt["r_k"][l], "lnx_g": Wt["lnx_g"][l],
             "lnx_b": Wt["lnx_b"][l]}
        for hp in range(8):
            stage_rwkv(nc, projT, loraA, yfT, yaT, W, cst, hp_list=[hp])
        stage_pool(nc, projT, Wt["pool_w"][l], Wt["pool_scale"][l], ybT, cst["cnt_inv"])
        stage_mix(nc, xin, projT, yaT, ybT, Wt["w_up_a"][l], Wt["w_up_b"][l], Wt["w_o"][l], Wt["norm_moe_g"][l], Wt["w_router"][l],
                  xmT, v_tm, affT, cst["identf"])
        stage_route(nc, affT, posD, gateD)
        stage_experts(nc, posD, v_tm, Wt["w_gate_e"][l], Wt["w_up_e"][l], Wt["w_down_e"][l], yD, cst["iota_row"])
        stage_combine(nc, posD, gateD, yD, xmT, xout, cst["slot_col"])
    stage_final(nc, xs[DEPTH], Wt["final_g"], outT)
    return nc


def kernel(**inputs):
    C = make_consts_all()
    nc = build_program(C)
    x = np.asarray(inputs["x"], dtype=np.float32)
    in_maps = []
    for b in range(N_CORES):
        m = {"xT": np.ascontiguousarray(x[b].T)}
        for k in _WSPEC:
            a = np.asarray(inputs[k], dtype=np.float32)
            if k == "r_k":
                a = a.reshape(DEPTH, 1024)
            m[k] = np.ascontiguousarray(a)
        for k, v in C.items():
            m["c_" + k] = v
        in_maps.append(m)
    res = run_bass_kernel_spmd(nc, in_maps, core_ids=list(range(N_CORES)))
    out = np.stack([np.ascontiguousarray(np.asarray(res.results[b]["outT"]).T) for b in range(N_CORES)], axis=0)
    return out.astype(np.float32)
```
